# Optimizing a Trainium2 kernel written in Bass

```python
import math
import jax, jax.numpy as jnp
from jax import lax
import numpy as np

D_MODEL = 1024
BATCH = 2
SEQ = 16384
DEPTH = 2

CHUNK = 64
N_MIXERS = 2
N_A_LAYERS = (DEPTH + 1) // 2
N_B_LAYERS = DEPTH // 2
RMS_EPS = 1e-6
LN_EPS = 1e-5

A_HEADS = 8
A_HEAD_DIM = 128
A_WIDTH = A_HEADS * A_HEAD_DIM
IDX_HEADS = 8
IDX_DIM = 64
TOPK_MAX = 256
Q_BLOCK = 128
A_IN_COLS = 4 * A_WIDTH + IDX_HEADS * IDX_DIM + IDX_DIM + IDX_HEADS

REL_BUCKETS = 32
REL_MAX_DIST = 128

SGU_CHUNK = 128
B_WIDTH = 2 * D_MODEL
B_GROUPS = 8
B_GROUP_DIM = B_WIDTH // B_GROUPS
B_IN_COLS = 3 * B_WIDTH

kernel_name = "hybrid_dsa_sgu_streaming_encoder"


def rmsnorm(x, g):
    x32 = x.astype(jnp.float32)
    y = x32 * lax.rsqrt(jnp.mean(x32 * x32, axis=-1, keepdims=True) + RMS_EPS)
    return y.astype(x.dtype) * g


def layernorm(x, g, b):
    x32 = x.astype(jnp.float32)
    mu = jnp.mean(x32, axis=-1, keepdims=True)
    var = jnp.mean(jnp.square(x32 - mu), axis=-1, keepdims=True)
    return ((x32 - mu) * lax.rsqrt(var + LN_EPS)).astype(x.dtype) * g + b


def t5_bucket(rel):
    half = REL_BUCKETS // 2
    max_exact = half // 2
    ret = jnp.where(rel < 0, half, 0)
    n = jnp.abs(rel)
    nf = jnp.maximum(n, 1).astype(jnp.float32)
    large = max_exact + (jnp.log(nf / max_exact) / math.log(REL_MAX_DIST / max_exact)
                         * (half - max_exact)).astype(jnp.int32)
    large = jnp.minimum(large, half - 1)
    return ret + jnp.where(n < max_exact, n, large)


def dsa_mixer(h, w_in, w_out, rel_bias):
    bsz, seq, _ = h.shape
    offs = np.cumsum([A_WIDTH, A_WIDTH, A_WIDTH, A_WIDTH,
                      IDX_HEADS * IDX_DIM, IDX_DIM]).tolist()
    q, k, v, z, qi, ki, wi = jnp.split(h @ w_in, offs, axis=-1)
    q = q.reshape(bsz, seq, A_HEADS, A_HEAD_DIM)
    k = k.reshape(bsz, seq, A_HEADS, A_HEAD_DIM)
    v = v.reshape(bsz, seq, A_HEADS, A_HEAD_DIM)
    qi = qi.reshape(bsz, seq, IDX_HEADS, IDX_DIM).astype(jnp.float32)
    ki = ki.astype(jnp.float32)
    wi = wi.astype(jnp.float32) * (IDX_HEADS ** -0.5)

    topk = min(TOPK_MAX, seq // 4)
    n_blk = seq // Q_BLOCK
    key_chunk = jnp.arange(seq, dtype=jnp.int32) // CHUNK
    attn_scale = A_HEAD_DIM ** -0.5
    idx_scale = IDX_DIM ** -0.5

    def gather_rows(arr, idx):
        return arr[idx]

    def block(i):
        start = i * Q_BLOCK
        qb = lax.dynamic_slice_in_dim(q, start, Q_BLOCK, axis=1)
        qib = lax.dynamic_slice_in_dim(qi, start, Q_BLOCK, axis=1)
        wib = lax.dynamic_slice_in_dim(wi, start, Q_BLOCK, axis=1)
        tpos = start + jnp.arange(Q_BLOCK, dtype=jnp.int32)
        tchunk = tpos // CHUNK
        dots = jnp.einsum('bqhd,bsd->bqhs', qib, ki) * idx_scale
        iscore = jnp.einsum('bqhs,bqh->bqs', jax.nn.relu(dots), wib)
        admissible = key_chunk[None, :] <= tchunk[:, None]
        iscore = jnp.where(admissible[None], iscore, -jnp.inf)
        _, idx = lax.top_k(iscore, topk)
        kg = jax.vmap(gather_rows)(k, idx)
        vg = jax.vmap(gather_rows)(v, idx)
        logits = jnp.einsum('bqhd,bqkhd->bhqk', qb, kg).astype(jnp.float32) * attn_scale
        bias = rel_bias[t5_bucket(tpos[None, :, None] - idx)]
        logits = logits + jnp.transpose(bias, (0, 3, 1, 2)).astype(jnp.float32)
        valid = (idx // CHUNK) <= tchunk[None, :, None]
        logits = jnp.where(valid[:, None], logits, -jnp.inf)
        p = jax.nn.softmax(logits, axis=-1).astype(vg.dtype)
        return jnp.einsum('bhqk,bqkhd->bqhd', p, vg)

    out = lax.map(block, jnp.arange(n_blk, dtype=jnp.int32))
    out = jnp.moveaxis(out, 0, 1).reshape(bsz, seq, A_WIDTH)
    return (out * jax.nn.silu(z)) @ w_out


def sgu_mixer(h, w_in, ln_g, ln_b, w_s, b_s, w_out):
    bsz, seq, _ = h.shape
    u, v, z = jnp.split(h @ w_in, 3, axis=-1)
    v = layernorm(v, ln_g, ln_b)
    n_c = seq // SGU_CHUNK
    vc = v.reshape(bsz, n_c, SGU_CHUNK, B_GROUPS, B_GROUP_DIM)
    tri = jnp.tril(jnp.ones((SGU_CHUNK, SGU_CHUNK), dtype=w_s.dtype))
    ws = w_s * tri[None]
    mixed = jnp.einsum('gts,bcsge->bctge', ws, vc) + jnp.transpose(b_s)[None, None, :, :, None]
    y = u * mixed.reshape(bsz, seq, B_WIDTH)
    return (y * jax.nn.silu(z)) @ w_out


def setup_inputs(seed: int = 0) -> dict:
    key = jax.random.key(seed)
    ks = jax.random.split(key, 14)
    f32 = jnp.float32
    x = jax.random.normal(ks[0], (BATCH, SEQ, D_MODEL), f32)
    norm_g = 1.0 + 0.05 * jax.random.normal(ks[1], (DEPTH, D_MODEL), f32)
    final_g = 1.0 + 0.05 * jax.random.normal(ks[2], (D_MODEL,), f32)
    rel_bias = 0.5 * jax.random.normal(ks[3], (REL_BUCKETS, A_HEADS), f32)
    a_w_in = jax.random.normal(ks[4], (N_A_LAYERS, D_MODEL, A_IN_COLS), f32) * D_MODEL ** -0.5
    a_w_out = jax.random.normal(ks[5], (N_A_LAYERS, A_WIDTH, D_MODEL), f32) * A_WIDTH ** -0.5
    b_w_in = jax.random.normal(ks[6], (N_B_LAYERS, D_MODEL, B_IN_COLS), f32) * D_MODEL ** -0.5
    b_ln_g = 1.0 + 0.05 * jax.random.normal(ks[7], (N_B_LAYERS, B_WIDTH), f32)
    b_ln_b = 0.02 * jax.random.normal(ks[8], (N_B_LAYERS, B_WIDTH), f32)
    b_w_s = jax.random.normal(ks[9], (N_B_LAYERS, B_GROUPS, SGU_CHUNK, SGU_CHUNK), f32) * SGU_CHUNK ** -0.5
    b_b_s = 1.0 + 0.1 * jax.random.normal(ks[10], (N_B_LAYERS, B_GROUPS, SGU_CHUNK), f32)
    b_w_out = jax.random.normal(ks[11], (N_B_LAYERS, B_WIDTH, D_MODEL), f32) * B_WIDTH ** -0.5
    return {"x": x, "norm_g": norm_g, "final_g": final_g, "rel_bias": rel_bias,
            "a_w_in": a_w_in, "a_w_out": a_w_out,
            "b_w_in": b_w_in, "b_ln_g": b_ln_g, "b_ln_b": b_ln_b,
            "b_w_s": b_w_s, "b_b_s": b_b_s, "b_w_out": b_w_out}


def reference(x, norm_g, final_g, rel_bias, a_w_in, a_w_out,
              b_w_in, b_ln_g, b_ln_b, b_w_s, b_b_s, b_w_out):
    for i in range(DEPTH):
        h = rmsnorm(x, norm_g[i])
        j = i // N_MIXERS
        if i % N_MIXERS == 0:
            x = x + dsa_mixer(h, a_w_in[j], a_w_out[j], rel_bias)
        else:
            x = x + sgu_mixer(h, b_w_in[j], b_ln_g[j], b_ln_b[j],
                              b_w_s[j], b_b_s[j], b_w_out[j])
    return rmsnorm(x, final_g)
```

```python
import contextlib
import math
import numpy as np
import concourse.bass as bass
import concourse.mybir as mybir
from concourse.bass_utils import run_bass_kernel_spmd

F32 = mybir.dt.float32
BF16 = mybir.dt.bfloat16
AF = mybir.ActivationFunctionType
ALU = mybir.AluOpType
AX = mybir.AxisListType

D = 1024
TOPK = 256
RMS_EPS = 1e-6
LN_EPS = 1e-5
KBIS = 16
KVG = 2
KVS = 3
WS = 2
NEG = -30000.0
SAME_ENGINE_SYNC = True
USE_ACT_COUNT = True
ACT_FRAC = 0.5
PV_LAG = 2
NHS_LIMIT = 10


class Buf:
    def __init__(self, name, ap=None):
        self.name = name
        self.ap = ap
        self.w = None
        self.r = {}
        self.dsem = None
        self.dcnt = 0


class Sched:
    def __init__(self, nc, es):
        self.nc = nc
        self.es = es
        self.eng = {"pe": nc.tensor, "act": nc.scalar, "dve": nc.vector, "pool": nc.gpsimd, "sp": nc.sync}
        self.sem = {k: es.enter_context(nc.semaphore("s_" + k)) for k in self.eng}
        self.cnt = {k: 0 for k in self.eng}
        self.seen = {k: {} for k in self.eng}
        self.anchors = []
        self.nwait = 0
        self.nins = 0

    def _semof(self, key):
        if isinstance(key, str):
            return self.sem[key]
        return key.dsem

    def _wait(self, E, key, val):
        if val <= 0:
            return
        if self.seen[E].get(key, 0) >= val:
            return
        self.eng[E].wait_ge(self._semof(key), val)
        self.seen[E][key] = val
        self.nwait += 1

    def _deps(self, E, R, W):
        deps = {}

        def add(tok):
            k, v = tok
            if deps.get(k, 0) < v:
                deps[k] = v
        for b in R:
            if b.w is not None:
                add(b.w)
        for b in W:
            if b.w is not None:
                add(b.w)
            for k, v in b.r.items():
                add((k, v))
        for k, v in deps.items():
            if k == E:
                if E == "pe" or not SAME_ENGINE_SYNC:
                    continue
            self._wait(E, k, v)

    def _mark(self, tok, R, W):
        k, v = tok
        for b in R:
            if b.r.get(k, 0) < v:
                b.r[k] = v
        for b in W:
            b.w = tok
            b.r = {}

    def op(self, E, fn, R=(), W=()):
        self._deps(E, R, W)
        ins = fn(self.eng[E])
        self.cnt[E] += 1
        ins.then_inc(self.sem[E], 1)
        self.nins += 1
        self._mark((E, self.cnt[E]), R, W)
        return ins

    def dma(self, out, in_, anchor, R=(), W=(), Q="sp"):
        self._deps(Q, R, W)
        if anchor.dsem is None:
            anchor.dsem = self.es.enter_context(self.nc.semaphore("d_" + anchor.name))
            self.anchors.append(anchor)
        ins = self.eng[Q].dma_start(out=out, in_=in_)
        anchor.dcnt += 16
        ins.then_inc(anchor.dsem, 16)
        self.nins += 1
        self._mark((anchor, anchor.dcnt), R, W)
        return ins

    def barrier(self):
        toks = [(k, self.cnt[k]) for k in ("pe", "act", "dve", "pool")]
        toks += [(a, a.dcnt) for a in self.anchors]
        for E in self.eng:
            for k, v in toks:
                if k == E:
                    continue
                self._wait(E, k, v)


def build(NBLK, NQ, dbg=False):
    assert NBLK == 4 * NQ
    S = NBLK * 128
    NG = NBLK // 4
    nc = bass.Bass("TRN2", target_bir_lowering=False)

    def din(name, shape):
        return nc.dram_tensor(name, list(shape), F32, kind="ExternalInput").ap()

    xb = din("xb", [S, D])
    xq = din("xq", [NQ * 128, D])
    w_in0 = din("w_in0", [D, 4680])
    w_out0 = din("w_out0", [D, D])
    w_in1 = din("w_in1", [D, 6144])
    w_out1 = din("w_out1", [2048, D])
    g0c_d = din("g0c", [128, 8])
    g1c_d = din("g1c", [128, 8])
    gf_d = din("gf", [128, D])
    lng_d = din("lng", [128, 2048])
    lnb_d = din("lnb", [128, 2048])
    bsT_d = din("bsT", [128, 8])
    wsT_d = din("wsT", [128, 8, 128])
    tri_d = din("tri", [128, 128])
    biasT_d = din("biasT", [128, 5 * 8 * 128])
    cb_d = din("cb", [128, 8])
    admis_d = din("admis", [128, 512])
    ident_d = din("ident", [128, 128])
    out_d = nc.dram_tensor("out", [NQ * 128, D], F32, kind="ExternalOutput").ap()
    if dbg:
        dbg_x1 = nc.dram_tensor("dbg_x1", [NQ * 128, D], F32, kind="ExternalOutput").ap()
        dbg_thr = nc.dram_tensor("dbg_thr", [NQ * 128, 4], F32, kind="ExternalOutput").ap()

    if dbg:
        dbg_isc = nc.dram_tensor("dbg_isc", [NQ * 128, S], F32, kind="ExternalOutput").ap()
        kT_scr = nc.dram_tensor("kT_scr", [NBLK, 128, 1024], BF16, kind="ExternalOutput").ap()
        v_scr = nc.dram_tensor("v_scr", [NBLK, 128, 1032], BF16, kind="ExternalOutput").ap()
    else:
        kT_scr = nc.dram_tensor("kT_scr", [NBLK, 128, 1024], BF16).ap()
        v_scr = nc.dram_tensor("v_scr", [NBLK, 128, 1032], BF16).ap()
    wq_scr = nc.dram_tensor("wq_scr", [5, 128, 8, 512], BF16).ap()
    wo0_scr = nc.dram_tensor("wo0_scr", [2, 128, 8, 512], BF16).ap()
    w1_scr = nc.dram_tensor("w1_scr", [12, 128, 8, 512], BF16).ap()
    wo1_scr = nc.dram_tensor("wo1_scr", [4, 128, 8, 512], BF16).ap()
    scrK = Buf("scrK")
    scrV = Buf("scrV")
    scrW = Buf("scrW")

    es = contextlib.ExitStack()
    with es:
        Sx = Sched(nc, es)
        op = Sx.op
        dma = Sx.dma

        def sb(name, shape, dt, stack=es):
            t = stack.enter_context(nc.sbuf_tensor("sb_" + name, list(shape), dt))
            return Buf(name, t)

        banks = []
        for i in range(8):
            t = es.enter_context(nc.psum_tensor("bank%d" % i, [128, 512], F32))
            banks.append(Buf("bank%d" % i, t))

        def bk(i):
            return banks[i].ap

        def bkb(i):
            return banks[i].ap[:, 0:512].bitcast(BF16)

        ident_f = sb("ident_f", [128, 128], F32)
        ident_b = sb("ident_b", [128, 128], BF16)
        ident4 = sb("ident4", [128, 512], BF16)
        gf = sb("gf", [128, D], F32)
        lng = sb("lng", [128, 2048], F32)
        lnb = sb("lnb", [128, 2048], F32)
        bsT = sb("bsT", [128, 8], F32)
        wsTm = sb("wsTm", [128, 8, 128], BF16)
        cb = sb("cb", [128, 8], F32)
        g0c = sb("g0c", [128, 8], F32)
        g1c = sb("g1c", [128, 8], F32)
        admis = sb("admis", [128, 512], F32)
        delta = sb("delta", [128, 5, 8, 128], BF16)
        wwi = sb("wwi", [128, 8, 8], BF16)
        kiT = sb("kiT", [128, S], BF16)
        cpow = sb("cpow", [128, KBIS + 1], F32)
        junk_act = sb("junk_act", [128, 2], BF16)
        junk_dve = sb("junk_dve", [128, 2], BF16)
        constA = Buf("constA")

        consts = []

        def cload(buf, src):
            dma(out=buf.ap[:], in_=src, anchor=constA, W=[buf])
            consts.append(buf)

        cload(ident_f, ident_d[:, :])
        cload(gf, gf_d[:, :])
        cload(lng, lng_d[:, :])
        cload(lnb, lnb_d[:, :])
        cload(bsT, bsT_d[:, :])
        cload(cb, cb_d[:, :])
        cload(g0c, g0c_d[:, :])
        cload(g1c, g1c_d[:, :])
        cload(admis, admis_d[:, :])

        p1 = contextlib.ExitStack()
        es.enter_context(p1)
        wkv = sb("wkv", [128, 8, 2112], BF16, p1)

        with contextlib.ExitStack() as ps:
            stgf = [sb("stgf%d" % i, [128, 6144], F32, ps) for i in range(2)]
            stgb = [sb("stgb%d" % i, [128, 6144], BF16, ps) for i in range(2)]
            wsT_f = sb("wsT_f", [128, 8, 128], F32, ps)
            tri_f = sb("tri_f", [128, 128], F32, ps)
            cload(wsT_f, wsT_d[:, :, :])
            cload(tri_f, tri_d[:, :])
            for b in consts:
                b.w = (constA, constA.dcnt)
            op("dve", lambda e: e.tensor_copy(out=ident_b.ap[:], in_=ident_f.ap[:]), R=[ident_f], W=[ident_b])
            for q in range(4):
                op("dve", lambda e, q=q: e.tensor_copy(out=ident4.ap[:, q * 128:(q + 1) * 128], in_=ident_f.ap[:]),
                   R=[ident_f], W=[ident4])
            for k in range(KBIS + 1):
                op("pool", lambda e, k=k: e.memset(cpow.ap[:, k:k + 1], float(2.0 ** -(k + 1))), W=[cpow])
            for g in range(8):
                op("dve", lambda e, g=g: e.tensor_tensor(out=wsTm.ap[:, g, :], in0=wsT_f.ap[:, g, :], in1=tri_f.ap[:],
                                                         op=ALU.mult), R=[wsT_f, tri_f], W=[wsTm])
            for i in range(5):
                st = stgf[i % 2]
                dma(out=st.ap[:, 0:1024], in_=biasT_d[:, i * 1024:(i + 1) * 1024], anchor=st, W=[st])
                for h in range(8):
                    op("dve", lambda e, i=i, h=h, st=st: e.tensor_scalar(
                        out=delta.ap[:, i, h, :], in0=st.ap[:, h * 128:(h + 1) * 128], scalar1=cb.ap[:, h:h + 1],
                        scalar2=None, op0=ALU.subtract), R=[st, cb], W=[delta])

            nprep = [0]

            def prep_weight(W_ap, K, C, gcol, dest_fn):
                for c in range(K // 128):
                    i = nprep[0] % 2
                    nprep[0] += 1
                    sf, sbb = stgf[i], stgb[i]
                    dma(out=sf.ap[:, 0:C], in_=W_ap[c * 128:(c + 1) * 128, :], anchor=sf, W=[sf])
                    if gcol is not None:
                        op("dve", lambda e, c=c: e.tensor_scalar(out=sbb.ap[:, 0:C], in0=sf.ap[:, 0:C],
                                                                 scalar1=gcol.ap[:, c:c + 1], scalar2=None,
                                                                 op0=ALU.mult), R=[sf, gcol], W=[sbb])
                    else:
                        op("act", lambda e: e.copy(out=sbb.ap[:, 0:C], in_=sf.ap[:, 0:C]), R=[sf], W=[sbb])
                    dest_fn(c, sbb)

            def units_store(scr, u0, nu, c, sbb, col0):
                dma(out=scr[u0:u0 + nu, :, c, :].rearrange("u p n -> p u n"),
                    in_=sbb.ap[:, col0:col0 + nu * 512].rearrange("p (u n) -> p u n", n=512),
                    anchor=sbb, R=[sbb], W=[scrW])

            def dest_in0(c, sbb):
                units_store(wq_scr, 0, 2, c, sbb, 0)
                units_store(wq_scr, 2, 2, c, sbb, 3072)
                units_store(wq_scr, 4, 1, c, sbb, 4096)
                op("pool", lambda e: e.tensor_copy(out=wkv.ap[:, c, 0:2048], in_=sbb.ap[:, 1024:3072]), R=[sbb], W=[wkv])
                op("pool", lambda e: e.tensor_copy(out=wkv.ap[:, c, 2048:2112], in_=sbb.ap[:, 4608:4672]), R=[sbb], W=[wkv])
                op("pool", lambda e: e.tensor_copy(out=wwi.ap[:, c, :], in_=sbb.ap[:, 4672:4680]), R=[sbb], W=[wwi])

            prep_weight(w_in0, 1024, 4680, g0c, dest_in0)
            prep_weight(w_out0, 1024, 1024, None, lambda c, sbb: units_store(wo0_scr, 0, 2, c, sbb, 0))
            prep_weight(w_in1, 1024, 6144, g1c, lambda c, sbb: units_store(w1_scr, 0, 12, c, sbb, 0))

            def dest_out1(kc, sbb):
                kh, c = kc // 8, kc % 8
                units_store(wo1_scr, kh, 1, c, sbb, 0)
                units_store(wo1_scr, 2 + kh, 1, c, sbb, 512)

            prep_weight(w_out1, 2048, 1024, None, dest_out1)
            Sx.barrier()

        with contextlib.ExitStack() as s1:
            xg = [sb("xg%d" % i, [128, 4, D], F32, s1) for i in range(2)]
            xn = sb("xn", [128, 4, D], BF16, s1)
            hT = [sb("hT%d" % i, [128, 8, 512], BF16, s1) for i in range(2)]
            kTs = [sb("kTs%d" % i, [128, 4, 8, 128], BF16, s1) for i in range(2)]
            Vs = [sb("Vs%d" % i, [128, 4, 8, 129], BF16, s1) for i in range(2)]
            st1 = sb("st1", [128, 16], F32, s1)
            for i in range(2):
                op("pool", lambda e, i=i: e.memset(Vs[i].ap[:], 1.0), W=[Vs[i]])
            op("pool", lambda e: e.memset(kiT.ap[64:128, :], 0.0), W=[kiT])
            ev = [0]

            def evac(fn_act, fn_dve, R, W):
                ev[0] += 1
                if ev[0] % 2 == 0:
                    op("act", fn_act, R=R, W=W)
                else:
                    op("dve", fn_dve, R=R, W=W)

            def load_xg(g):
                b = xg[g % 2]
                dma(out=b.ap[:], in_=xb[g * 512:(g + 1) * 512, :].rearrange("(t p) d -> p t d", p=128), anchor=b, W=[b])

            load_xg(0)
            for g in range(NG):
                if g + 1 < NG:
                    load_xg(g + 1)
                xgb = xg[g % 2]
                h_ = hT[g % 2]
                for t in range(4):
                    op("act", lambda e, t=t: e.activation(out=junk_act.ap[:, 0:1].to_broadcast([128, D]), in_=xgb.ap[:, t, :], func=AF.Square,
                                                          accum_out=st1.ap[:, t:t + 1]), R=[xgb], W=[junk_act, st1])
                op("dve", lambda e: e.tensor_scalar(out=st1.ap[:, 4:8], in0=st1.ap[:, 0:4], scalar1=1.0 / D,
                                                    scalar2=RMS_EPS, op0=ALU.mult, op1=ALU.add), R=[st1], W=[st1])
                op("act", lambda e: e.sqrt(out=st1.ap[:, 8:12], in_=st1.ap[:, 4:8]), R=[st1], W=[st1])
                op("dve", lambda e: e.reciprocal(out=st1.ap[:, 12:16], in_=st1.ap[:, 8:12]), R=[st1], W=[st1])
                for t in range(4):
                    op("dve", lambda e, t=t: e.tensor_scalar(out=xn.ap[:, t, :], in0=xgb.ap[:, t, :],
                                                             scalar1=st1.ap[:, 12 + t:13 + t], scalar2=None,
                                                             op0=ALU.mult), R=[xgb, st1], W=[xn])
                    tb = 6 + (t % 2)
                    for c in range(8):
                        op("pe", lambda e, t=t, c=c, tb=tb: e.transpose(out=bkb(tb)[:, c * 128:(c + 1) * 128],
                                                                        in_=xn.ap[:, t, c * 128:(c + 1) * 128],
                                                                        identity=ident_b.ap[:]),
                           R=[xn, ident_b], W=[banks[tb]])
                    evac(lambda e, t=t, tb=tb: e.copy(out=h_.ap[:, :, t * 128:(t + 1) * 128],
                                                      in_=bkb(tb).rearrange("p (c n) -> p c n", n=128)),
                         lambda e, t=t, tb=tb: e.tensor_copy(out=h_.ap[:, :, t * 128:(t + 1) * 128],
                                                             in_=bkb(tb).rearrange("p (c n) -> p c n", n=128)),
                         R=[banks[tb]], W=[h_])
                kb_, vb_ = kTs[g % 2], Vs[g % 2]
                for h in range(8):
                    pb = h % 4
                    for c in range(8):
                        op("pe", lambda e, h=h, c=c, pb=pb: e.matmul(bk(pb)[:, :], lhsT=wkv.ap[:, c, h * 128:(h + 1) * 128],
                                                                     rhs=h_.ap[:, c, :], start=(c == 0), stop=(c == 7)),
                           R=[wkv, h_], W=[banks[pb]])
                    evac(lambda e, h=h, pb=pb: e.copy(out=kb_.ap[:, :, h, :], in_=bk(pb).rearrange("p (b n) -> p b n", n=128)),
                         lambda e, h=h, pb=pb: e.tensor_copy(out=kb_.ap[:, :, h, :], in_=bk(pb).rearrange("p (b n) -> p b n", n=128)),
                         R=[banks[pb]], W=[kb_])
                for t in range(4):
                    for hg in range(2):
                        pb = (t * 2 + hg) % 4
                        for c in range(8):
                            op("pe", lambda e, t=t, hg=hg, c=c, pb=pb: e.matmul(
                                bk(pb)[:, :], lhsT=h_.ap[:, c, t * 128:(t + 1) * 128],
                                rhs=wkv.ap[:, c, 1024 + hg * 512:1024 + (hg + 1) * 512], start=(c == 0), stop=(c == 7)),
                               R=[wkv, h_], W=[banks[pb]])
                        evac(lambda e, t=t, hg=hg, pb=pb: e.copy(out=vb_.ap[:, t, hg * 4:(hg + 1) * 4, 0:128],
                                                                 in_=bk(pb).rearrange("p (h n) -> p h n", n=128)),
                             lambda e, t=t, hg=hg, pb=pb: e.tensor_copy(out=vb_.ap[:, t, hg * 4:(hg + 1) * 4, 0:128],
                                                                        in_=bk(pb).rearrange("p (h n) -> p h n", n=128)),
                             R=[banks[pb]], W=[vb_])
                for c in range(8):
                    op("pe", lambda e, c=c: e.matmul(bk(4)[0:64, :], lhsT=wkv.ap[:, c, 2048:2112], rhs=h_.ap[:, c, :],
                                                     start=(c == 0), stop=(c == 7)), R=[wkv, h_], W=[banks[4]])
                evac(lambda e: e.copy(out=kiT.ap[0:64, g * 512:(g + 1) * 512], in_=bk(4)[0:64, :]),
                     lambda e: e.tensor_copy(out=kiT.ap[0:64, g * 512:(g + 1) * 512], in_=bk(4)[0:64, :]),
                     R=[banks[4]], W=[kiT])
                dma(out=kT_scr[g * 4:(g + 1) * 4].rearrange("b d n -> d b n"),
                    in_=kb_.ap[:].rearrange("p b h n -> p b (h n)"), anchor=kb_, R=[kb_], W=[scrK])
                dma(out=v_scr[g * 4:(g + 1) * 4].rearrange("b s n -> s b n"),
                    in_=vb_.ap[:].rearrange("p b h n -> p b (h n)"), anchor=vb_, R=[vb_], W=[scrV])
            Sx.barrier()
        p1.close()

        big = sb("big", [128, 16384], F32)
        Kc = [sb("Kc%d" % i, [128, KVG, 1024], BF16) for i in range(KVS)]
        Vc = [sb("Vc%d" % i, [128, KVG, 1032], BF16) for i in range(KVS)]
        wslh = [sb("wslh%d" % i, [128, 4, 512], BF16) for i in range(4)]
        xqt = [sb("xqt%d" % i, [128, D], F32) for i in range(2)]
        xnq = sb("xnq", [128, D], BF16)
        hTq = sb("hTq", [128, 8, 128], BF16)
        QT = sb("QT", [128, 8, 128], BF16)
        qiT = sb("qiT", [128, 8, 128], BF16)
        zs = sb("zs", [128, D], F32)
        diagw = sb("diagw", [128, 8, 128], BF16)
        Rb_all = sb("Rb_all", [128, 6, 512], BF16)
        Rb = [Buf("Rb%d" % i, Rb_all.ap[:, i, :]) for i in range(6)]
        Rb_flat = Rb_all.ap[:].rearrange("p a n -> p (a n)")
        bisa = sb("bisa", [128, 8], F32)
        bisc = sb("bisc", [128, 2], F32)
        PT = [sb("PT%d" % i, [128, 512], BF16) for i in range(4)]
        mb = [sb("mb%d" % i, [128, 512], BF16) for i in range(2)]
        sm = sb("sm", [128, 64], F32)
        Wt = sb("Wt", [128, KBIS + 1], F32)
        bis = sb("bis", [128, 8], F32)

        def ov(name, off, n, dt):
            a = big.ap[:, off:off + n]
            if dt == BF16:
                a = a.bitcast(BF16)
            return Buf(name, a)
        o_y = ov("o_y", 0, 512, BF16)
        o_yT = ov("o_yT", 512, 512, BF16)
        o_x1 = ov("o_x1", 1024, 1024, F32)
        o_h1 = ov("o_h1", 2048, 512, BF16)
        o_h1T = ov("o_h1T", 2560, 512, BF16)
        o_v = ov("o_v", 3072, 2048, F32)
        o_u = ov("o_u", 5120, 2048, F32)
        o_vln = ov("o_vln", 7168, 1024, BF16)
        o_um = ov("o_um", 8192, 2048, F32)
        o_zs1 = [ov("o_zs1%d" % i, 10240 + 512 * i, 512, F32) for i in range(2)]
        o_y1 = ov("o_y1", 11264, 1024, BF16)
        o_y1T = ov("o_y1T", 12288, 1024, BF16)
        o_x2 = ov("o_x2", 13312, 1024, F32)
        o_o = ov("o_o", 14336, 1024, F32)

        units = []
        for j in range(NQ):
            units += [wq_scr[0], wq_scr[1], wq_scr[4], wq_scr[2], wq_scr[3], wo0_scr[0], wo0_scr[1]]
            units += [w1_scr[u] for u in (4, 5, 6, 7, 0, 1, 2, 3, 8, 9, 10, 11)]
            units += [wo1_scr[u] for u in range(4)]
        hslots = [(b, b.ap[:]) for b in wslh]
        hslots += [(b, b.ap[:].rearrange("p a (b n) -> p (a b) n", n=512)) for b in Kc]
        hslots += [(b, b.ap[:].rearrange("p a n -> p (a n)")[:, 0:2048].rearrange("p (c n) -> p c n", n=512)) for b in Vc]
        hslots = hslots[:NHS_LIMIT]
        NHS = len(hslots)
        halves = []
        seg_sizes = [5] + [23] * (NQ - 1) + [18]
        ui = 0
        for sg, nu in enumerate(seg_sizes):
            for q in range(2 * nu):
                halves.append((units[ui + q // 2], q % 2, sg, q))
            ui += nu
        assert ui == len(units)
        wst = {"issued": 0, "next": 0, "released": {0}, "occ": [None] * NHS}

        def w_pump():
            consumed = 2 * wst["next"]
            while wst["issued"] < len(halves) and wst["issued"] <= consumed + 9:
                m = wst["issued"]
                uap, hf, sg, q = halves[m]
                sl = q % NHS
                if q >= 4 and sg not in wst["released"]:
                    break
                oc = wst["occ"][sl]
                if oc is not None and oc >= consumed:
                    break
                b, view = hslots[sl]
                dma(out=view, in_=uap[:, 4 * hf:4 * hf + 4, :], anchor=b, R=[scrW], W=[b])
                wst["occ"][sl] = m
                wst["issued"] += 1

        class WU:
            def __init__(self, n):
                self.h = []
                for hf in range(2):
                    uap, hf_, sg, q = halves[2 * n + hf]
                    self.h.append(hslots[q % NHS])

            def c(self, c):
                return self.h[c // 4][1][:, c % 4, :]

            def b(self, c):
                return self.h[c // 4][0]

        def get_unit():
            n = wst["next"]
            w_pump()
            assert wst["issued"] >= 2 * n + 2, "weight half-unit not issued"
            wst["next"] += 1
            return WU(n)

        def kv_slots_free_of_weights():
            consumed = 2 * wst["next"]
            for sl in range(4, NHS):
                oc = wst["occ"][sl]
                assert oc is None or oc < consumed, "K/V slot still holds unconsumed weights"

        kvst = {"issued": 0, "next": 0, "limit": 0}
        kvlist = []
        for j in range(NQ):
            for gk in range((4 * j + 4) // KVG):
                kvlist.append(gk * KVG)

        def kv_prefetch(upto):
            while kvst["issued"] < min(upto, kvst["limit"]):
                m = kvst["issued"]
                kb0 = kvlist[m]
                kbuf, vbuf = Kc[m % KVS], Vc[m % KVS]
                dma(out=kbuf.ap[:], in_=kT_scr[kb0:kb0 + KVG].rearrange("b d n -> d b n"), anchor=kbuf, R=[scrK], W=[kbuf])
                dma(out=vbuf.ap[:], in_=v_scr[kb0:kb0 + KVG].rearrange("b s n -> s b n"), anchor=vbuf, R=[scrV], W=[vbuf])
                kvst["issued"] += 1

        def get_kv():
            n = kvst["next"]
            kvst["next"] += 1
            kv_prefetch(n + KVS - 1)
            return Kc[n % KVS], Vc[n % KVS]

        ev2 = [0]

        def evac2(fn_act, fn_dve, R, W):
            ev2[0] += 1
            if ev2[0] % 2 == 0:
                op("act", fn_act, R=R, W=W)
            else:
                op("dve", fn_dve, R=R, W=W)

        def rms_rstd(src_buf, src_ap, col):
            op("act", lambda e: e.activation(out=junk_act.ap[:, 0:1].to_broadcast([128, D]), in_=src_ap, func=AF.Square,
                                             accum_out=sm.ap[:, col:col + 1]), R=[src_buf], W=[junk_act, sm])
            op("dve", lambda e: e.tensor_scalar(out=sm.ap[:, col + 1:col + 2], in0=sm.ap[:, col:col + 1], scalar1=1.0 / D,
                                                scalar2=RMS_EPS, op0=ALU.mult, op1=ALU.add), R=[sm], W=[sm])
            op("act", lambda e: e.sqrt(out=sm.ap[:, col + 2:col + 3], in_=sm.ap[:, col + 1:col + 2]), R=[sm], W=[sm])
            op("dve", lambda e: e.reciprocal(out=sm.ap[:, col:col + 1], in_=sm.ap[:, col + 2:col + 3]), R=[sm], W=[sm])

        def transpose_to(src_buf, src_ap_fn, nch, dst_buf, dst_ap_fn):
            for r0 in range(0, nch, 8):
                for c in range(r0, r0 + 8):
                    op("pe", lambda e, c=c: e.transpose(out=bkb(7)[:, (c - r0) * 128:(c - r0 + 1) * 128],
                                                        in_=src_ap_fn(c), identity=ident_b.ap[:]),
                       R=[src_buf, ident_b], W=[banks[7]])
                evac2(lambda e: e.copy(out=dst_ap_fn(r0), in_=bkb(7)),
                      lambda e: e.tensor_copy(out=dst_ap_fn(r0), in_=bkb(7)), R=[banks[7]], W=[dst_buf])

        op("pool", lambda e: e.memset(qiT.ap[64:128, :, :], 0.0), W=[qiT])
        dmA = Buf("storeA")
        dmB = Buf("storeB")
        dmC = Buf("storeC")
        dmD = Buf("storeD")

        dma(out=xqt[0].ap[:], in_=xq[0:128, :], anchor=xqt[0], W=[xqt[0]])
        for j in range(NQ):
            n_kb = 4 * j + 4
            n_ch = j + 1
            N = n_ch * 512
            xt = xqt[j % 2]
            if j + 1 < NQ:
                nx = xqt[(j + 1) % 2]
                dma(out=nx.ap[:], in_=xq[(j + 1) * 128:(j + 2) * 128, :], anchor=nx, W=[nx])
            rms_rstd(xt, xt.ap[:], 0)
            op("dve", lambda e: e.tensor_scalar(out=xnq.ap[:], in0=xt.ap[:], scalar1=sm.ap[:, 0:1], scalar2=None,
                                                op0=ALU.mult), R=[xt, sm], W=[xnq])
            transpose_to(xnq, lambda c: xnq.ap[:, c * 128:(c + 1) * 128], 8, hTq,
                         lambda r0: hTq.ap[:].rearrange("p c n -> p (c n)"))
            for u in range(2):
                wu = get_unit()
                pb = u % 4
                first = True
                for h4 in range(4):
                    for c in range(8):
                        op("pe", lambda e, h4=h4, c=c, first=first: e.matmul(
                            bk(pb)[:, h4 * 128:(h4 + 1) * 128], lhsT=wu.c(c)[:, h4 * 128:(h4 + 1) * 128],
                            rhs=hTq.ap[:, c, :], start=first, stop=(c == 7), skip_group_check=True),
                           R=[wu.b(c), hTq], W=[banks[pb]])
                        first = False
                op("act", lambda e, u=u: e.mul(out=QT.ap[:, u * 4:(u + 1) * 4, :].rearrange("p h n -> p (h n)"),
                                               in_=bk(pb)[:, :], mul=128.0 ** -0.5), R=[banks[pb]], W=[QT])
            wu = get_unit()
            for hg in range(2):
                pb = 2 + hg
                first = True
                for h4 in range(4):
                    h = hg * 4 + h4
                    for c in range(8):
                        op("pe", lambda e, h=h, h4=h4, c=c, first=first: e.matmul(
                            bk(pb)[0:64, h4 * 128:(h4 + 1) * 128], lhsT=wu.c(c)[:, h * 64:(h + 1) * 64],
                            rhs=hTq.ap[:, c, :], start=first, stop=(c == 7), skip_group_check=True),
                           R=[wu.b(c), hTq], W=[banks[pb]])
                        first = False
                op("dve", lambda e, hg=hg: e.tensor_scalar(
                    out=qiT.ap[0:64, hg * 4:(hg + 1) * 4, :].rearrange("p h n -> p (h n)"), in0=bk(pb)[0:64, :],
                    scalar1=64.0 ** -0.5, scalar2=None, op0=ALU.mult), R=[banks[pb]], W=[qiT])
            for c in range(8):
                op("pe", lambda e, c=c: e.matmul(bk(4)[:, 0:8], lhsT=hTq.ap[:, c, :], rhs=wwi.ap[:, c, :],
                                                 start=(c == 0), stop=(c == 7)), R=[hTq, wwi], W=[banks[4]])
            op("dve", lambda e: e.tensor_scalar(out=sm.ap[:, 8:16], in0=bk(4)[:, 0:8], scalar1=8.0 ** -0.5, scalar2=None,
                                                op0=ALU.mult), R=[banks[4]], W=[sm])
            for h in range(8):
                op("dve", lambda e, h=h: e.tensor_scalar(out=diagw.ap[:, h, :], in0=ident_f.ap[:],
                                                         scalar1=sm.ap[:, 8 + h:9 + h], scalar2=None, op0=ALU.mult),
                   R=[ident_f, sm], W=[diagw])
            for u in range(2):
                wu = get_unit()
                pb = u % 2
                for c in range(8):
                    op("pe", lambda e, c=c: e.matmul(bk(pb)[:, :], lhsT=hTq.ap[:, c, :], rhs=wu.c(c),
                                                     start=(c == 0), stop=(c == 7)), R=[wu.b(c), hTq], W=[banks[pb]])
                op("act", lambda e, u=u: e.activation(out=zs.ap[:, u * 512:(u + 1) * 512], in_=bk(pb)[:, :], func=AF.Silu),
                   R=[banks[pb]], W=[zs])

            kv_slots_free_of_weights()
            kvst["limit"] = kvst["next"] + n_kb // KVG
            kv_prefetch(kvst["next"] + KVS - 1)
            Sx.barrier()
            tot = n_ch * 8
            DB = (0, 1, 2, 3, 6, 7)
            LAG = 3

            def wsum(n):
                c, h = n // 8, n % 8
                acc = 4 + (c % 2)
                R_ = Rb[n % 6]
                op("pe", lambda e: e.matmul(bk(acc)[:, :], lhsT=diagw.ap[:, h, :], rhs=R_.ap, start=(h == 0),
                                            stop=(h == 7)), R=[diagw, R_], W=[banks[acc]])
                if h == 7:
                    evac2(lambda e: e.copy(out=big.ap[:, c * 512:(c + 1) * 512], in_=bk(acc)[:, :]),
                          lambda e: e.tensor_copy(out=big.ap[:, c * 512:(c + 1) * 512], in_=bk(acc)[:, :]),
                          R=[banks[acc]], W=[big])

            for n in range(tot):
                c, h = n // 8, n % 8
                pb = DB[n % 6]
                R_ = Rb[n % 6]
                op("pe", lambda e: e.matmul(bk(pb)[:, :], lhsT=qiT.ap[:, h, :], rhs=kiT.ap[:, c * 512:(c + 1) * 512],
                                            start=True, stop=True), R=[qiT, kiT], W=[banks[pb]])
                if n % 2 == 0:
                    op("act", lambda e: e.activation(out=R_.ap, in_=bk(pb)[:, :], func=AF.Relu), R=[banks[pb]], W=[R_])
                else:
                    op("dve", lambda e: e.tensor_scalar(out=R_.ap, in0=bk(pb)[:, :], scalar1=0.0, scalar2=None,
                                                        op0=ALU.max), R=[banks[pb]], W=[R_])
                if n >= LAG:
                    wsum(n - LAG)
            for n in range(max(0, tot - LAG), tot):
                wsum(n)

            isc = big.ap[:, 0:N]
            nA = (int(N * ACT_FRAC) // 64) * 64 if USE_ACT_COUNT else 0
            nD = N - nA
            op("dve", lambda e: e.tensor_reduce(out=sm.ap[:, 16:17], in_=big.ap[:, 0:512], axis=AX.X, op=ALU.min), R=[big], W=[sm])
            op("dve", lambda e: e.tensor_reduce(out=sm.ap[:, 17:18], in_=isc, axis=AX.X, op=ALU.max), R=[big], W=[sm])
            op("dve", lambda e: e.tensor_tensor(out=big.ap[:, N - 512:N], in0=big.ap[:, N - 512:N], in1=admis.ap[:],
                                                op=ALU.add), R=[big, admis], W=[big])
            op("dve", lambda e: e.tensor_scalar(out=bis.ap[:, 0:1], in0=sm.ap[:, 16:17], scalar1=-1.0, scalar2=None,
                                                op0=ALU.add), R=[sm], W=[bis])
            op("dve", lambda e: e.tensor_tensor(out=sm.ap[:, 18:19], in0=sm.ap[:, 17:18], in1=sm.ap[:, 16:17],
                                                op=ALU.subtract), R=[sm], W=[sm])
            op("dve", lambda e: e.tensor_scalar(out=sm.ap[:, 18:19], in0=sm.ap[:, 18:19], scalar1=2.0, scalar2=None,
                                                op0=ALU.add), R=[sm], W=[sm])
            op("dve", lambda e: e.tensor_scalar(out=Wt.ap[:], in0=cpow.ap[:], scalar1=sm.ap[:, 18:19], scalar2=None,
                                                op0=ALU.mult), R=[cpow, sm], W=[Wt])
            op("dve", lambda e: e.tensor_tensor(out=bis.ap[:, 1:2], in0=bis.ap[:, 0:1], in1=Wt.ap[:, 0:1], op=ALU.add),
               R=[bis, Wt], W=[bis])
            for k in range(KBIS):
                if nA > 0:
                    npc = 0
                    for p0 in range(nD, N, 3072):
                        sz = min(3072, N - p0)
                        op("act", lambda e, p0=p0, sz=sz, npc=npc: e.activation(
                            out=Rb_flat[:, 0:sz], in_=big.ap[:, p0:p0 + sz], func=AF.Sign, bias=bis.ap[:, 1:2], scale=-1.0,
                            accum_out=bisa.ap[:, npc:npc + 1]), R=[big, bis], W=[bisa] + Rb)
                        npc += 1
                op("dve", lambda e: e.tensor_scalar(out=junk_dve.ap[:, 0:1].to_broadcast([128, nD]), in0=big.ap[:, 0:nD],
                                                    scalar1=bis.ap[:, 1:2], scalar2=None, op0=ALU.is_ge, op1=ALU.add,
                                                    accum_out=bisc.ap[:, 0:1]), R=[big, bis], W=[bisc, junk_dve])
                if nA > 0:
                    if npc > 1:
                        op("dve", lambda e, npc=npc: e.tensor_reduce(out=bisa.ap[:, 7:8], in_=bisa.ap[:, 0:npc], axis=AX.X,
                                                                     op=ALU.add), R=[bisa], W=[bisa])
                    sa_col = 7 if npc > 1 else 0
                    op("dve", lambda e: e.scalar_tensor_tensor(out=bisc.ap[:, 1:2], in0=bisc.ap[:, 0:1], scalar=2.0,
                                                               in1=bisa.ap[:, sa_col:sa_col + 1], op0=ALU.mult,
                                                               op1=ALU.subtract), R=[bisc, bisa], W=[bisc])
                    op("dve", lambda e, k=k: e.tensor_scalar(out=bis.ap[:, 3:4], in0=bisc.ap[:, 1:2],
                                                             scalar1=float(2 * TOPK - 1 - nA), scalar2=Wt.ap[:, k:k + 1],
                                                             op0=ALU.is_ge, op1=ALU.mult), R=[bisc, Wt], W=[bis])
                else:
                    op("dve", lambda e, k=k: e.tensor_scalar(out=bis.ap[:, 3:4], in0=bisc.ap[:, 0:1], scalar1=TOPK - 0.5,
                                                             scalar2=Wt.ap[:, k:k + 1], op0=ALU.is_ge, op1=ALU.mult),
                       R=[bisc, Wt], W=[bis])
                op("dve", lambda e, k=k: e.scalar_tensor_tensor(out=bis.ap[:, 1:2], in0=bis.ap[:, 1:2],
                                                                scalar=Wt.ap[:, k + 1:k + 2], in1=bis.ap[:, 3:4],
                                                                op0=ALU.subtract, op1=ALU.add), R=[bis, Wt], W=[bis])
            op("dve", lambda e: e.tensor_tensor(out=bis.ap[:, 0:1], in0=bis.ap[:, 1:2], in1=Wt.ap[:, KBIS:KBIS + 1],
                                                op=ALU.subtract), R=[bis, Wt], W=[bis])
            if dbg:
                dma(out=dbg_thr[j * 128:(j + 1) * 128, :], in_=bis.ap[:, 0:4], anchor=dmB, R=[bis])
                dma(out=dbg_isc[j * 128:(j + 1) * 128, 0:N], in_=big.ap[:, 0:N], anchor=dmD, R=[big])

            obank = lambda h: 4 + h // 3
            ooff = lambda h: (h % 3) * 129
            steps = []
            pend = []

            def emit_pv(item):
                kb, hg, kbuf, vbuf, kbl, pt = item
                for h4 in range(4):
                    h = hg * 4 + h4
                    ob = obank(h)
                    first = (kb == 0 and h % 3 == 0)
                    op("pe", lambda e, h=h, h4=h4, ob=ob, first=first: e.matmul(
                        bk(ob)[:, ooff(h):ooff(h) + 129], lhsT=pt.ap[:, h4 * 128:(h4 + 1) * 128],
                        rhs=vbuf.ap[:, kbl, h * 129:(h + 1) * 129], start=first, stop=(kb == n_kb - 1),
                        skip_group_check=True), R=[pt, vbuf], W=[banks[ob]])

            nqk = 0
            for gk in range(n_kb // KVG):
                kbuf, vbuf = get_kv()
                for kbl in range(KVG):
                    kb = gk * KVG + kbl
                    c = kb // 4
                    if kb % 4 == 0:
                        mbuf = mb[c % 2]
                        op("dve", lambda e, c=c, mbuf=mbuf: e.tensor_scalar(
                            out=mbuf.ap[:], in0=big.ap[:, c * 512:(c + 1) * 512], scalar1=bis.ap[:, 0:1], scalar2=NEG,
                            op0=ALU.is_lt, op1=ALU.mult), R=[big, bis], W=[mbuf])
                    mbuf = mb[c % 2]
                    near_i = kb - (4 * j - 1)
                    near = near_i >= 0
                    for hg in range(2):
                        lb = nqk % 4
                        pt = PT[nqk % 4]
                        nqk += 1
                        op("pe", lambda e, lb=lb, kb=kb, mbuf=mbuf: e.matmul(
                            bk(lb)[:, :], lhsT=mbuf.ap[:, (kb % 4) * 128:(kb % 4 + 1) * 128], rhs=ident4.ap[:],
                            start=True, stop=False, skip_group_check=True), R=[mbuf, ident4], W=[banks[lb]])
                        for h4 in range(4):
                            h = hg * 4 + h4
                            op("pe", lambda e, lb=lb, h=h, h4=h4, kbl=kbl, kbuf=kbuf, near=near: e.matmul(
                                bk(lb)[:, h4 * 128:(h4 + 1) * 128], lhsT=kbuf.ap[:, kbl, h * 128:(h + 1) * 128],
                                rhs=QT.ap[:, h, :], start=False, stop=(not near), skip_group_check=True),
                               R=[kbuf, QT], W=[banks[lb]])
                        if near:
                            for h4 in range(4):
                                h = hg * 4 + h4
                                op("pe", lambda e, lb=lb, h=h, h4=h4, near_i=near_i: e.matmul(
                                    bk(lb)[:, h4 * 128:(h4 + 1) * 128], lhsT=ident_b.ap[:], rhs=delta.ap[:, near_i, h, :],
                                    start=False, stop=True, skip_group_check=True), R=[ident_b, delta], W=[banks[lb]])
                        op("act", lambda e, lb=lb, pt=pt: e.activation(out=pt.ap[:], in_=bk(lb)[:, :], func=AF.Exp),
                           R=[banks[lb]], W=[pt])
                        pend.append((kb, hg, kbuf, vbuf, kbl, pt))
                        if len(pend) > PV_LAG:
                            emit_pv(pend.pop(0))
            while pend:
                emit_pv(pend.pop(0))
            wst["released"].add(j + 1)
            w_pump()

            Sx.barrier()
            for b3, nh in ((4, 3), (5, 3), (6, 2)):
                h0 = (b3 - 4) * 3
                op("dve", lambda e, b3=b3, nh=nh, h0=h0: e.reciprocal(
                    out=sm.ap[:, 24 + h0:24 + h0 + nh],
                    in_=bk(b3)[:, 0:nh * 129].rearrange("p (h n) -> p h n", n=129)[:, :, 128]),
                   R=[banks[b3]], W=[sm])
            for h in range(8):
                op("dve", lambda e, h=h: e.scalar_tensor_tensor(
                    out=o_y.ap[:, h * 128:(h + 1) * 128], in0=bk(obank(h))[:, ooff(h):ooff(h) + 128],
                    scalar=sm.ap[:, 24 + h:25 + h], in1=zs.ap[:, h * 128:(h + 1) * 128], op0=ALU.mult, op1=ALU.mult),
                   R=[banks[obank(h)], sm, zs], W=[o_y])
            transpose_to(o_y, lambda c: o_y.ap[:, c * 128:(c + 1) * 128], 8, o_yT, lambda r0: o_yT.ap[:, :])
            for nh in range(2):
                wu = get_unit()
                pb = nh
                for c in range(8):
                    op("pe", lambda e, c=c: e.matmul(bk(pb)[:, :], lhsT=o_yT.ap[:, c * 128:(c + 1) * 128], rhs=wu.c(c),
                                                     start=(c == 0), stop=(c == 7)), R=[wu.b(c), o_yT], W=[banks[pb]])
                op("dve", lambda e, nh=nh: e.tensor_tensor(out=o_x1.ap[:, nh * 512:(nh + 1) * 512], in0=bk(pb)[:, :],
                                                           in1=xt.ap[:, nh * 512:(nh + 1) * 512], op=ALU.add),
                   R=[banks[pb], xt], W=[o_x1])
            if dbg:
                dma(out=dbg_x1[j * 128:(j + 1) * 128, :], in_=o_x1.ap[:, :], anchor=dmC, R=[o_x1])

            rms_rstd(o_x1, o_x1.ap[:, :], 32)
            op("dve", lambda e: e.tensor_scalar(out=o_h1.ap[:, :], in0=o_x1.ap[:, :], scalar1=sm.ap[:, 32:33], scalar2=None,
                                                op0=ALU.mult), R=[o_x1, sm], W=[o_h1])
            transpose_to(o_h1, lambda c: o_h1.ap[:, c * 128:(c + 1) * 128], 8, o_h1T, lambda r0: o_h1T.ap[:, :])

            def proj_unit(pb):
                wu = get_unit()
                for c in range(8):
                    op("pe", lambda e, c=c: e.matmul(bk(pb)[:, :], lhsT=o_h1T.ap[:, c * 128:(c + 1) * 128], rhs=wu.c(c),
                                                     start=(c == 0), stop=(c == 7)), R=[wu.b(c), o_h1T], W=[banks[pb]])
            for uu in range(4):
                pb = uu % 3
                proj_unit(pb)
                op("act", lambda e, uu=uu, pb=pb: e.copy(out=o_v.ap[:, uu * 512:(uu + 1) * 512], in_=bk(pb)[:, :]),
                   R=[banks[pb]], W=[o_v])
                op("dve", lambda e, uu=uu: e.bn_stats(out=sm.ap[:, 36 + 6 * uu:42 + 6 * uu], in_=o_v.ap[:, uu * 512:(uu + 1) * 512]),
                   R=[o_v], W=[sm])
            op("dve", lambda e: e.bn_aggr(out=sm.ap[:, 60:62], in_=sm.ap[:, 36:60]), R=[sm], W=[sm])
            op("dve", lambda e: e.tensor_scalar(out=sm.ap[:, 62:63], in0=sm.ap[:, 61:62], scalar1=LN_EPS, scalar2=None,
                                                op0=ALU.add), R=[sm], W=[sm])
            op("act", lambda e: e.sqrt(out=sm.ap[:, 63:64], in_=sm.ap[:, 62:63]), R=[sm], W=[sm])
            op("dve", lambda e: e.reciprocal(out=sm.ap[:, 62:63], in_=sm.ap[:, 63:64]), R=[sm], W=[sm])
            op("dve", lambda e: e.tensor_scalar(out=o_v.ap[:, :], in0=o_v.ap[:, :], scalar1=sm.ap[:, 60:61],
                                                scalar2=sm.ap[:, 62:63], op0=ALU.subtract, op1=ALU.mult), R=[o_v, sm], W=[o_v])
            op("dve", lambda e: e.tensor_tensor(out=o_v.ap[:, :], in0=o_v.ap[:, :], in1=lng.ap[:], op=ALU.mult),
               R=[o_v, lng], W=[o_v])
            op("dve", lambda e: e.tensor_tensor(out=o_vln.ap[:, :], in0=o_v.ap[:, :], in1=lnb.ap[:], op=ALU.add),
               R=[o_v, lnb], W=[o_vln])
            for g in range(8):
                mbk = 3 + g // 2
                op("pe", lambda e, g=g, mbk=mbk: e.matmul(bk(mbk)[:, (g % 2) * 256:(g % 2 + 1) * 256], lhsT=wsTm.ap[:, g, :],
                                                          rhs=o_vln.ap[:, g * 256:(g + 1) * 256], start=(g % 2 == 0),
                                                          stop=True, skip_group_check=True), R=[wsTm, o_vln], W=[banks[mbk]])
            for uu in range(4):
                pb = uu % 3
                proj_unit(pb)
                op("act", lambda e, uu=uu, pb=pb: e.copy(out=o_u.ap[:, uu * 512:(uu + 1) * 512], in_=bk(pb)[:, :]),
                   R=[banks[pb]], W=[o_u])
            for g in range(8):
                mbk = 3 + g // 2
                op("dve", lambda e, g=g, mbk=mbk: e.scalar_tensor_tensor(
                    out=o_um.ap[:, g * 256:(g + 1) * 256], in0=bk(mbk)[:, (g % 2) * 256:(g % 2 + 1) * 256],
                    scalar=bsT.ap[:, g:g + 1], in1=o_u.ap[:, g * 256:(g + 1) * 256], op0=ALU.add, op1=ALU.mult),
                   R=[banks[mbk], bsT, o_u], W=[o_um])
            for uu in range(4):
                pb = uu % 3
                proj_unit(pb)
                zb = o_zs1[uu % 2]
                op("act", lambda e, pb=pb, zb=zb: e.activation(out=zb.ap[:, :], in_=bk(pb)[:, :], func=AF.Silu),
                   R=[banks[pb]], W=[zb])
                op("dve", lambda e, uu=uu, zb=zb: e.tensor_tensor(out=o_y1.ap[:, uu * 512:(uu + 1) * 512], in0=zb.ap[:, :],
                                                                   in1=o_um.ap[:, uu * 512:(uu + 1) * 512], op=ALU.mult),
                   R=[zb, o_um], W=[o_y1])
            transpose_to(o_y1, lambda c: o_y1.ap[:, c * 128:(c + 1) * 128], 16, o_y1T,
                         lambda r0: o_y1T.ap[:, r0 * 128:(r0 + 8) * 128])
            for nh in range(2):
                pb = nh
                for kh in range(2):
                    wu = get_unit()
                    for c in range(8):
                        kc = kh * 8 + c
                        op("pe", lambda e, c=c, kc=kc: e.matmul(bk(pb)[:, :], lhsT=o_y1T.ap[:, kc * 128:(kc + 1) * 128],
                                                                rhs=wu.c(c), start=(kc == 0), stop=(kc == 15)),
                           R=[wu.b(c), o_y1T], W=[banks[pb]])
                op("dve", lambda e, nh=nh: e.tensor_tensor(out=o_x2.ap[:, nh * 512:(nh + 1) * 512], in0=bk(pb)[:, :],
                                                           in1=o_x1.ap[:, nh * 512:(nh + 1) * 512], op=ALU.add),
                   R=[banks[pb], o_x1], W=[o_x2])
            rms_rstd(o_x2, o_x2.ap[:, :], 20)
            op("dve", lambda e: e.scalar_tensor_tensor(out=o_o.ap[:, :], in0=o_x2.ap[:, :], scalar=sm.ap[:, 20:21],
                                                       in1=gf.ap[:], op0=ALU.mult, op1=ALU.mult), R=[o_x2, sm, gf], W=[o_o])
            dma(out=out_d[j * 128:(j + 1) * 128, :], in_=o_o.ap[:, :], anchor=dmA, R=[o_o])
        Sx.barrier()
        print("instructions:", Sx.nins, "waits:", Sx.nwait, "sems:", len(Sx.anchors) + 5)
    return nc


def _t5_bucket_np(rel):
    import jax
    import jax.numpy as jnp
    with jax.default_device(jax.devices("cpu")[0]):
        return _t5_bucket_cpu(jnp, rel)


def _t5_bucket_cpu(jnp, rel):
    rel = jnp.asarray(rel, dtype=jnp.int32)
    half = 16
    max_exact = 8
    ret = jnp.where(rel < 0, half, 0)
    n = jnp.abs(rel)
    nf = jnp.maximum(n, 1).astype(jnp.float32)
    large = max_exact + (jnp.log(nf / max_exact) / math.log(128 / max_exact) * (half - max_exact)).astype(jnp.int32)
    large = jnp.minimum(large, half - 1)
    return np.asarray(ret + jnp.where(n < max_exact, n, large))


_NC_CACHE = {}


def _host_inputs(x, norm_g, final_g, rel_bias, a_w_in, a_w_out, b_w_in, b_ln_g, b_ln_b, b_w_s, b_b_s, b_w_out):
    B, S, _ = x.shape
    NBLK = S // 128
    NQ = NBLK // 4
    f = np.float32
    bc = lambda v, n: np.ascontiguousarray(np.broadcast_to(np.asarray(v, f)[None, :], (128, n)))
    common = {
        "w_in0": np.ascontiguousarray(a_w_in[0], f), "w_out0": np.ascontiguousarray(a_w_out[0], f),
        "w_in1": np.ascontiguousarray(b_w_in[0], f), "w_out1": np.ascontiguousarray(b_w_out[0], f),
        "g0c": np.ascontiguousarray(np.asarray(norm_g[0], f).reshape(8, 128).T),
        "g1c": np.ascontiguousarray(np.asarray(norm_g[1], f).reshape(8, 128).T),
        "gf": bc(final_g, D), "lng": bc(b_ln_g[0], 2048), "lnb": bc(b_ln_b[0], 2048),
        "bsT": np.ascontiguousarray(np.asarray(b_b_s[0], f).T),
        "wsT": np.ascontiguousarray(np.transpose(np.asarray(b_w_s[0], f), (2, 0, 1))),
        "tri": np.ascontiguousarray(np.triu(np.ones((128, 128), f))),
        "cb": bc(np.asarray(rel_bias, f)[15], 8),
        "ident": np.eye(128, dtype=f),
    }
    s_loc = np.arange(128)[:, None]
    t_loc = np.arange(128)[None, :]
    rb = np.asarray(rel_bias, f)
    in_maps = []
    for c in range(8):
        b, r = c // 4, c % 4
        m = dict(common)
        m["xb"] = np.ascontiguousarray(x[b], f)
        xr = np.asarray(x[b], f).reshape(NBLK, 128, D)
        m["xq"] = np.ascontiguousarray(xr[r::4].reshape(NQ * 128, D))
        bt = np.zeros((128, 5, 8, 128), f)
        for i in range(5):
            rel = (t_loc - s_loc) - 128 * (i - 1 - r)
            bidx = _t5_bucket_np(rel)
            bt[:, i, :, :] = np.transpose(rb[bidx], (0, 2, 1))
        m["biasT"] = np.ascontiguousarray(bt.reshape(128, 5 * 8 * 128))
        ad = np.zeros((128, 4, 128), f)
        for rp in range(4):
            if rp > r:
                ad[:, rp, :] = -1e30
            elif rp == r:
                ad[:64, rp, 64:] = -1e30
        m["admis"] = np.ascontiguousarray(ad.reshape(128, 512))
        in_maps.append(m)
    return in_maps, NBLK, NQ


def kernel(x, norm_g, final_g, rel_bias, a_w_in, a_w_out, b_w_in, b_ln_g, b_ln_b, b_w_s, b_b_s, b_w_out, _dbg=False):
    x = np.asarray(x)
    in_maps, NBLK, NQ = _host_inputs(x, norm_g, final_g, rel_bias, a_w_in, a_w_out, b_w_in, b_ln_g, b_ln_b,
                                     b_w_s, b_b_s, b_w_out)
    key = (NBLK, NQ, _dbg)
    if key not in _NC_CACHE:
        _NC_CACHE[key] = build(NBLK, NQ, dbg=_dbg)
    nc = _NC_CACHE[key]
    res = run_bass_kernel_spmd(nc, in_maps, core_ids=list(range(8)))
    B, S, _ = x.shape
    out = np.zeros((B, NBLK, 128, D), np.float32)
    for c in range(8):
        b, r = c // 4, c % 4
        out[b, r::4] = np.asarray(res.results[c]["out"]).reshape(NQ, 128, D)
    out = out.reshape(B, S, D)
    if _dbg:
        return out, res
    return out
```

```python
import contextlib
import math
import numpy as np
import concourse.bass as bass
import concourse.mybir as mybir
from concourse.bass_utils import run_bass_kernel_spmd

F32 = mybir.dt.float32
BF16 = mybir.dt.bfloat16
AF = mybir.ActivationFunctionType
ALU = mybir.AluOpType
AX = mybir.AxisListType

D = 1024
TOPK = 256
RMS_EPS = 1e-6
LN_EPS = 1e-5
KBIS = 16
KVG = 2
KVS = 3
WS = 2
NEG = -30000.0
SAME_ENGINE_SYNC = True
USE_ACT_COUNT = True
ACT_FRAC = 0.5
PV_LAG = 3
NHS_LIMIT = 10


class Buf:
    def __init__(self, name, ap=None):
        self.name = name
        self.ap = ap
        self.w = None
        self.r = {}
        self.dsem = None
        self.dcnt = 0


class Sched:
    def __init__(self, nc, es):
        self.nc = nc
        self.es = es
        self.eng = {"pe": nc.tensor, "act": nc.scalar, "dve": nc.vector, "pool": nc.gpsimd, "sp": nc.sync}
        self.sem = {k: es.enter_context(nc.semaphore("s_" + k)) for k in self.eng}
        self.cnt = {k: 0 for k in self.eng}
        self.seen = {k: {} for k in self.eng}
        self.anchors = []
        self.nwait = 0
        self.nins = 0

    def _semof(self, key):
        if isinstance(key, str):
            return self.sem[key]
        return key.dsem

    def _wait(self, E, key, val):
        if val <= 0:
            return
        if self.seen[E].get(key, 0) >= val:
            return
        self.eng[E].wait_ge(self._semof(key), val)
        self.seen[E][key] = val
        self.nwait += 1

    def _deps(self, E, R, W):
        deps = {}

        def add(tok):
            k, v = tok
            if deps.get(k, 0) < v:
                deps[k] = v
        for b in R:
            if b.w is not None:
                add(b.w)
        for b in W:
            if b.w is not None:
                add(b.w)
            for k, v in b.r.items():
                add((k, v))
        for k, v in deps.items():
            if k == E:
                if E == "pe" or not SAME_ENGINE_SYNC:
                    continue
            self._wait(E, k, v)

    def _mark(self, tok, R, W):
        k, v = tok
        for b in R:
            if b.r.get(k, 0) < v:
                b.r[k] = v
        for b in W:
            b.w = tok
            b.r = {}

    def op(self, E, fn, R=(), W=()):
        self._deps(E, R, W)
        ins = fn(self.eng[E])
        self.cnt[E] += 1
        ins.then_inc(self.sem[E], 1)
        self.nins += 1
        self._mark((E, self.cnt[E]), R, W)
        return ins

    def dma(self, out, in_, anchor, R=(), W=(), Q="sp"):
        self._deps(Q, R, W)
        if anchor.dsem is None:
            anchor.dsem = self.es.enter_context(self.nc.semaphore("d_" + anchor.name))
            self.anchors.append(anchor)
        ins = self.eng[Q].dma_start(out=out, in_=in_)
        anchor.dcnt += 16
        ins.then_inc(anchor.dsem, 16)
        self.nins += 1
        self._mark((anchor, anchor.dcnt), R, W)
        return ins

    def barrier(self):
        toks = [(k, self.cnt[k]) for k in ("pe", "act", "dve", "pool")]
        toks += [(a, a.dcnt) for a in self.anchors]
        for E in self.eng:
            for k, v in toks:
                if k == E:
                    continue
                self._wait(E, k, v)


def build(NBLK, NQ, dbg=False):
    assert NBLK == 4 * NQ
    S = NBLK * 128
    NG = NBLK // 4
    nc = bass.Bass("TRN2", target_bir_lowering=False)

    def din(name, shape):
        return nc.dram_tensor(name, list(shape), F32, kind="ExternalInput").ap()

    xb = din("xb", [S, D])
    xq = din("xq", [NQ * 128, D])
    w_in0 = din("w_in0", [D, 4680])
    w_out0 = din("w_out0", [D, D])
    w_in1 = din("w_in1", [D, 6144])
    w_out1 = din("w_out1", [2048, D])
    g0c_d = din("g0c", [128, 8])
    g1c_d = din("g1c", [128, 8])
    gf_d = din("gf", [128, D])
    lng_d = din("lng", [128, 2048])
    lnb_d = din("lnb", [128, 2048])
    bsT_d = din("bsT", [128, 8])
    wsT_d = din("wsT", [128, 8, 128])
    tri_d = din("tri", [128, 128])
    biasT_d = din("biasT", [128, 5 * 8 * 128])
    cb_d = din("cb", [128, 8])
    admis_d = din("admis", [128, 512])
    ident_d = din("ident", [128, 128])
    out_d = nc.dram_tensor("out", [NQ * 128, D], F32, kind="ExternalOutput").ap()
    if dbg:
        dbg_x1 = nc.dram_tensor("dbg_x1", [NQ * 128, D], F32, kind="ExternalOutput").ap()
        dbg_thr = nc.dram_tensor("dbg_thr", [NQ * 128, 4], F32, kind="ExternalOutput").ap()

    if dbg:
        dbg_isc = nc.dram_tensor("dbg_isc", [NQ * 128, S], F32, kind="ExternalOutput").ap()
        kT_scr = nc.dram_tensor("kT_scr", [NBLK, 128, 1024], BF16, kind="ExternalOutput").ap()
        v_scr = nc.dram_tensor("v_scr", [NBLK, 128, 1032], BF16, kind="ExternalOutput").ap()
    else:
        kT_scr = nc.dram_tensor("kT_scr", [NBLK, 128, 1024], BF16).ap()
        v_scr = nc.dram_tensor("v_scr", [NBLK, 128, 1032], BF16).ap()
    wq_scr = nc.dram_tensor("wq_scr", [5, 128, 8, 512], BF16).ap()
    wo0_scr = nc.dram_tensor("wo0_scr", [2, 128, 8, 512], BF16).ap()
    w1_scr = nc.dram_tensor("w1_scr", [12, 128, 8, 512], BF16).ap()
    wo1_scr = nc.dram_tensor("wo1_scr", [4, 128, 8, 512], BF16).ap()
    scrK = Buf("scrK")
    scrV = Buf("scrV")
    scrW = Buf("scrW")

    es = contextlib.ExitStack()
    with es:
        Sx = Sched(nc, es)
        op = Sx.op
        dma = Sx.dma

        def sb(name, shape, dt, stack=es):
            t = stack.enter_context(nc.sbuf_tensor("sb_" + name, list(shape), dt))
            return Buf(name, t)

        banks = []
        for i in range(8):
            t = es.enter_context(nc.psum_tensor("bank%d" % i, [128, 512], F32))
            banks.append(Buf("bank%d" % i, t))

        def bk(i):
            return banks[i].ap

        def bkb(i):
            return banks[i].ap[:, 0:512].bitcast(BF16)

        ident_f = sb("ident_f", [128, 128], F32)
        ident_b = sb("ident_b", [128, 128], BF16)
        ident4 = sb("ident4", [128, 512], BF16)
        gf = sb("gf", [128, D], F32)
        lng = sb("lng", [128, 2048], F32)
        lnb = sb("lnb", [128, 2048], F32)
        bsT = sb("bsT", [128, 8], F32)
        wsTm = sb("wsTm", [128, 8, 128], BF16)
        cb = sb("cb", [128, 8], F32)
        g0c = sb("g0c", [128, 8], F32)
        g1c = sb("g1c", [128, 8], F32)
        admis = sb("admis", [128, 512], F32)
        delta = sb("delta", [128, 5, 8, 128], BF16)
        wwi = sb("wwi", [128, 8, 8], BF16)
        kiT = sb("kiT", [128, S], BF16)
        cpow = sb("cpow", [128, KBIS + 1], F32)
        junk_act = sb("junk_act", [128, 2], BF16)
        junk_dve = sb("junk_dve", [128, 2], BF16)
        constA = Buf("constA")

        consts = []

        def cload(buf, src):
            dma(out=buf.ap[:], in_=src, anchor=constA, W=[buf])
            consts.append(buf)

        cload(ident_f, ident_d[:, :])
        cload(gf, gf_d[:, :])
        cload(lng, lng_d[:, :])
        cload(lnb, lnb_d[:, :])
        cload(bsT, bsT_d[:, :])
        cload(cb, cb_d[:, :])
        cload(g0c, g0c_d[:, :])
        cload(g1c, g1c_d[:, :])
        cload(admis, admis_d[:, :])

        p1 = contextlib.ExitStack()
        es.enter_context(p1)
        wkv = sb("wkv", [128, 8, 2112], BF16, p1)

        with contextlib.ExitStack() as ps:
            stgf = [sb("stgf%d" % i, [128, 6144], F32, ps) for i in range(2)]
            stgb = [sb("stgb%d" % i, [128, 6144], BF16, ps) for i in range(2)]
            wsT_f = sb("wsT_f", [128, 8, 128], F32, ps)
            tri_f = sb("tri_f", [128, 128], F32, ps)
            cload(wsT_f, wsT_d[:, :, :])
            cload(tri_f, tri_d[:, :])
            for b in consts:
                b.w = (constA, constA.dcnt)
            op("dve", lambda e: e.tensor_copy(out=ident_b.ap[:], in_=ident_f.ap[:]), R=[ident_f], W=[ident_b])
            for q in range(4):
                op("dve", lambda e, q=q: e.tensor_copy(out=ident4.ap[:, q * 128:(q + 1) * 128], in_=ident_f.ap[:]),
                   R=[ident_f], W=[ident4])
            for k in range(KBIS + 1):
                op("pool", lambda e, k=k: e.memset(cpow.ap[:, k:k + 1], float(2.0 ** -(k + 1))), W=[cpow])
            for g in range(8):
                op("dve", lambda e, g=g: e.tensor_tensor(out=wsTm.ap[:, g, :], in0=wsT_f.ap[:, g, :], in1=tri_f.ap[:],
                                                         op=ALU.mult), R=[wsT_f, tri_f], W=[wsTm])
            for i in range(5):
                st = stgf[i % 2]
                dma(out=st.ap[:, 0:1024], in_=biasT_d[:, i * 1024:(i + 1) * 1024], anchor=st, W=[st])
                for h in range(8):
                    op("dve", lambda e, i=i, h=h, st=st: e.tensor_scalar(
                        out=delta.ap[:, i, h, :], in0=st.ap[:, h * 128:(h + 1) * 128], scalar1=cb.ap[:, h:h + 1],
                        scalar2=None, op0=ALU.subtract), R=[st, cb], W=[delta])

            nprep = [0]

            def prep_weight(W_ap, K, C, gcol, dest_fn):
                for c in range(K // 128):
                    i = nprep[0] % 2
                    nprep[0] += 1
                    sf, sbb = stgf[i], stgb[i]
                    dma(out=sf.ap[:, 0:C], in_=W_ap[c * 128:(c + 1) * 128, :], anchor=sf, W=[sf])
                    if gcol is not None:
                        op("dve", lambda e, c=c: e.tensor_scalar(out=sbb.ap[:, 0:C], in0=sf.ap[:, 0:C],
                                                                 scalar1=gcol.ap[:, c:c + 1], scalar2=None,
                                                                 op0=ALU.mult), R=[sf, gcol], W=[sbb])
                    else:
                        op("act", lambda e: e.copy(out=sbb.ap[:, 0:C], in_=sf.ap[:, 0:C]), R=[sf], W=[sbb])
                    dest_fn(c, sbb)

            def units_store(scr, u0, nu, c, sbb, col0):
                dma(out=scr[u0:u0 + nu, :, c, :].rearrange("u p n -> p u n"),
                    in_=sbb.ap[:, col0:col0 + nu * 512].rearrange("p (u n) -> p u n", n=512),
                    anchor=sbb, R=[sbb], W=[scrW])

            def dest_in0(c, sbb):
                units_store(wq_scr, 0, 2, c, sbb, 0)
                units_store(wq_scr, 2, 2, c, sbb, 3072)
                units_store(wq_scr, 4, 1, c, sbb, 4096)
                op("pool", lambda e: e.tensor_copy(out=wkv.ap[:, c, 0:2048], in_=sbb.ap[:, 1024:3072]), R=[sbb], W=[wkv])
                op("pool", lambda e: e.tensor_copy(out=wkv.ap[:, c, 2048:2112], in_=sbb.ap[:, 4608:4672]), R=[sbb], W=[wkv])
                op("pool", lambda e: e.tensor_copy(out=wwi.ap[:, c, :], in_=sbb.ap[:, 4672:4680]), R=[sbb], W=[wwi])

            prep_weight(w_in0, 1024, 4680, g0c, dest_in0)
            prep_weight(w_out0, 1024, 1024, None, lambda c, sbb: units_store(wo0_scr, 0, 2, c, sbb, 0))
            prep_weight(w_in1, 1024, 6144, g1c, lambda c, sbb: units_store(w1_scr, 0, 12, c, sbb, 0))

            def dest_out1(kc, sbb):
                kh, c = kc // 8, kc % 8
                units_store(wo1_scr, kh, 1, c, sbb, 0)
                units_store(wo1_scr, 2 + kh, 1, c, sbb, 512)

            prep_weight(w_out1, 2048, 1024, None, dest_out1)
            Sx.barrier()

        with contextlib.ExitStack() as s1:
            xg = [sb("xg%d" % i, [128, 4, D], F32, s1) for i in range(2)]
            xn = sb("xn", [128, 4, D], BF16, s1)
            hT = [sb("hT%d" % i, [128, 8, 512], BF16, s1) for i in range(2)]
            kTs = [sb("kTs%d" % i, [128, 4, 8, 128], BF16, s1) for i in range(2)]
            Vs = [sb("Vs%d" % i, [128, 4, 8, 129], BF16, s1) for i in range(2)]
            st1 = sb("st1", [128, 16], F32, s1)
            for i in range(2):
                op("pool", lambda e, i=i: e.memset(Vs[i].ap[:], 1.0), W=[Vs[i]])
            op("pool", lambda e: e.memset(kiT.ap[64:128, :], 0.0), W=[kiT])
            ev = [0]

            def evac(fn_act, fn_dve, R, W):
                ev[0] += 1
                if ev[0] % 2 == 0:
                    op("act", fn_act, R=R, W=W)
                else:
                    op("dve", fn_dve, R=R, W=W)

            def load_xg(g):
                b = xg[g % 2]
                dma(out=b.ap[:], in_=xb[g * 512:(g + 1) * 512, :].rearrange("(t p) d -> p t d", p=128), anchor=b, W=[b])

            load_xg(0)
            for g in range(NG):
                if g + 1 < NG:
                    load_xg(g + 1)
                xgb = xg[g % 2]
                h_ = hT[g % 2]
                for t in range(4):
                    op("act", lambda e, t=t: e.activation(out=junk_act.ap[:, 0:1].to_broadcast([128, D]), in_=xgb.ap[:, t, :], func=AF.Square,
                                                          accum_out=st1.ap[:, t:t + 1]), R=[xgb], W=[junk_act, st1])
                op("dve", lambda e: e.tensor_scalar(out=st1.ap[:, 4:8], in0=st1.ap[:, 0:4], scalar1=1.0 / D,
                                                    scalar2=RMS_EPS, op0=ALU.mult, op1=ALU.add), R=[st1], W=[st1])
                op("act", lambda e: e.sqrt(out=st1.ap[:, 8:12], in_=st1.ap[:, 4:8]), R=[st1], W=[st1])
                op("dve", lambda e: e.reciprocal(out=st1.ap[:, 12:16], in_=st1.ap[:, 8:12]), R=[st1], W=[st1])
                for t in range(4):
                    op("dve", lambda e, t=t: e.tensor_scalar(out=xn.ap[:, t, :], in0=xgb.ap[:, t, :],
                                                             scalar1=st1.ap[:, 12 + t:13 + t], scalar2=None,
                                                             op0=ALU.mult), R=[xgb, st1], W=[xn])
                    tb = 6 + (t % 2)
                    for c in range(8):
                        op("pe", lambda e, t=t, c=c, tb=tb: e.transpose(out=bkb(tb)[:, c * 128:(c + 1) * 128],
                                                                        in_=xn.ap[:, t, c * 128:(c + 1) * 128],
                                                                        identity=ident_b.ap[:]),
                           R=[xn, ident_b], W=[banks[tb]])
                    evac(lambda e, t=t, tb=tb: e.copy(out=h_.ap[:, :, t * 128:(t + 1) * 128],
                                                      in_=bkb(tb).rearrange("p (c n) -> p c n", n=128)),
                         lambda e, t=t, tb=tb: e.tensor_copy(out=h_.ap[:, :, t * 128:(t + 1) * 128],
                                                             in_=bkb(tb).rearrange("p (c n) -> p c n", n=128)),
                         R=[banks[tb]], W=[h_])
                kb_, vb_ = kTs[g % 2], Vs[g % 2]
                for h in range(8):
                    pb = h % 4
                    for c in range(8):
                        op("pe", lambda e, h=h, c=c, pb=pb: e.matmul(bk(pb)[:, :], lhsT=wkv.ap[:, c, h * 128:(h + 1) * 128],
                                                                     rhs=h_.ap[:, c, :], start=(c == 0), stop=(c == 7)),
                           R=[wkv, h_], W=[banks[pb]])
                    evac(lambda e, h=h, pb=pb: e.copy(out=kb_.ap[:, :, h, :], in_=bk(pb).rearrange("p (b n) -> p b n", n=128)),
                         lambda e, h=h, pb=pb: e.tensor_copy(out=kb_.ap[:, :, h, :], in_=bk(pb).rearrange("p (b n) -> p b n", n=128)),
                         R=[banks[pb]], W=[kb_])
                for t in range(4):
                    for hg in range(2):
                        pb = (t * 2 + hg) % 4
                        for c in range(8):
                            op("pe", lambda e, t=t, hg=hg, c=c, pb=pb: e.matmul(
                                bk(pb)[:, :], lhsT=h_.ap[:, c, t * 128:(t + 1) * 128],
                                rhs=wkv.ap[:, c, 1024 + hg * 512:1024 + (hg + 1) * 512], start=(c == 0), stop=(c == 7)),
                               R=[wkv, h_], W=[banks[pb]])
                        evac(lambda e, t=t, hg=hg, pb=pb: e.copy(out=vb_.ap[:, t, hg * 4:(hg + 1) * 4, 0:128],
                                                                 in_=bk(pb).rearrange("p (h n) -> p h n", n=128)),
                             lambda e, t=t, hg=hg, pb=pb: e.tensor_copy(out=vb_.ap[:, t, hg * 4:(hg + 1) * 4, 0:128],
                                                                        in_=bk(pb).rearrange("p (h n) -> p h n", n=128)),
                             R=[banks[pb]], W=[vb_])
                for c in range(8):
                    op("pe", lambda e, c=c: e.matmul(bk(4)[0:64, :], lhsT=wkv.ap[:, c, 2048:2112], rhs=h_.ap[:, c, :],
                                                     start=(c == 0), stop=(c == 7)), R=[wkv, h_], W=[banks[4]])
                evac(lambda e: e.copy(out=kiT.ap[0:64, g * 512:(g + 1) * 512], in_=bk(4)[0:64, :]),
                     lambda e: e.tensor_copy(out=kiT.ap[0:64, g * 512:(g + 1) * 512], in_=bk(4)[0:64, :]),
                     R=[banks[4]], W=[kiT])
                dma(out=kT_scr[g * 4:(g + 1) * 4].rearrange("b d n -> d b n"),
                    in_=kb_.ap[:].rearrange("p b h n -> p b (h n)"), anchor=kb_, R=[kb_], W=[scrK])
                dma(out=v_scr[g * 4:(g + 1) * 4].rearrange("b s n -> s b n"),
                    in_=vb_.ap[:].rearrange("p b h n -> p b (h n)"), anchor=vb_, R=[vb_], W=[scrV])
            Sx.barrier()
        p1.close()

        big = sb("big", [128, 16384], F32)
        Kc = [sb("Kc%d" % i, [128, KVG, 1024], BF16) for i in range(KVS)]
        Vc = [sb("Vc%d" % i, [128, KVG, 1032], BF16) for i in range(KVS)]
        wslh = [sb("wslh%d" % i, [128, 4, 512], BF16) for i in range(4)]
        xqt = [sb("xqt%d" % i, [128, D], F32) for i in range(2)]
        xnq = sb("xnq", [128, D], BF16)
        hTq = sb("hTq", [128, 8, 128], BF16)
        QT = sb("QT", [128, 8, 128], BF16)
        qiT = sb("qiT", [128, 8, 128], BF16)
        zs = sb("zs", [128, D], F32)
        diagw = sb("diagw", [128, 8, 128], BF16)
        Rb_all = sb("Rb_all", [128, 6, 512], BF16)
        Rb = [Buf("Rb%d" % i, Rb_all.ap[:, i, :]) for i in range(6)]
        Rb_flat = Rb_all.ap[:].rearrange("p a n -> p (a n)")
        bisa = sb("bisa", [128, 8], F32)
        bisc = sb("bisc", [128, 2], F32)
        PT = [sb("PT%d" % i, [128, 512], BF16) for i in range(4)]
        mb = [sb("mb%d" % i, [128, 512], BF16) for i in range(2)]
        sm = sb("sm", [128, 64], F32)
        Wt = sb("Wt", [128, KBIS + 1], F32)
        bis = sb("bis", [128, 8], F32)

        def ov(name, off, n, dt):
            a = big.ap[:, off:off + n]
            if dt == BF16:
                a = a.bitcast(BF16)
            return Buf(name, a)
        o_y = ov("o_y", 0, 512, BF16)
        o_yT = ov("o_yT", 512, 512, BF16)
        o_x1 = ov("o_x1", 1024, 1024, F32)
        o_h1 = ov("o_h1", 2048, 512, BF16)
        o_h1T = ov("o_h1T", 2560, 512, BF16)
        o_v = ov("o_v", 3072, 2048, F32)
        o_u = ov("o_u", 5120, 2048, F32)
        o_vln = ov("o_vln", 7168, 1024, BF16)
        o_um = ov("o_um", 8192, 2048, F32)
        o_zs1 = [ov("o_zs1%d" % i, 10240 + 512 * i, 512, F32) for i in range(2)]
        o_y1 = ov("o_y1", 11264, 1024, BF16)
        o_y1T = ov("o_y1T", 12288, 1024, BF16)
        o_x2 = ov("o_x2", 13312, 1024, F32)
        o_o = ov("o_o", 14336, 1024, F32)

        units = []
        for j in range(NQ):
            units += [wq_scr[0], wq_scr[1], wq_scr[4], wq_scr[2], wq_scr[3], wo0_scr[0], wo0_scr[1]]
            units += [w1_scr[u] for u in (4, 5, 6, 7, 0, 1, 2, 3, 8, 9, 10, 11)]
            units += [wo1_scr[u] for u in range(4)]
        hslots = [(b, b.ap[:]) for b in wslh]
        hslots += [(b, b.ap[:].rearrange("p a (b n) -> p (a b) n", n=512)) for b in Kc]
        hslots += [(b, b.ap[:].rearrange("p a n -> p (a n)")[:, 0:2048].rearrange("p (c n) -> p c n", n=512)) for b in Vc]
        hslots = hslots[:NHS_LIMIT]
        NHS = len(hslots)
        halves = []
        seg_sizes = [5] + [23] * (NQ - 1) + [18]
        ui = 0
        for sg, nu in enumerate(seg_sizes):
            for q in range(2 * nu):
                halves.append((units[ui + q // 2], q % 2, sg, q))
            ui += nu
        assert ui == len(units)
        wst = {"issued": 0, "next": 0, "released": {0}, "occ": [None] * NHS}

        def w_pump():
            consumed = 2 * wst["next"]
            while wst["issued"] < len(halves) and wst["issued"] <= consumed + 9:
                m = wst["issued"]
                uap, hf, sg, q = halves[m]
                sl = q % NHS
                if q >= 4 and sg not in wst["released"]:
                    break
                oc = wst["occ"][sl]
                if oc is not None and oc >= consumed:
                    break
                b, view = hslots[sl]
                dma(out=view, in_=uap[:, 4 * hf:4 * hf + 4, :], anchor=b, R=[scrW], W=[b])
                wst["occ"][sl] = m
                wst["issued"] += 1

        class WU:
            def __init__(self, n):
                self.h = []
                for hf in range(2):
                    uap, hf_, sg, q = halves[2 * n + hf]
                    self.h.append(hslots[q % NHS])

            def c(self, c):
                return self.h[c // 4][1][:, c % 4, :]

            def b(self, c):
                return self.h[c // 4][0]

        def get_unit():
            n = wst["next"]
            w_pump()
            assert wst["issued"] >= 2 * n + 2, "weight half-unit not issued"
            wst["next"] += 1
            return WU(n)

        def kv_slots_free_of_weights():
            consumed = 2 * wst["next"]
            for sl in range(4, NHS):
                oc = wst["occ"][sl]
                assert oc is None or oc < consumed, "K/V slot still holds unconsumed weights"

        kvst = {"issued": 0, "next": 0, "limit": 0}
        kvlist = []
        for j in range(NQ):
            for gk in range((4 * j + 4) // KVG):
                kvlist.append(gk * KVG)

        def kv_prefetch(upto):
            while kvst["issued"] < min(upto, kvst["limit"]):
                m = kvst["issued"]
                kb0 = kvlist[m]
                kbuf, vbuf = Kc[m % KVS], Vc[m % KVS]
                dma(out=kbuf.ap[:], in_=kT_scr[kb0:kb0 + KVG].rearrange("b d n -> d b n"), anchor=kbuf, R=[scrK], W=[kbuf])
                dma(out=vbuf.ap[:], in_=v_scr[kb0:kb0 + KVG].rearrange("b s n -> s b n"), anchor=vbuf, R=[scrV], W=[vbuf])
                kvst["issued"] += 1

        def get_kv():
            n = kvst["next"]
            kvst["next"] += 1
            kv_prefetch(n + KVS - 1)
            return Kc[n % KVS], Vc[n % KVS]

        ev2 = [0]

        def evac2(fn_act, fn_dve, R, W):
            ev2[0] += 1
            if ev2[0] % 2 == 0:
                op("act", fn_act, R=R, W=W)
            else:
                op("dve", fn_dve, R=R, W=W)

        def rms_rstd(src_buf, src_ap, col):
            op("act", lambda e: e.activation(out=junk_act.ap[:, 0:1].to_broadcast([128, D]), in_=src_ap, func=AF.Square,
                                             accum_out=sm.ap[:, col:col + 1]), R=[src_buf], W=[junk_act, sm])
            op("dve", lambda e: e.tensor_scalar(out=sm.ap[:, col + 1:col + 2], in0=sm.ap[:, col:col + 1], scalar1=1.0 / D,
                                                scalar2=RMS_EPS, op0=ALU.mult, op1=ALU.add), R=[sm], W=[sm])
            op("act", lambda e: e.sqrt(out=sm.ap[:, col + 2:col + 3], in_=sm.ap[:, col + 1:col + 2]), R=[sm], W=[sm])
            op("dve", lambda e: e.reciprocal(out=sm.ap[:, col:col + 1], in_=sm.ap[:, col + 2:col + 3]), R=[sm], W=[sm])

        def transpose_to(src_buf, src_ap_fn, nch, dst_buf, dst_ap_fn):
            for r0 in range(0, nch, 8):
                for c in range(r0, r0 + 8):
                    op("pe", lambda e, c=c: e.transpose(out=bkb(7)[:, (c - r0) * 128:(c - r0 + 1) * 128],
                                                        in_=src_ap_fn(c), identity=ident_b.ap[:]),
                       R=[src_buf, ident_b], W=[banks[7]])
                evac2(lambda e: e.copy(out=dst_ap_fn(r0), in_=bkb(7)),
                      lambda e: e.tensor_copy(out=dst_ap_fn(r0), in_=bkb(7)), R=[banks[7]], W=[dst_buf])

        op("pool", lambda e: e.memset(qiT.ap[64:128, :, :], 0.0), W=[qiT])
        dmA = Buf("storeA")
        dmB = Buf("storeB")
        dmC = Buf("storeC")
        dmD = Buf("storeD")

        dma(out=xqt[0].ap[:], in_=xq[0:128, :], anchor=xqt[0], W=[xqt[0]])
        for j in range(NQ):
            n_kb = 4 * j + 4
            n_ch = j + 1
            N = n_ch * 512
            xt = xqt[j % 2]
            if j + 1 < NQ:
                nx = xqt[(j + 1) % 2]
                dma(out=nx.ap[:], in_=xq[(j + 1) * 128:(j + 2) * 128, :], anchor=nx, W=[nx])
            rms_rstd(xt, xt.ap[:], 0)
            op("dve", lambda e: e.tensor_scalar(out=xnq.ap[:], in0=xt.ap[:], scalar1=sm.ap[:, 0:1], scalar2=None,
                                                op0=ALU.mult), R=[xt, sm], W=[xnq])
            transpose_to(xnq, lambda c: xnq.ap[:, c * 128:(c + 1) * 128], 8, hTq,
                         lambda r0: hTq.ap[:].rearrange("p c n -> p (c n)"))
            for u in range(2):
                wu = get_unit()
                pb = u % 4
                first = True
                for h4 in range(4):
                    for c in range(8):
                        op("pe", lambda e, h4=h4, c=c, first=first: e.matmul(
                            bk(pb)[:, h4 * 128:(h4 + 1) * 128], lhsT=wu.c(c)[:, h4 * 128:(h4 + 1) * 128],
                            rhs=hTq.ap[:, c, :], start=first, stop=(c == 7), skip_group_check=True),
                           R=[wu.b(c), hTq], W=[banks[pb]])
                        first = False
                op("act", lambda e, u=u: e.mul(out=QT.ap[:, u * 4:(u + 1) * 4, :].rearrange("p h n -> p (h n)"),
                                               in_=bk(pb)[:, :], mul=128.0 ** -0.5), R=[banks[pb]], W=[QT])
            wu = get_unit()
            for hg in range(2):
                pb = 2 + hg
                first = True
                for h4 in range(4):
                    h = hg * 4 + h4
                    for c in range(8):
                        op("pe", lambda e, h=h, h4=h4, c=c, first=first: e.matmul(
                            bk(pb)[0:64, h4 * 128:(h4 + 1) * 128], lhsT=wu.c(c)[:, h * 64:(h + 1) * 64],
                            rhs=hTq.ap[:, c, :], start=first, stop=(c == 7), skip_group_check=True),
                           R=[wu.b(c), hTq], W=[banks[pb]])
                        first = False
                op("dve", lambda e, hg=hg: e.tensor_scalar(
                    out=qiT.ap[0:64, hg * 4:(hg + 1) * 4, :].rearrange("p h n -> p (h n)"), in0=bk(pb)[0:64, :],
                    scalar1=64.0 ** -0.5, scalar2=None, op0=ALU.mult), R=[banks[pb]], W=[qiT])
            for c in range(8):
                op("pe", lambda e, c=c: e.matmul(bk(4)[:, 0:8], lhsT=hTq.ap[:, c, :], rhs=wwi.ap[:, c, :],
                                                 start=(c == 0), stop=(c == 7)), R=[hTq, wwi], W=[banks[4]])
            op("dve", lambda e: e.tensor_scalar(out=sm.ap[:, 8:16], in0=bk(4)[:, 0:8], scalar1=8.0 ** -0.5, scalar2=None,
                                                op0=ALU.mult), R=[banks[4]], W=[sm])
            for h in range(8):
                op("dve", lambda e, h=h: e.tensor_scalar(out=diagw.ap[:, h, :], in0=ident_f.ap[:],
                                                         scalar1=sm.ap[:, 8 + h:9 + h], scalar2=None, op0=ALU.mult),
                   R=[ident_f, sm], W=[diagw])
            for u in range(2):
                wu = get_unit()
                pb = u % 2
                for c in range(8):
                    op("pe", lambda e, c=c: e.matmul(bk(pb)[:, :], lhsT=hTq.ap[:, c, :], rhs=wu.c(c),
                                                     start=(c == 0), stop=(c == 7)), R=[wu.b(c), hTq], W=[banks[pb]])
                op("act", lambda e, u=u: e.activation(out=zs.ap[:, u * 512:(u + 1) * 512], in_=bk(pb)[:, :], func=AF.Silu),
                   R=[banks[pb]], W=[zs])

            kv_slots_free_of_weights()
            kvst["limit"] = kvst["next"] + n_kb // KVG
            kv_prefetch(kvst["next"] + KVS - 1)
            Sx.barrier()
            tot = n_ch * 8
            DB = (0, 1, 2, 3, 6, 7)
            LAG = 3

            def wsum(n):
                c, h = n // 8, n % 8
                acc = 4 + (c % 2)
                R_ = Rb[n % 6]
                op("pe", lambda e: e.matmul(bk(acc)[:, :], lhsT=diagw.ap[:, h, :], rhs=R_.ap, start=(h == 0),
                                            stop=(h == 7)), R=[diagw, R_], W=[banks[acc]])
                if h == 7:
                    evac2(lambda e: e.copy(out=big.ap[:, c * 512:(c + 1) * 512], in_=bk(acc)[:, :]),
                          lambda e: e.tensor_copy(out=big.ap[:, c * 512:(c + 1) * 512], in_=bk(acc)[:, :]),
                          R=[banks[acc]], W=[big])

            for n in range(tot):
                c, h = n // 8, n % 8
                pb = DB[n % 6]
                R_ = Rb[n % 6]
                op("pe", lambda e: e.matmul(bk(pb)[:, :], lhsT=qiT.ap[:, h, :], rhs=kiT.ap[:, c * 512:(c + 1) * 512],
                                            start=True, stop=True), R=[qiT, kiT], W=[banks[pb]])
                if n % 2 == 0:
                    op("act", lambda e: e.activation(out=R_.ap, in_=bk(pb)[:, :], func=AF.Relu), R=[banks[pb]], W=[R_])
                else:
                    op("dve", lambda e: e.tensor_scalar(out=R_.ap, in0=bk(pb)[:, :], scalar1=0.0, scalar2=None,
                                                        op0=ALU.max), R=[banks[pb]], W=[R_])
                if n >= LAG:
                    wsum(n - LAG)
            for n in range(max(0, tot - LAG), tot):
                wsum(n)

            isc = big.ap[:, 0:N]
            nA = (int(N * ACT_FRAC) // 64) * 64 if USE_ACT_COUNT else 0
            nD = N - nA
            op("dve", lambda e: e.tensor_reduce(out=sm.ap[:, 16:17], in_=big.ap[:, 0:512], axis=AX.X, op=ALU.min), R=[big], W=[sm])
            op("dve", lambda e: e.tensor_reduce(out=sm.ap[:, 17:18], in_=isc, axis=AX.X, op=ALU.max), R=[big], W=[sm])
            op("dve", lambda e: e.tensor_tensor(out=big.ap[:, N - 512:N], in0=big.ap[:, N - 512:N], in1=admis.ap[:],
                                                op=ALU.add), R=[big, admis], W=[big])
            op("dve", lambda e: e.tensor_scalar(out=bis.ap[:, 0:1], in0=sm.ap[:, 16:17], scalar1=-1.0, scalar2=None,
                                                op0=ALU.add), R=[sm], W=[bis])
            op("dve", lambda e: e.tensor_tensor(out=sm.ap[:, 18:19], in0=sm.ap[:, 17:18], in1=sm.ap[:, 16:17],
                                                op=ALU.subtract), R=[sm], W=[sm])
            op("dve", lambda e: e.tensor_scalar(out=sm.ap[:, 18:19], in0=sm.ap[:, 18:19], scalar1=2.0, scalar2=None,
                                                op0=ALU.add), R=[sm], W=[sm])
            op("dve", lambda e: e.tensor_scalar(out=Wt.ap[:], in0=cpow.ap[:], scalar1=sm.ap[:, 18:19], scalar2=None,
                                                op0=ALU.mult), R=[cpow, sm], W=[Wt])
            op("dve", lambda e: e.tensor_tensor(out=bis.ap[:, 1:2], in0=bis.ap[:, 0:1], in1=Wt.ap[:, 0:1], op=ALU.add),
               R=[bis, Wt], W=[bis])
            for k in range(KBIS):
                if nA > 0:
                    npc = 0
                    for p0 in range(nD, N, 3072):
                        sz = min(3072, N - p0)
                        op("act", lambda e, p0=p0, sz=sz, npc=npc: e.activation(
                            out=Rb_flat[:, 0:sz], in_=big.ap[:, p0:p0 + sz], func=AF.Sign, bias=bis.ap[:, 1:2], scale=-1.0,
                            accum_out=bisa.ap[:, npc:npc + 1]), R=[big, bis], W=[bisa] + Rb)
                        npc += 1
                op("dve", lambda e: e.tensor_scalar(out=junk_dve.ap[:, 0:1].to_broadcast([128, nD]), in0=big.ap[:, 0:nD],
                                                    scalar1=bis.ap[:, 1:2], scalar2=None, op0=ALU.is_ge, op1=ALU.add,
                                                    accum_out=bisc.ap[:, 0:1]), R=[big, bis], W=[bisc, junk_dve])
                if nA > 0:
                    if npc > 1:
                        op("dve", lambda e, npc=npc: e.tensor_reduce(out=bisa.ap[:, 7:8], in_=bisa.ap[:, 0:npc], axis=AX.X,
                                                                     op=ALU.add), R=[bisa], W=[bisa])
                    sa_col = 7 if npc > 1 else 0
                    op("dve", lambda e: e.scalar_tensor_tensor(out=bisc.ap[:, 1:2], in0=bisc.ap[:, 0:1], scalar=2.0,
                                                               in1=bisa.ap[:, sa_col:sa_col + 1], op0=ALU.mult,
                                                               op1=ALU.subtract), R=[bisc, bisa], W=[bisc])
                    op("dve", lambda e, k=k: e.tensor_scalar(out=bis.ap[:, 3:4], in0=bisc.ap[:, 1:2],
                                                             scalar1=float(2 * TOPK - 1 - nA), scalar2=Wt.ap[:, k:k + 1],
                                                             op0=ALU.is_ge, op1=ALU.mult), R=[bisc, Wt], W=[bis])
                else:
                    op("dve", lambda e, k=k: e.tensor_scalar(out=bis.ap[:, 3:4], in0=bisc.ap[:, 0:1], scalar1=TOPK - 0.5,
                                                             scalar2=Wt.ap[:, k:k + 1], op0=ALU.is_ge, op1=ALU.mult),
                       R=[bisc, Wt], W=[bis])
                op("dve", lambda e, k=k: e.scalar_tensor_tensor(out=bis.ap[:, 1:2], in0=bis.ap[:, 1:2],
                                                                scalar=Wt.ap[:, k + 1:k + 2], in1=bis.ap[:, 3:4],
                                                                op0=ALU.subtract, op1=ALU.add), R=[bis, Wt], W=[bis])
            op("dve", lambda e: e.tensor_tensor(out=bis.ap[:, 0:1], in0=bis.ap[:, 1:2], in1=Wt.ap[:, KBIS:KBIS + 1],
                                                op=ALU.subtract), R=[bis, Wt], W=[bis])
            if dbg:
                dma(out=dbg_thr[j * 128:(j + 1) * 128, :], in_=bis.ap[:, 0:4], anchor=dmB, R=[bis])
                dma(out=dbg_isc[j * 128:(j + 1) * 128, 0:N], in_=big.ap[:, 0:N], anchor=dmD, R=[big])

            obank = lambda h: 4 + h // 3
            ooff = lambda h: (h % 3) * 129
            steps = []
            pend = []

            def emit_pv(item):
                kb, hg, kbuf, vbuf, kbl, pt = item
                for h4 in range(4):
                    h = hg * 4 + h4
                    ob = obank(h)
                    first = (kb == 0 and h % 3 == 0)
                    op("pe", lambda e, h=h, h4=h4, ob=ob, first=first: e.matmul(
                        bk(ob)[:, ooff(h):ooff(h) + 129], lhsT=pt.ap[:, h4 * 128:(h4 + 1) * 128],
                        rhs=vbuf.ap[:, kbl, h * 129:(h + 1) * 129], start=first, stop=(kb == n_kb - 1),
                        skip_group_check=True), R=[pt, vbuf], W=[banks[ob]])

            nqk = 0
            for gk in range(n_kb // KVG):
                kbuf, vbuf = get_kv()
                for kbl in range(KVG):
                    kb = gk * KVG + kbl
                    c = kb // 4
                    if kb % 4 == 0:
                        mbuf = mb[c % 2]
                        op("dve", lambda e, c=c, mbuf=mbuf: e.tensor_scalar(
                            out=mbuf.ap[:], in0=big.ap[:, c * 512:(c + 1) * 512], scalar1=bis.ap[:, 0:1], scalar2=NEG,
                            op0=ALU.is_lt, op1=ALU.mult), R=[big, bis], W=[mbuf])
                    mbuf = mb[c % 2]
                    near_i = kb - (4 * j - 1)
                    near = near_i >= 0
                    for hg in range(2):
                        lb = nqk % 4
                        pt = PT[nqk % 4]
                        nqk += 1
                        op("pe", lambda e, lb=lb, kb=kb, mbuf=mbuf: e.matmul(
                            bk(lb)[:, :], lhsT=mbuf.ap[:, (kb % 4) * 128:(kb % 4 + 1) * 128], rhs=ident4.ap[:],
                            start=True, stop=False, skip_group_check=True), R=[mbuf, ident4], W=[banks[lb]])
                        for h4 in range(4):
                            h = hg * 4 + h4
                            op("pe", lambda e, lb=lb, h=h, h4=h4, kbl=kbl, kbuf=kbuf, near=near: e.matmul(
                                bk(lb)[:, h4 * 128:(h4 + 1) * 128], lhsT=kbuf.ap[:, kbl, h * 128:(h + 1) * 128],
                                rhs=QT.ap[:, h, :], start=False, stop=(not near), skip_group_check=True),
                               R=[kbuf, QT], W=[banks[lb]])
                        if near:
                            for h4 in range(4):
                                h = hg * 4 + h4
                                op("pe", lambda e, lb=lb, h=h, h4=h4, near_i=near_i: e.matmul(
                                    bk(lb)[:, h4 * 128:(h4 + 1) * 128], lhsT=ident_b.ap[:], rhs=delta.ap[:, near_i, h, :],
                                    start=False, stop=True, skip_group_check=True), R=[ident_b, delta], W=[banks[lb]])
                        op("act", lambda e, lb=lb, pt=pt: e.activation(out=pt.ap[:], in_=bk(lb)[:, :], func=AF.Exp),
                           R=[banks[lb]], W=[pt])
                        pend.append((kb, hg, kbuf, vbuf, kbl, pt))
                        if len(pend) > PV_LAG:
                            emit_pv(pend.pop(0))
            while pend:
                emit_pv(pend.pop(0))
            wst["released"].add(j + 1)
            w_pump()

            Sx.barrier()
            for b3, nh in ((4, 3), (5, 3), (6, 2)):
                h0 = (b3 - 4) * 3
                op("dve", lambda e, b3=b3, nh=nh, h0=h0: e.reciprocal(
                    out=sm.ap[:, 24 + h0:24 + h0 + nh],
                    in_=bk(b3)[:, 0:nh * 129].rearrange("p (h n) -> p h n", n=129)[:, :, 128]),
                   R=[banks[b3]], W=[sm])
            for h in range(8):
                op("dve", lambda e, h=h: e.scalar_tensor_tensor(
                    out=o_y.ap[:, h * 128:(h + 1) * 128], in0=bk(obank(h))[:, ooff(h):ooff(h) + 128],
                    scalar=sm.ap[:, 24 + h:25 + h], in1=zs.ap[:, h * 128:(h + 1) * 128], op0=ALU.mult, op1=ALU.mult),
                   R=[banks[obank(h)], sm, zs], W=[o_y])
            transpose_to(o_y, lambda c: o_y.ap[:, c * 128:(c + 1) * 128], 8, o_yT, lambda r0: o_yT.ap[:, :])
            for nh in range(2):
                wu = get_unit()
                pb = nh
                for c in range(8):
                    op("pe", lambda e, c=c: e.matmul(bk(pb)[:, :], lhsT=o_yT.ap[:, c * 128:(c + 1) * 128], rhs=wu.c(c),
                                                     start=(c == 0), stop=(c == 7)), R=[wu.b(c), o_yT], W=[banks[pb]])
                op("dve", lambda e, nh=nh: e.tensor_tensor(out=o_x1.ap[:, nh * 512:(nh + 1) * 512], in0=bk(pb)[:, :],
                                                           in1=xt.ap[:, nh * 512:(nh + 1) * 512], op=ALU.add),
                   R=[banks[pb], xt], W=[o_x1])
            if dbg:
                dma(out=dbg_x1[j * 128:(j + 1) * 128, :], in_=o_x1.ap[:, :], anchor=dmC, R=[o_x1])

            rms_rstd(o_x1, o_x1.ap[:, :], 32)
            op("dve", lambda e: e.tensor_scalar(out=o_h1.ap[:, :], in0=o_x1.ap[:, :], scalar1=sm.ap[:, 32:33], scalar2=None,
                                                op0=ALU.mult), R=[o_x1, sm], W=[o_h1])
            transpose_to(o_h1, lambda c: o_h1.ap[:, c * 128:(c + 1) * 128], 8, o_h1T, lambda r0: o_h1T.ap[:, :])

            def proj_unit(pb):
                wu = get_unit()
                for c in range(8):
                    op("pe", lambda e, c=c: e.matmul(bk(pb)[:, :], lhsT=o_h1T.ap[:, c * 128:(c + 1) * 128], rhs=wu.c(c),
                                                     start=(c == 0), stop=(c == 7)), R=[wu.b(c), o_h1T], W=[banks[pb]])
            for uu in range(4):
                pb = uu % 3
                proj_unit(pb)
                op("act", lambda e, uu=uu, pb=pb: e.copy(out=o_v.ap[:, uu * 512:(uu + 1) * 512], in_=bk(pb)[:, :]),
                   R=[banks[pb]], W=[o_v])
                op("dve", lambda e, uu=uu: e.bn_stats(out=sm.ap[:, 36 + 6 * uu:42 + 6 * uu], in_=o_v.ap[:, uu * 512:(uu + 1) * 512]),
                   R=[o_v], W=[sm])
            op("dve", lambda e: e.bn_aggr(out=sm.ap[:, 60:62], in_=sm.ap[:, 36:60]), R=[sm], W=[sm])
            op("dve", lambda e: e.tensor_scalar(out=sm.ap[:, 62:63], in0=sm.ap[:, 61:62], scalar1=LN_EPS, scalar2=None,
                                                op0=ALU.add), R=[sm], W=[sm])
            op("act", lambda e: e.sqrt(out=sm.ap[:, 63:64], in_=sm.ap[:, 62:63]), R=[sm], W=[sm])
            op("dve", lambda e: e.reciprocal(out=sm.ap[:, 62:63], in_=sm.ap[:, 63:64]), R=[sm], W=[sm])
            o_vq = [Buf("o_vq%d" % q, o_v.ap[:, q * 512:(q + 1) * 512]) for q in range(4)]
            o_vlnq = [Buf("o_vlnq%d" % q, o_vln.ap[:, q * 512:(q + 1) * 512]) for q in range(4)]
            for q in range(4):
                vq, lq = o_vq[q], o_vlnq[q]
                if o_v.w is not None:
                    vq.w = o_v.w
                op("dve", lambda e: e.tensor_scalar(out=vq.ap, in0=vq.ap, scalar1=sm.ap[:, 60:61], scalar2=sm.ap[:, 62:63],
                                                    op0=ALU.subtract, op1=ALU.mult), R=[o_v, sm], W=[vq])
                op("dve", lambda e, q=q: e.tensor_tensor(out=vq.ap, in0=vq.ap, in1=lng.ap[:, q * 512:(q + 1) * 512],
                                                         op=ALU.mult), R=[vq, lng], W=[vq])
                lq.w = o_vln.w
                lq.r = dict(o_vln.r)
                op("dve", lambda e, q=q: e.tensor_tensor(out=lq.ap, in0=vq.ap, in1=lnb.ap[:, q * 512:(q + 1) * 512],
                                                         op=ALU.add), R=[vq, lnb], W=[lq])
            for g in range(8):
                mbk = 3 + g // 2
                op("pe", lambda e, g=g, mbk=mbk: e.matmul(bk(mbk)[:, (g % 2) * 256:(g % 2 + 1) * 256], lhsT=wsTm.ap[:, g, :],
                                                          rhs=o_vln.ap[:, g * 256:(g + 1) * 256], start=(g % 2 == 0),
                                                          stop=True, skip_group_check=True), R=[wsTm, o_vlnq[g // 2]], W=[banks[mbk]])
            for q in range(4):
                for src, dst in ((o_vq[q], o_v), (o_vlnq[q], o_vln)):
                    for tok in [src.w] + list(src.r.items()):
                        if tok is not None and dst.r.get(tok[0], 0) < tok[1]:
                            dst.r[tok[0]] = tok[1]
            for uu in range(4):
                pb = uu % 3
                proj_unit(pb)
                op("act", lambda e, uu=uu, pb=pb: e.copy(out=o_u.ap[:, uu * 512:(uu + 1) * 512], in_=bk(pb)[:, :]),
                   R=[banks[pb]], W=[o_u])
            for g in range(8):
                mbk = 3 + g // 2
                op("dve", lambda e, g=g, mbk=mbk: e.scalar_tensor_tensor(
                    out=o_um.ap[:, g * 256:(g + 1) * 256], in0=bk(mbk)[:, (g % 2) * 256:(g % 2 + 1) * 256],
                    scalar=bsT.ap[:, g:g + 1], in1=o_u.ap[:, g * 256:(g + 1) * 256], op0=ALU.add, op1=ALU.mult),
                   R=[banks[mbk], bsT, o_u], W=[o_um])
            for uu in range(4):
                pb = uu % 3
                proj_unit(pb)
                zb = o_zs1[uu % 2]
                op("act", lambda e, pb=pb, zb=zb: e.activation(out=zb.ap[:, :], in_=bk(pb)[:, :], func=AF.Silu),
                   R=[banks[pb]], W=[zb])
                op("dve", lambda e, uu=uu, zb=zb: e.tensor_tensor(out=o_y1.ap[:, uu * 512:(uu + 1) * 512], in0=zb.ap[:, :],
                                                                   in1=o_um.ap[:, uu * 512:(uu + 1) * 512], op=ALU.mult),
                   R=[zb, o_um], W=[o_y1])
            transpose_to(o_y1, lambda c: o_y1.ap[:, c * 128:(c + 1) * 128], 16, o_y1T,
                         lambda r0: o_y1T.ap[:, r0 * 128:(r0 + 8) * 128])
            for nh in range(2):
                pb = nh
                for kh in range(2):
                    wu = get_unit()
                    for c in range(8):
                        kc = kh * 8 + c
                        op("pe", lambda e, c=c, kc=kc: e.matmul(bk(pb)[:, :], lhsT=o_y1T.ap[:, kc * 128:(kc + 1) * 128],
                                                                rhs=wu.c(c), start=(kc == 0), stop=(kc == 15)),
                           R=[wu.b(c), o_y1T], W=[banks[pb]])
                op("dve", lambda e, nh=nh: e.tensor_tensor(out=o_x2.ap[:, nh * 512:(nh + 1) * 512], in0=bk(pb)[:, :],
                                                           in1=o_x1.ap[:, nh * 512:(nh + 1) * 512], op=ALU.add),
                   R=[banks[pb], o_x1], W=[o_x2])
            rms_rstd(o_x2, o_x2.ap[:, :], 20)
            op("dve", lambda e: e.scalar_tensor_tensor(out=o_o.ap[:, :], in0=o_x2.ap[:, :], scalar=sm.ap[:, 20:21],
                                                       in1=gf.ap[:], op0=ALU.mult, op1=ALU.mult), R=[o_x2, sm, gf], W=[o_o])
            dma(out=out_d[j * 128:(j + 1) * 128, :], in_=o_o.ap[:, :], anchor=dmA, R=[o_o])
        Sx.barrier()
        print("instructions:", Sx.nins, "waits:", Sx.nwait, "sems:", len(Sx.anchors) + 5)
    return nc


def _t5_bucket_np(rel):
    import jax
    import jax.numpy as jnp
    with jax.default_device(jax.devices("cpu")[0]):
        return _t5_bucket_cpu(jnp, rel)


def _t5_bucket_cpu(jnp, rel):
    rel = jnp.asarray(rel, dtype=jnp.int32)
    half = 16
    max_exact = 8
    ret = jnp.where(rel < 0, half, 0)
    n = jnp.abs(rel)
    nf = jnp.maximum(n, 1).astype(jnp.float32)
    large = max_exact + (jnp.log(nf / max_exact) / math.log(128 / max_exact) * (half - max_exact)).astype(jnp.int32)
    large = jnp.minimum(large, half - 1)
    return np.asarray(ret + jnp.where(n < max_exact, n, large))


_NC_CACHE = {}


def _host_inputs(x, norm_g, final_g, rel_bias, a_w_in, a_w_out, b_w_in, b_ln_g, b_ln_b, b_w_s, b_b_s, b_w_out):
    B, S, _ = x.shape
    NBLK = S // 128
    NQ = NBLK // 4
    f = np.float32
    bc = lambda v, n: np.ascontiguousarray(np.broadcast_to(np.asarray(v, f)[None, :], (128, n)))
    common = {
        "w_in0": np.ascontiguousarray(a_w_in[0], f), "w_out0": np.ascontiguousarray(a_w_out[0], f),
        "w_in1": np.ascontiguousarray(b_w_in[0], f), "w_out1": np.ascontiguousarray(b_w_out[0], f),
        "g0c": np.ascontiguousarray(np.asarray(norm_g[0], f).reshape(8, 128).T),
        "g1c": np.ascontiguousarray(np.asarray(norm_g[1], f).reshape(8, 128).T),
        "gf": bc(final_g, D), "lng": bc(b_ln_g[0], 2048), "lnb": bc(b_ln_b[0], 2048),
        "bsT": np.ascontiguousarray(np.asarray(b_b_s[0], f).T),
        "wsT": np.ascontiguousarray(np.transpose(np.asarray(b_w_s[0], f), (2, 0, 1))),
        "tri": np.ascontiguousarray(np.triu(np.ones((128, 128), f))),
        "cb": bc(np.asarray(rel_bias, f)[15], 8),
        "ident": np.eye(128, dtype=f),
    }
    s_loc = np.arange(128)[:, None]
    t_loc = np.arange(128)[None, :]
    rb = np.asarray(rel_bias, f)
    in_maps = []
    for c in range(8):
        b, r = c // 4, c % 4
        m = dict(common)
        m["xb"] = np.ascontiguousarray(x[b], f)
        xr = np.asarray(x[b], f).reshape(NBLK, 128, D)
        m["xq"] = np.ascontiguousarray(xr[r::4].reshape(NQ * 128, D))
        bt = np.zeros((128, 5, 8, 128), f)
        for i in range(5):
            rel = (t_loc - s_loc) - 128 * (i - 1 - r)
            bidx = _t5_bucket_np(rel)
            bt[:, i, :, :] = np.transpose(rb[bidx], (0, 2, 1))
        m["biasT"] = np.ascontiguousarray(bt.reshape(128, 5 * 8 * 128))
        ad = np.zeros((128, 4, 128), f)
        for rp in range(4):
            if rp > r:
                ad[:, rp, :] = -1e30
            elif rp == r:
                ad[:64, rp, 64:] = -1e30
        m["admis"] = np.ascontiguousarray(ad.reshape(128, 512))
        in_maps.append(m)
    return in_maps, NBLK, NQ


def kernel(x, norm_g, final_g, rel_bias, a_w_in, a_w_out, b_w_in, b_ln_g, b_ln_b, b_w_s, b_b_s, b_w_out, _dbg=False):
    x = np.asarray(x)
    in_maps, NBLK, NQ = _host_inputs(x, norm_g, final_g, rel_bias, a_w_in, a_w_out, b_w_in, b_ln_g, b_ln_b,
                                     b_w_s, b_b_s, b_w_out)
    key = (NBLK, NQ, _dbg)
    if key not in _NC_CACHE:
        _NC_CACHE[key] = build(NBLK, NQ, dbg=_dbg)
    nc = _NC_CACHE[key]
    res = run_bass_kernel_spmd(nc, in_maps, core_ids=list(range(8)))
    B, S, _ = x.shape
    out = np.zeros((B, NBLK, 128, D), np.float32)
    for c in range(8):
        b, r = c // 4, c % 4
        out[b, r::4] = np.asarray(res.results[c]["out"]).reshape(NQ, 128, D)
    out = out.reshape(B, S, D)
    if _dbg:
        return out, res
    return out
```

```python
import contextlib
import math
import numpy as np
import concourse.bass as bass
import concourse.mybir as mybir
from concourse.bass_utils import run_bass_kernel_spmd

F32 = mybir.dt.float32
BF16 = mybir.dt.bfloat16
AF = mybir.ActivationFunctionType
ALU = mybir.AluOpType
AX = mybir.AxisListType

D = 1024
TOPK = 256
RMS_EPS = 1e-6
LN_EPS = 1e-5
KBIS = 16
KVG = 2
KVS = 3
WS = 2
NEG = -30000.0
SAME_ENGINE_SYNC = True
USE_ACT_COUNT = True
ACT_FRAC = 0.5
PV_LAG = 2
NHS_LIMIT = 10


class Buf:
    def __init__(self, name, ap=None):
        self.name = name
        self.ap = ap
        self.w = None
        self.r = {}
        self.dsem = None
        self.dcnt = 0


class Sched:
    def __init__(self, nc, es):
        self.nc = nc
        self.es = es
        self.eng = {"pe": nc.tensor, "act": nc.scalar, "dve": nc.vector, "pool": nc.gpsimd, "sp": nc.sync}
        self.sem = {k: es.enter_context(nc.semaphore("s_" + k)) for k in self.eng}
        self.cnt = {k: 0 for k in self.eng}
        self.seen = {k: {} for k in self.eng}
        self.anchors = []
        self.nwait = 0
        self.nins = 0

    def _semof(self, key):
        if isinstance(key, str):
            return self.sem[key]
        return key.dsem

    def _wait(self, E, key, val):
        if val <= 0:
            return
        if self.seen[E].get(key, 0) >= val:
            return
        self.eng[E].wait_ge(self._semof(key), val)
        self.seen[E][key] = val
        self.nwait += 1

    def _deps(self, E, R, W):
        deps = {}

        def add(tok):
            k, v = tok
            if deps.get(k, 0) < v:
                deps[k] = v
        for b in R:
            if b.w is not None:
                add(b.w)
        for b in W:
            if b.w is not None:
                add(b.w)
            for k, v in b.r.items():
                add((k, v))
        for k, v in deps.items():
            if k == E:
                if E == "pe" or not SAME_ENGINE_SYNC:
                    continue
            self._wait(E, k, v)

    def _mark(self, tok, R, W):
        k, v = tok
        for b in R:
            if b.r.get(k, 0) < v:
                b.r[k] = v
        for b in W:
            b.w = tok
            b.r = {}

    def op(self, E, fn, R=(), W=()):
        self._deps(E, R, W)
        ins = fn(self.eng[E])
        self.cnt[E] += 1
        ins.then_inc(self.sem[E], 1)
        self.nins += 1
        self._mark((E, self.cnt[E]), R, W)
        return ins

    def dma(self, out, in_, anchor, R=(), W=(), Q="sp"):
        self._deps(Q, R, W)
        if anchor.dsem is None:
            anchor.dsem = self.es.enter_context(self.nc.semaphore("d_" + anchor.name))
            self.anchors.append(anchor)
        ins = self.eng[Q].dma_start(out=out, in_=in_)
        anchor.dcnt += 16
        ins.then_inc(anchor.dsem, 16)
        self.nins += 1
        self._mark((anchor, anchor.dcnt), R, W)
        return ins

    def barrier(self, skip=()):
        skip_ids = {id(b) for b in skip}
        toks = [(k, self.cnt[k]) for k in ("pe", "act", "dve", "pool")]
        toks += [(a, a.dcnt) for a in self.anchors if id(a) not in skip_ids]
        for E in self.eng:
            for k, v in toks:
                if k == E:
                    continue
                self._wait(E, k, v)


def build(NBLK, NQ, dbg=False):
    assert NBLK == 4 * NQ
    S = NBLK * 128
    NG = NBLK // 4
    nc = bass.Bass("TRN2", target_bir_lowering=False)

    def din(name, shape):
        return nc.dram_tensor(name, list(shape), F32, kind="ExternalInput").ap()

    xb = din("xb", [S, D])
    xq = din("xq", [NQ * 128, D])
    w_in0 = din("w_in0", [D, 4680])
    w_out0 = din("w_out0", [D, D])
    w_in1 = din("w_in1", [D, 6144])
    w_out1 = din("w_out1", [2048, D])
    g0c_d = din("g0c", [128, 8])
    g1c_d = din("g1c", [128, 8])
    gf_d = din("gf", [128, D])
    lng_d = din("lng", [128, 2048])
    lnb_d = din("lnb", [128, 2048])
    bsT_d = din("bsT", [128, 8])
    wsT_d = din("wsT", [128, 8, 128])
    tri_d = din("tri", [128, 128])
    biasT_d = din("biasT", [128, 5 * 8 * 128])
    cb_d = din("cb", [128, 8])
    admis_d = din("admis", [128, 512])
    ident_d = din("ident", [128, 128])
    out_d = nc.dram_tensor("out", [NQ * 128, D], F32, kind="ExternalOutput").ap()
    if dbg:
        dbg_x1 = nc.dram_tensor("dbg_x1", [NQ * 128, D], F32, kind="ExternalOutput").ap()
        dbg_thr = nc.dram_tensor("dbg_thr", [NQ * 128, 4], F32, kind="ExternalOutput").ap()

    if dbg:
        dbg_isc = nc.dram_tensor("dbg_isc", [NQ * 128, S], F32, kind="ExternalOutput").ap()
        kT_scr = nc.dram_tensor("kT_scr", [NBLK, 128, 1024], BF16, kind="ExternalOutput").ap()
        v_scr = nc.dram_tensor("v_scr", [NBLK, 128, 1032], BF16, kind="ExternalOutput").ap()
    else:
        kT_scr = nc.dram_tensor("kT_scr", [NBLK, 128, 1024], BF16).ap()
        v_scr = nc.dram_tensor("v_scr", [NBLK, 128, 1032], BF16).ap()
    wq_scr = nc.dram_tensor("wq_scr", [5, 128, 8, 512], BF16).ap()
    wo0_scr = nc.dram_tensor("wo0_scr", [2, 128, 8, 512], BF16).ap()
    w1_scr = nc.dram_tensor("w1_scr", [12, 128, 8, 512], BF16).ap()
    wo1_scr = nc.dram_tensor("wo1_scr", [4, 128, 8, 512], BF16).ap()
    scrK = Buf("scrK")
    scrV = Buf("scrV")
    scrW = Buf("scrW")

    es = contextlib.ExitStack()
    with es:
        Sx = Sched(nc, es)
        op = Sx.op
        dma = Sx.dma

        def sb(name, shape, dt, stack=es):
            t = stack.enter_context(nc.sbuf_tensor("sb_" + name, list(shape), dt))
            return Buf(name, t)

        banks = []
        for i in range(8):
            t = es.enter_context(nc.psum_tensor("bank%d" % i, [128, 512], F32))
            banks.append(Buf("bank%d" % i, t))

        def bk(i):
            return banks[i].ap

        def bkb(i):
            return banks[i].ap[:, 0:512].bitcast(BF16)

        ident_f = sb("ident_f", [128, 128], F32)
        ident_b = sb("ident_b", [128, 128], BF16)
        ident4 = sb("ident4", [128, 512], BF16)
        gf = sb("gf", [128, D], F32)
        lng = sb("lng", [128, 2048], F32)
        lnb = sb("lnb", [128, 2048], F32)
        bsT = sb("bsT", [128, 8], F32)
        wsTm = sb("wsTm", [128, 8, 128], BF16)
        cb = sb("cb", [128, 8], F32)
        g0c = sb("g0c", [128, 8], F32)
        g1c = sb("g1c", [128, 8], F32)
        admis = sb("admis", [128, 512], F32)
        delta = sb("delta", [128, 5, 8, 128], BF16)
        wwi = sb("wwi", [128, 8, 8], BF16)
        kiT = sb("kiT", [128, S], BF16)
        cpow = sb("cpow", [128, KBIS + 1], F32)
        junk_act = sb("junk_act", [128, 2], BF16)
        junk_dve = sb("junk_dve", [128, 2], BF16)
        constA = Buf("constA")

        consts = []

        def cload(buf, src):
            dma(out=buf.ap[:], in_=src, anchor=constA, W=[buf])
            consts.append(buf)

        cload(ident_f, ident_d[:, :])
        cload(gf, gf_d[:, :])
        cload(lng, lng_d[:, :])
        cload(lnb, lnb_d[:, :])
        cload(bsT, bsT_d[:, :])
        cload(cb, cb_d[:, :])
        cload(g0c, g0c_d[:, :])
        cload(g1c, g1c_d[:, :])
        cload(admis, admis_d[:, :])

        p1 = contextlib.ExitStack()
        es.enter_context(p1)
        wkv = sb("wkv", [128, 8, 2112], BF16, p1)

        with contextlib.ExitStack() as ps:
            stgf = [sb("stgf%d" % i, [128, 6144], F32, ps) for i in range(2)]
            stgb = [sb("stgb%d" % i, [128, 6144], BF16, ps) for i in range(2)]
            wsT_f = sb("wsT_f", [128, 8, 128], F32, ps)
            tri_f = sb("tri_f", [128, 128], F32, ps)
            cload(wsT_f, wsT_d[:, :, :])
            cload(tri_f, tri_d[:, :])
            for b in consts:
                b.w = (constA, constA.dcnt)
            op("dve", lambda e: e.tensor_copy(out=ident_b.ap[:], in_=ident_f.ap[:]), R=[ident_f], W=[ident_b])
            for q in range(4):
                op("dve", lambda e, q=q: e.tensor_copy(out=ident4.ap[:, q * 128:(q + 1) * 128], in_=ident_f.ap[:]),
                   R=[ident_f], W=[ident4])
            for k in range(KBIS + 1):
                op("pool", lambda e, k=k: e.memset(cpow.ap[:, k:k + 1], float(2.0 ** -(k + 1))), W=[cpow])
            for g in range(8):
                op("dve", lambda e, g=g: e.tensor_tensor(out=wsTm.ap[:, g, :], in0=wsT_f.ap[:, g, :], in1=tri_f.ap[:],
                                                         op=ALU.mult), R=[wsT_f, tri_f], W=[wsTm])
            for i in range(5):
                st = stgf[i % 2]
                dma(out=st.ap[:, 0:1024], in_=biasT_d[:, i * 1024:(i + 1) * 1024], anchor=st, W=[st])
                for h in range(8):
                    op("dve", lambda e, i=i, h=h, st=st: e.tensor_scalar(
                        out=delta.ap[:, i, h, :], in0=st.ap[:, h * 128:(h + 1) * 128], scalar1=cb.ap[:, h:h + 1],
                        scalar2=None, op0=ALU.subtract), R=[st, cb], W=[delta])

            nprep = [0]

            def prep_weight(W_ap, K, C, gcol, dest_fn):
                for c in range(K // 128):
                    i = nprep[0] % 2
                    nprep[0] += 1
                    sf, sbb = stgf[i], stgb[i]
                    dma(out=sf.ap[:, 0:C], in_=W_ap[c * 128:(c + 1) * 128, :], anchor=sf, W=[sf])
                    if gcol is not None:
                        op("dve", lambda e, c=c: e.tensor_scalar(out=sbb.ap[:, 0:C], in0=sf.ap[:, 0:C],
                                                                 scalar1=gcol.ap[:, c:c + 1], scalar2=None,
                                                                 op0=ALU.mult), R=[sf, gcol], W=[sbb])
                    else:
                        op("act", lambda e: e.copy(out=sbb.ap[:, 0:C], in_=sf.ap[:, 0:C]), R=[sf], W=[sbb])
                    dest_fn(c, sbb)

            def units_store(scr, u0, nu, c, sbb, col0):
                dma(out=scr[u0:u0 + nu, :, c, :].rearrange("u p n -> p u n"),
                    in_=sbb.ap[:, col0:col0 + nu * 512].rearrange("p (u n) -> p u n", n=512),
                    anchor=sbb, R=[sbb], W=[scrW])

            def dest_in0(c, sbb):
                units_store(wq_scr, 0, 2, c, sbb, 0)
                units_store(wq_scr, 2, 2, c, sbb, 3072)
                units_store(wq_scr, 4, 1, c, sbb, 4096)
                op("pool", lambda e: e.tensor_copy(out=wkv.ap[:, c, 0:2048], in_=sbb.ap[:, 1024:3072]), R=[sbb], W=[wkv])
                op("pool", lambda e: e.tensor_copy(out=wkv.ap[:, c, 2048:2112], in_=sbb.ap[:, 4608:4672]), R=[sbb], W=[wkv])
                op("pool", lambda e: e.tensor_copy(out=wwi.ap[:, c, :], in_=sbb.ap[:, 4672:4680]), R=[sbb], W=[wwi])

            prep_weight(w_in0, 1024, 4680, g0c, dest_in0)
            prep_weight(w_out0, 1024, 1024, None, lambda c, sbb: units_store(wo0_scr, 0, 2, c, sbb, 0))
            prep_weight(w_in1, 1024, 6144, g1c, lambda c, sbb: units_store(w1_scr, 0, 12, c, sbb, 0))

            def dest_out1(kc, sbb):
                kh, c = kc // 8, kc % 8
                units_store(wo1_scr, kh, 1, c, sbb, 0)
                units_store(wo1_scr, 2 + kh, 1, c, sbb, 512)

            prep_weight(w_out1, 2048, 1024, None, dest_out1)
            Sx.barrier()

        with contextlib.ExitStack() as s1:
            xg = [sb("xg%d" % i, [128, 4, D], F32, s1) for i in range(2)]
            xn = sb("xn", [128, 4, D], BF16, s1)
            hT = [sb("hT%d" % i, [128, 8, 512], BF16, s1) for i in range(2)]
            kTs = [sb("kTs%d" % i, [128, 4, 8, 128], BF16, s1) for i in range(2)]
            Vs = [sb("Vs%d" % i, [128, 4, 8, 129], BF16, s1) for i in range(2)]
            st1 = sb("st1", [128, 16], F32, s1)
            for i in range(2):
                op("pool", lambda e, i=i: e.memset(Vs[i].ap[:], 1.0), W=[Vs[i]])
            op("pool", lambda e: e.memset(kiT.ap[64:128, :], 0.0), W=[kiT])
            ev = [0]

            def evac(fn_act, fn_dve, R, W):
                ev[0] += 1
                if ev[0] % 2 == 0:
                    op("act", fn_act, R=R, W=W)
                else:
                    op("dve", fn_dve, R=R, W=W)

            def load_xg(g):
                b = xg[g % 2]
                dma(out=b.ap[:], in_=xb[g * 512:(g + 1) * 512, :].rearrange("(t p) d -> p t d", p=128), anchor=b, W=[b])

            load_xg(0)
            for g in range(NG):
                if g + 1 < NG:
                    load_xg(g + 1)
                xgb = xg[g % 2]
                h_ = hT[g % 2]
                for t in range(4):
                    op("act", lambda e, t=t: e.activation(out=junk_act.ap[:, 0:1].to_broadcast([128, D]), in_=xgb.ap[:, t, :], func=AF.Square,
                                                          accum_out=st1.ap[:, t:t + 1]), R=[xgb], W=[junk_act, st1])
                op("dve", lambda e: e.tensor_scalar(out=st1.ap[:, 4:8], in0=st1.ap[:, 0:4], scalar1=1.0 / D,
                                                    scalar2=RMS_EPS, op0=ALU.mult, op1=ALU.add), R=[st1], W=[st1])
                op("act", lambda e: e.sqrt(out=st1.ap[:, 8:12], in_=st1.ap[:, 4:8]), R=[st1], W=[st1])
                op("dve", lambda e: e.reciprocal(out=st1.ap[:, 12:16], in_=st1.ap[:, 8:12]), R=[st1], W=[st1])
                for t in range(4):
                    op("dve", lambda e, t=t: e.tensor_scalar(out=xn.ap[:, t, :], in0=xgb.ap[:, t, :],
                                                             scalar1=st1.ap[:, 12 + t:13 + t], scalar2=None,
                                                             op0=ALU.mult), R=[xgb, st1], W=[xn])
                    tb = 6 + (t % 2)
                    for c in range(8):
                        op("pe", lambda e, t=t, c=c, tb=tb: e.transpose(out=bkb(tb)[:, c * 128:(c + 1) * 128],
                                                                        in_=xn.ap[:, t, c * 128:(c + 1) * 128],
                                                                        identity=ident_b.ap[:]),
                           R=[xn, ident_b], W=[banks[tb]])
                    evac(lambda e, t=t, tb=tb: e.copy(out=h_.ap[:, :, t * 128:(t + 1) * 128],
                                                      in_=bkb(tb).rearrange("p (c n) -> p c n", n=128)),
                         lambda e, t=t, tb=tb: e.tensor_copy(out=h_.ap[:, :, t * 128:(t + 1) * 128],
                                                             in_=bkb(tb).rearrange("p (c n) -> p c n", n=128)),
                         R=[banks[tb]], W=[h_])
                kb_, vb_ = kTs[g % 2], Vs[g % 2]
                for h in range(8):
                    pb = h % 4
                    for c in range(8):
                        op("pe", lambda e, h=h, c=c, pb=pb: e.matmul(bk(pb)[:, :], lhsT=wkv.ap[:, c, h * 128:(h + 1) * 128],
                                                                     rhs=h_.ap[:, c, :], start=(c == 0), stop=(c == 7)),
                           R=[wkv, h_], W=[banks[pb]])
                    evac(lambda e, h=h, pb=pb: e.copy(out=kb_.ap[:, :, h, :], in_=bk(pb).rearrange("p (b n) -> p b n", n=128)),
                         lambda e, h=h, pb=pb: e.tensor_copy(out=kb_.ap[:, :, h, :], in_=bk(pb).rearrange("p (b n) -> p b n", n=128)),
                         R=[banks[pb]], W=[kb_])
                for t in range(4):
                    for hg in range(2):
                        pb = (t * 2 + hg) % 4
                        for c in range(8):
                            op("pe", lambda e, t=t, hg=hg, c=c, pb=pb: e.matmul(
                                bk(pb)[:, :], lhsT=h_.ap[:, c, t * 128:(t + 1) * 128],
                                rhs=wkv.ap[:, c, 1024 + hg * 512:1024 + (hg + 1) * 512], start=(c == 0), stop=(c == 7)),
                               R=[wkv, h_], W=[banks[pb]])
                        evac(lambda e, t=t, hg=hg, pb=pb: e.copy(out=vb_.ap[:, t, hg * 4:(hg + 1) * 4, 0:128],
                                                                 in_=bk(pb).rearrange("p (h n) -> p h n", n=128)),
                             lambda e, t=t, hg=hg, pb=pb: e.tensor_copy(out=vb_.ap[:, t, hg * 4:(hg + 1) * 4, 0:128],
                                                                        in_=bk(pb).rearrange("p (h n) -> p h n", n=128)),
                             R=[banks[pb]], W=[vb_])
                for c in range(8):
                    op("pe", lambda e, c=c: e.matmul(bk(4)[0:64, :], lhsT=wkv.ap[:, c, 2048:2112], rhs=h_.ap[:, c, :],
                                                     start=(c == 0), stop=(c == 7)), R=[wkv, h_], W=[banks[4]])
                evac(lambda e: e.copy(out=kiT.ap[0:64, g * 512:(g + 1) * 512], in_=bk(4)[0:64, :]),
                     lambda e: e.tensor_copy(out=kiT.ap[0:64, g * 512:(g + 1) * 512], in_=bk(4)[0:64, :]),
                     R=[banks[4]], W=[kiT])
                dma(out=kT_scr[g * 4:(g + 1) * 4].rearrange("b d n -> d b n"),
                    in_=kb_.ap[:].rearrange("p b h n -> p b (h n)"), anchor=kb_, R=[kb_], W=[scrK])
                dma(out=v_scr[g * 4:(g + 1) * 4].rearrange("b s n -> s b n"),
                    in_=vb_.ap[:].rearrange("p b h n -> p b (h n)"), anchor=vb_, R=[vb_], W=[scrV])
            Sx.barrier()
        p1.close()

        big = sb("big", [128, 16384], F32)
        Kc = [sb("Kc%d" % i, [128, KVG, 1024], BF16) for i in range(KVS)]
        Vc = [sb("Vc%d" % i, [128, KVG, 1032], BF16) for i in range(KVS)]
        wslh = [sb("wslh%d" % i, [128, 4, 512], BF16) for i in range(4)]
        xqt = [sb("xqt%d" % i, [128, D], F32) for i in range(2)]
        xnq = sb("xnq", [128, D], BF16)
        hTq = sb("hTq", [128, 8, 128], BF16)
        QT = sb("QT", [128, 8, 128], BF16)
        qiT = sb("qiT", [128, 8, 128], BF16)
        zs = sb("zs", [128, D], F32)
        diagw = sb("diagw", [128, 8, 128], BF16)
        Rb_all = sb("Rb_all", [128, 6, 512], BF16)
        Rb = [Buf("Rb%d" % i, Rb_all.ap[:, i, :]) for i in range(6)]
        Rb_flat = Rb_all.ap[:].rearrange("p a n -> p (a n)")
        bisa = sb("bisa", [128, 8], F32)
        bisc = sb("bisc", [128, 2], F32)
        PT = [sb("PT%d" % i, [128, 512], BF16) for i in range(4)]
        mb = [sb("mb%d" % i, [128, 512], BF16) for i in range(2)]
        sm = sb("sm", [128, 64], F32)
        Wt = sb("Wt", [128, KBIS + 1], F32)
        bis = sb("bis", [128, 8], F32)

        def ov(name, off, n, dt):
            a = big.ap[:, off:off + n]
            if dt == BF16:
                a = a.bitcast(BF16)
            return Buf(name, a)
        o_y = ov("o_y", 0, 512, BF16)
        o_yT = ov("o_yT", 512, 512, BF16)
        o_x1 = ov("o_x1", 1024, 1024, F32)
        o_h1 = ov("o_h1", 2048, 512, BF16)
        o_h1T = ov("o_h1T", 2560, 512, BF16)
        o_v = ov("o_v", 3072, 2048, F32)
        o_u = ov("o_u", 5120, 2048, F32)
        o_vln = ov("o_vln", 7168, 1024, BF16)
        o_um = ov("o_um", 8192, 2048, F32)
        o_zs1 = [ov("o_zs1%d" % i, 10240 + 512 * i, 512, F32) for i in range(2)]
        o_y1 = ov("o_y1", 11264, 1024, BF16)
        o_y1T = ov("o_y1T", 12288, 1024, BF16)
        o_x2 = ov("o_x2", 13312, 1024, F32)
        o_o = ov("o_o", 14336, 1024, F32)

        units = []
        for j in range(NQ):
            units += [wq_scr[0], wq_scr[1], wq_scr[4], wq_scr[2], wq_scr[3], wo0_scr[0], wo0_scr[1]]
            units += [w1_scr[u] for u in (4, 5, 6, 7, 0, 1, 2, 3, 8, 9, 10, 11)]
            units += [wo1_scr[u] for u in range(4)]
        hslots = [(b, b.ap[:]) for b in wslh]
        hslots += [(b, b.ap[:].rearrange("p a (b n) -> p (a b) n", n=512)) for b in Kc]
        hslots += [(b, b.ap[:].rearrange("p a n -> p (a n)")[:, 0:2048].rearrange("p (c n) -> p c n", n=512)) for b in Vc]
        hslots = hslots[:NHS_LIMIT]
        NHS = len(hslots)
        halves = []
        seg_sizes = [5] + [23] * (NQ - 1) + [18]
        ui = 0
        for sg, nu in enumerate(seg_sizes):
            for q in range(2 * nu):
                halves.append((units[ui + q // 2], q % 2, sg, q))
            ui += nu
        assert ui == len(units)
        wst = {"issued": 0, "next": 0, "released": {0}, "occ": [None] * NHS}

        def w_pump():
            consumed = 2 * wst["next"]
            while wst["issued"] < len(halves) and wst["issued"] <= consumed + 9:
                m = wst["issued"]
                uap, hf, sg, q = halves[m]
                sl = q % NHS
                if q >= 4 and sg not in wst["released"]:
                    break
                oc = wst["occ"][sl]
                if oc is not None and oc >= consumed:
                    break
                b, view = hslots[sl]
                dma(out=view, in_=uap[:, 4 * hf:4 * hf + 4, :], anchor=b, R=[scrW], W=[b])
                wst["occ"][sl] = m
                wst["issued"] += 1

        class WU:
            def __init__(self, n):
                self.h = []
                for hf in range(2):
                    uap, hf_, sg, q = halves[2 * n + hf]
                    self.h.append(hslots[q % NHS])

            def c(self, c):
                return self.h[c // 4][1][:, c % 4, :]

            def b(self, c):
                return self.h[c // 4][0]

        def get_unit():
            n = wst["next"]
            w_pump()
            assert wst["issued"] >= 2 * n + 2, "weight half-unit not issued"
            wst["next"] += 1
            return WU(n)

        def kv_slots_free_of_weights():
            consumed = 2 * wst["next"]
            for sl in range(4, NHS):
                oc = wst["occ"][sl]
                assert oc is None or oc < consumed, "K/V slot still holds unconsumed weights"

        kvst = {"issued": 0, "next": 0, "limit": 0}
        kvlist = []
        for j in range(NQ):
            for gk in range((4 * j + 4) // KVG):
                kvlist.append(gk * KVG)

        def kv_prefetch(upto):
            while kvst["issued"] < min(upto, kvst["limit"]):
                m = kvst["issued"]
                kb0 = kvlist[m]
                kbuf, vbuf = Kc[m % KVS], Vc[m % KVS]
                dma(out=kbuf.ap[:], in_=kT_scr[kb0:kb0 + KVG].rearrange("b d n -> d b n"), anchor=kbuf, R=[scrK], W=[kbuf])
                dma(out=vbuf.ap[:], in_=v_scr[kb0:kb0 + KVG].rearrange("b s n -> s b n"), anchor=vbuf, R=[scrV], W=[vbuf])
                kvst["issued"] += 1

        def get_kv():
            n = kvst["next"]
            kvst["next"] += 1
            kv_prefetch(n + KVS - 1)
            return Kc[n % KVS], Vc[n % KVS]

        ev2 = [0]

        def evac2(fn_act, fn_dve, R, W):
            ev2[0] += 1
            if ev2[0] % 2 == 0:
                op("act", fn_act, R=R, W=W)
            else:
                op("dve", fn_dve, R=R, W=W)

        def rms_rstd(src_buf, src_ap, col):
            op("act", lambda e: e.activation(out=junk_act.ap[:, 0:1].to_broadcast([128, D]), in_=src_ap, func=AF.Square,
                                             accum_out=sm.ap[:, col:col + 1]), R=[src_buf], W=[junk_act, sm])
            op("dve", lambda e: e.tensor_scalar(out=sm.ap[:, col + 1:col + 2], in0=sm.ap[:, col:col + 1], scalar1=1.0 / D,
                                                scalar2=RMS_EPS, op0=ALU.mult, op1=ALU.add), R=[sm], W=[sm])
            op("act", lambda e: e.sqrt(out=sm.ap[:, col + 2:col + 3], in_=sm.ap[:, col + 1:col + 2]), R=[sm], W=[sm])
            op("dve", lambda e: e.reciprocal(out=sm.ap[:, col:col + 1], in_=sm.ap[:, col + 2:col + 3]), R=[sm], W=[sm])

        def transpose_to(src_buf, src_ap_fn, nch, dst_buf, dst_ap_fn):
            for r0 in range(0, nch, 8):
                for c in range(r0, r0 + 8):
                    op("pe", lambda e, c=c: e.transpose(out=bkb(7)[:, (c - r0) * 128:(c - r0 + 1) * 128],
                                                        in_=src_ap_fn(c), identity=ident_b.ap[:]),
                       R=[src_buf, ident_b], W=[banks[7]])
                evac2(lambda e: e.copy(out=dst_ap_fn(r0), in_=bkb(7)),
                      lambda e: e.tensor_copy(out=dst_ap_fn(r0), in_=bkb(7)), R=[banks[7]], W=[dst_buf])

        op("pool", lambda e: e.memset(qiT.ap[64:128, :, :], 0.0), W=[qiT])
        prefetch_bufs = wslh + Kc + Vc + xqt
        dmA = Buf("storeA")
        dmB = Buf("storeB")
        dmC = Buf("storeC")
        dmD = Buf("storeD")

        dma(out=xqt[0].ap[:], in_=xq[0:128, :], anchor=xqt[0], W=[xqt[0]])
        for j in range(NQ):
            n_kb = 4 * j + 4
            n_ch = j + 1
            N = n_ch * 512
            xt = xqt[j % 2]
            if j + 1 < NQ:
                nx = xqt[(j + 1) % 2]
                dma(out=nx.ap[:], in_=xq[(j + 1) * 128:(j + 2) * 128, :], anchor=nx, W=[nx])
            rms_rstd(xt, xt.ap[:], 0)
            op("dve", lambda e: e.tensor_scalar(out=xnq.ap[:], in0=xt.ap[:], scalar1=sm.ap[:, 0:1], scalar2=None,
                                                op0=ALU.mult), R=[xt, sm], W=[xnq])
            transpose_to(xnq, lambda c: xnq.ap[:, c * 128:(c + 1) * 128], 8, hTq,
                         lambda r0: hTq.ap[:].rearrange("p c n -> p (c n)"))
            for u in range(2):
                wu = get_unit()
                pb = u % 4
                first = True
                for h4 in range(4):
                    for c in range(8):
                        op("pe", lambda e, h4=h4, c=c, first=first: e.matmul(
                            bk(pb)[:, h4 * 128:(h4 + 1) * 128], lhsT=wu.c(c)[:, h4 * 128:(h4 + 1) * 128],
                            rhs=hTq.ap[:, c, :], start=first, stop=(c == 7), skip_group_check=True),
                           R=[wu.b(c), hTq], W=[banks[pb]])
                        first = False
                op("act", lambda e, u=u: e.mul(out=QT.ap[:, u * 4:(u + 1) * 4, :].rearrange("p h n -> p (h n)"),
                                               in_=bk(pb)[:, :], mul=128.0 ** -0.5), R=[banks[pb]], W=[QT])
            wu = get_unit()
            for hg in range(2):
                pb = 2 + hg
                first = True
                for h4 in range(4):
                    h = hg * 4 + h4
                    for c in range(8):
                        op("pe", lambda e, h=h, h4=h4, c=c, first=first: e.matmul(
                            bk(pb)[0:64, h4 * 128:(h4 + 1) * 128], lhsT=wu.c(c)[:, h * 64:(h + 1) * 64],
                            rhs=hTq.ap[:, c, :], start=first, stop=(c == 7), skip_group_check=True),
                           R=[wu.b(c), hTq], W=[banks[pb]])
                        first = False
                op("dve", lambda e, hg=hg: e.tensor_scalar(
                    out=qiT.ap[0:64, hg * 4:(hg + 1) * 4, :].rearrange("p h n -> p (h n)"), in0=bk(pb)[0:64, :],
                    scalar1=64.0 ** -0.5, scalar2=None, op0=ALU.mult), R=[banks[pb]], W=[qiT])
            for c in range(8):
                op("pe", lambda e, c=c: e.matmul(bk(4)[:, 0:8], lhsT=hTq.ap[:, c, :], rhs=wwi.ap[:, c, :],
                                                 start=(c == 0), stop=(c == 7)), R=[hTq, wwi], W=[banks[4]])
            op("dve", lambda e: e.tensor_scalar(out=sm.ap[:, 8:16], in0=bk(4)[:, 0:8], scalar1=8.0 ** -0.5, scalar2=None,
                                                op0=ALU.mult), R=[banks[4]], W=[sm])
            for h in range(8):
                op("dve", lambda e, h=h: e.tensor_scalar(out=diagw.ap[:, h, :], in0=ident_f.ap[:],
                                                         scalar1=sm.ap[:, 8 + h:9 + h], scalar2=None, op0=ALU.mult),
                   R=[ident_f, sm], W=[diagw])
            for u in range(2):
                wu = get_unit()
                pb = u % 2
                for c in range(8):
                    op("pe", lambda e, c=c: e.matmul(bk(pb)[:, :], lhsT=hTq.ap[:, c, :], rhs=wu.c(c),
                                                     start=(c == 0), stop=(c == 7)), R=[wu.b(c), hTq], W=[banks[pb]])
                op("act", lambda e, u=u: e.activation(out=zs.ap[:, u * 512:(u + 1) * 512], in_=bk(pb)[:, :], func=AF.Silu),
                   R=[banks[pb]], W=[zs])

            kv_slots_free_of_weights()
            kvst["limit"] = kvst["next"] + n_kb // KVG
            kv_prefetch(kvst["next"] + KVS - 1)
            Sx.barrier(skip=prefetch_bufs)
            tot = n_ch * 8
            DB = (0, 1, 2, 3, 6, 7)
            LAG = 3

            def wsum(n):
                c, h = n // 8, n % 8
                acc = 4 + (c % 2)
                R_ = Rb[n % 6]
                op("pe", lambda e: e.matmul(bk(acc)[:, :], lhsT=diagw.ap[:, h, :], rhs=R_.ap, start=(h == 0),
                                            stop=(h == 7)), R=[diagw, R_], W=[banks[acc]])
                if h == 7:
                    evac2(lambda e: e.copy(out=big.ap[:, c * 512:(c + 1) * 512], in_=bk(acc)[:, :]),
                          lambda e: e.tensor_copy(out=big.ap[:, c * 512:(c + 1) * 512], in_=bk(acc)[:, :]),
                          R=[banks[acc]], W=[big])

            for n in range(tot):
                c, h = n // 8, n % 8
                pb = DB[n % 6]
                R_ = Rb[n % 6]
                op("pe", lambda e: e.matmul(bk(pb)[:, :], lhsT=qiT.ap[:, h, :], rhs=kiT.ap[:, c * 512:(c + 1) * 512],
                                            start=True, stop=True), R=[qiT, kiT], W=[banks[pb]])
                if n % 2 == 0:
                    op("act", lambda e: e.activation(out=R_.ap, in_=bk(pb)[:, :], func=AF.Relu), R=[banks[pb]], W=[R_])
                else:
                    op("dve", lambda e: e.tensor_scalar(out=R_.ap, in0=bk(pb)[:, :], scalar1=0.0, scalar2=None,
                                                        op0=ALU.max), R=[banks[pb]], W=[R_])
                if n >= LAG:
                    wsum(n - LAG)
            for n in range(max(0, tot - LAG), tot):
                wsum(n)

            isc = big.ap[:, 0:N]
            nA = (int(N * ACT_FRAC) // 64) * 64 if USE_ACT_COUNT else 0
            nD = N - nA
            op("dve", lambda e: e.tensor_reduce(out=sm.ap[:, 16:17], in_=big.ap[:, 0:512], axis=AX.X, op=ALU.min), R=[big], W=[sm])
            op("dve", lambda e: e.tensor_reduce(out=sm.ap[:, 17:18], in_=isc, axis=AX.X, op=ALU.max), R=[big], W=[sm])
            op("dve", lambda e: e.tensor_tensor(out=big.ap[:, N - 512:N], in0=big.ap[:, N - 512:N], in1=admis.ap[:],
                                                op=ALU.add), R=[big, admis], W=[big])
            op("dve", lambda e: e.tensor_scalar(out=bis.ap[:, 0:1], in0=sm.ap[:, 16:17], scalar1=-1.0, scalar2=None,
                                                op0=ALU.add), R=[sm], W=[bis])
            op("dve", lambda e: e.tensor_tensor(out=sm.ap[:, 18:19], in0=sm.ap[:, 17:18], in1=sm.ap[:, 16:17],
                                                op=ALU.subtract), R=[sm], W=[sm])
            op("dve", lambda e: e.tensor_scalar(out=sm.ap[:, 18:19], in0=sm.ap[:, 18:19], scalar1=2.0, scalar2=None,
                                                op0=ALU.add), R=[sm], W=[sm])
            op("dve", lambda e: e.tensor_scalar(out=Wt.ap[:], in0=cpow.ap[:], scalar1=sm.ap[:, 18:19], scalar2=None,
                                                op0=ALU.mult), R=[cpow, sm], W=[Wt])
            op("dve", lambda e: e.tensor_tensor(out=bis.ap[:, 1:2], in0=bis.ap[:, 0:1], in1=Wt.ap[:, 0:1], op=ALU.add),
               R=[bis, Wt], W=[bis])
            for k in range(KBIS):
                if nA > 0:
                    npc = 0
                    for p0 in range(nD, N, 3072):
                        sz = min(3072, N - p0)
                        op("act", lambda e, p0=p0, sz=sz, npc=npc: e.activation(
                            out=Rb_flat[:, 0:sz], in_=big.ap[:, p0:p0 + sz], func=AF.Sign, bias=bis.ap[:, 1:2], scale=-1.0,
                            accum_out=bisa.ap[:, npc:npc + 1]), R=[big, bis], W=[bisa] + Rb)
                        npc += 1
                op("dve", lambda e: e.tensor_scalar(out=junk_dve.ap[:, 0:1].to_broadcast([128, nD]), in0=big.ap[:, 0:nD],
                                                    scalar1=bis.ap[:, 1:2], scalar2=None, op0=ALU.is_ge, op1=ALU.add,
                                                    accum_out=bisc.ap[:, 0:1]), R=[big, bis], W=[bisc, junk_dve])
                if nA > 0:
                    if npc > 1:
                        op("dve", lambda e, npc=npc: e.tensor_reduce(out=bisa.ap[:, 7:8], in_=bisa.ap[:, 0:npc], axis=AX.X,
                                                                     op=ALU.add), R=[bisa], W=[bisa])
                    sa_col = 7 if npc > 1 else 0
                    op("dve", lambda e: e.scalar_tensor_tensor(out=bisc.ap[:, 1:2], in0=bisc.ap[:, 0:1], scalar=2.0,
                                                               in1=bisa.ap[:, sa_col:sa_col + 1], op0=ALU.mult,
                                                               op1=ALU.subtract), R=[bisc, bisa], W=[bisc])
                    op("dve", lambda e, k=k: e.tensor_scalar(out=bis.ap[:, 3:4], in0=bisc.ap[:, 1:2],
                                                             scalar1=float(2 * TOPK - 1 - nA), scalar2=Wt.ap[:, k:k + 1],
                                                             op0=ALU.is_ge, op1=ALU.mult), R=[bisc, Wt], W=[bis])
                else:
                    op("dve", lambda e, k=k: e.tensor_scalar(out=bis.ap[:, 3:4], in0=bisc.ap[:, 0:1], scalar1=TOPK - 0.5,
                                                             scalar2=Wt.ap[:, k:k + 1], op0=ALU.is_ge, op1=ALU.mult),
                       R=[bisc, Wt], W=[bis])
                op("dve", lambda e, k=k: e.scalar_tensor_tensor(out=bis.ap[:, 1:2], in0=bis.ap[:, 1:2],
                                                                scalar=Wt.ap[:, k + 1:k + 2], in1=bis.ap[:, 3:4],
                                                                op0=ALU.subtract, op1=ALU.add), R=[bis, Wt], W=[bis])
            op("dve", lambda e: e.tensor_tensor(out=bis.ap[:, 0:1], in0=bis.ap[:, 1:2], in1=Wt.ap[:, KBIS:KBIS + 1],
                                                op=ALU.subtract), R=[bis, Wt], W=[bis])
            if dbg:
                dma(out=dbg_thr[j * 128:(j + 1) * 128, :], in_=bis.ap[:, 0:4], anchor=dmB, R=[bis])
                dma(out=dbg_isc[j * 128:(j + 1) * 128, 0:N], in_=big.ap[:, 0:N], anchor=dmD, R=[big])

            obank = lambda h: 4 + h // 3
            ooff = lambda h: (h % 3) * 129
            steps = []
            pend = []

            def emit_pv(item):
                kb, hg, kbuf, vbuf, kbl, pt = item
                for h4 in range(4):
                    h = hg * 4 + h4
                    ob = obank(h)
                    first = (kb == 0 and h % 3 == 0)
                    op("pe", lambda e, h=h, h4=h4, ob=ob, first=first: e.matmul(
                        bk(ob)[:, ooff(h):ooff(h) + 129], lhsT=pt.ap[:, h4 * 128:(h4 + 1) * 128],
                        rhs=vbuf.ap[:, kbl, h * 129:(h + 1) * 129], start=first, stop=(kb == n_kb - 1),
                        skip_group_check=True), R=[pt, vbuf], W=[banks[ob]])

            nqk = 0
            for gk in range(n_kb // KVG):
                kbuf, vbuf = get_kv()
                for kbl in range(KVG):
                    kb = gk * KVG + kbl
                    c = kb // 4
                    if kb % 4 == 0:
                        mbuf = mb[c % 2]
                        op("dve", lambda e, c=c, mbuf=mbuf: e.tensor_scalar(
                            out=mbuf.ap[:], in0=big.ap[:, c * 512:(c + 1) * 512], scalar1=bis.ap[:, 0:1], scalar2=NEG,
                            op0=ALU.is_lt, op1=ALU.mult), R=[big, bis], W=[mbuf])
                    mbuf = mb[c % 2]
                    near_i = kb - (4 * j - 1)
                    near = near_i >= 0
                    for hg in range(2):
                        lb = nqk % 4
                        pt = PT[nqk % 4]
                        nqk += 1
                        op("pe", lambda e, lb=lb, kb=kb, mbuf=mbuf: e.matmul(
                            bk(lb)[:, :], lhsT=mbuf.ap[:, (kb % 4) * 128:(kb % 4 + 1) * 128], rhs=ident4.ap[:],
                            start=True, stop=False, skip_group_check=True), R=[mbuf, ident4], W=[banks[lb]])
                        for h4 in range(4):
                            h = hg * 4 + h4
                            op("pe", lambda e, lb=lb, h=h, h4=h4, kbl=kbl, kbuf=kbuf, near=near: e.matmul(
                                bk(lb)[:, h4 * 128:(h4 + 1) * 128], lhsT=kbuf.ap[:, kbl, h * 128:(h + 1) * 128],
                                rhs=QT.ap[:, h, :], start=False, stop=(not near), skip_group_check=True),
                               R=[kbuf, QT], W=[banks[lb]])
                        if near:
                            for h4 in range(4):
                                h = hg * 4 + h4
                                op("pe", lambda e, lb=lb, h=h, h4=h4, near_i=near_i: e.matmul(
                                    bk(lb)[:, h4 * 128:(h4 + 1) * 128], lhsT=ident_b.ap[:], rhs=delta.ap[:, near_i, h, :],
                                    start=False, stop=True, skip_group_check=True), R=[ident_b, delta], W=[banks[lb]])
                        op("act", lambda e, lb=lb, pt=pt: e.activation(out=pt.ap[:], in_=bk(lb)[:, :], func=AF.Exp),
                           R=[banks[lb]], W=[pt])
                        pend.append((kb, hg, kbuf, vbuf, kbl, pt))
                        if len(pend) > PV_LAG:
                            emit_pv(pend.pop(0))
            while pend:
                emit_pv(pend.pop(0))
            wst["released"].add(j + 1)
            w_pump()

            Sx.barrier(skip=prefetch_bufs)
            for b3, nh in ((4, 3), (5, 3), (6, 2)):
                h0 = (b3 - 4) * 3
                op("dve", lambda e, b3=b3, nh=nh, h0=h0: e.reciprocal(
                    out=sm.ap[:, 24 + h0:24 + h0 + nh],
                    in_=bk(b3)[:, 0:nh * 129].rearrange("p (h n) -> p h n", n=129)[:, :, 128]),
                   R=[banks[b3]], W=[sm])
            for h in range(8):
                op("dve", lambda e, h=h: e.scalar_tensor_tensor(
                    out=o_y.ap[:, h * 128:(h + 1) * 128], in0=bk(obank(h))[:, ooff(h):ooff(h) + 128],
                    scalar=sm.ap[:, 24 + h:25 + h], in1=zs.ap[:, h * 128:(h + 1) * 128], op0=ALU.mult, op1=ALU.mult),
                   R=[banks[obank(h)], sm, zs], W=[o_y])
            transpose_to(o_y, lambda c: o_y.ap[:, c * 128:(c + 1) * 128], 8, o_yT, lambda r0: o_yT.ap[:, :])
            for nh in range(2):
                wu = get_unit()
                pb = nh
                for c in range(8):
                    op("pe", lambda e, c=c: e.matmul(bk(pb)[:, :], lhsT=o_yT.ap[:, c * 128:(c + 1) * 128], rhs=wu.c(c),
                                                     start=(c == 0), stop=(c == 7)), R=[wu.b(c), o_yT], W=[banks[pb]])
                op("dve", lambda e, nh=nh: e.tensor_tensor(out=o_x1.ap[:, nh * 512:(nh + 1) * 512], in0=bk(pb)[:, :],
                                                           in1=xt.ap[:, nh * 512:(nh + 1) * 512], op=ALU.add),
                   R=[banks[pb], xt], W=[o_x1])
            if dbg:
                dma(out=dbg_x1[j * 128:(j + 1) * 128, :], in_=o_x1.ap[:, :], anchor=dmC, R=[o_x1])

            rms_rstd(o_x1, o_x1.ap[:, :], 32)
            op("dve", lambda e: e.tensor_scalar(out=o_h1.ap[:, :], in0=o_x1.ap[:, :], scalar1=sm.ap[:, 32:33], scalar2=None,
                                                op0=ALU.mult), R=[o_x1, sm], W=[o_h1])
            transpose_to(o_h1, lambda c: o_h1.ap[:, c * 128:(c + 1) * 128], 8, o_h1T, lambda r0: o_h1T.ap[:, :])

            def proj_unit(pb):
                wu = get_unit()
                for c in range(8):
                    op("pe", lambda e, c=c: e.matmul(bk(pb)[:, :], lhsT=o_h1T.ap[:, c * 128:(c + 1) * 128], rhs=wu.c(c),
                                                     start=(c == 0), stop=(c == 7)), R=[wu.b(c), o_h1T], W=[banks[pb]])
            for uu in range(4):
                pb = uu % 3
                proj_unit(pb)
                op("act", lambda e, uu=uu, pb=pb: e.copy(out=o_v.ap[:, uu * 512:(uu + 1) * 512], in_=bk(pb)[:, :]),
                   R=[banks[pb]], W=[o_v])
                op("dve", lambda e, uu=uu: e.bn_stats(out=sm.ap[:, 36 + 6 * uu:42 + 6 * uu], in_=o_v.ap[:, uu * 512:(uu + 1) * 512]),
                   R=[o_v], W=[sm])
            op("dve", lambda e: e.bn_aggr(out=sm.ap[:, 60:62], in_=sm.ap[:, 36:60]), R=[sm], W=[sm])
            op("dve", lambda e: e.tensor_scalar(out=sm.ap[:, 62:63], in0=sm.ap[:, 61:62], scalar1=LN_EPS, scalar2=None,
                                                op0=ALU.add), R=[sm], W=[sm])
            op("act", lambda e: e.sqrt(out=sm.ap[:, 63:64], in_=sm.ap[:, 62:63]), R=[sm], W=[sm])
            op("dve", lambda e: e.reciprocal(out=sm.ap[:, 62:63], in_=sm.ap[:, 63:64]), R=[sm], W=[sm])
            op("dve", lambda e: e.tensor_scalar(out=o_v.ap[:, :], in0=o_v.ap[:, :], scalar1=sm.ap[:, 60:61],
                                                scalar2=sm.ap[:, 62:63], op0=ALU.subtract, op1=ALU.mult), R=[o_v, sm], W=[o_v])
            op("dve", lambda e: e.tensor_tensor(out=o_v.ap[:, :], in0=o_v.ap[:, :], in1=lng.ap[:], op=ALU.mult),
               R=[o_v, lng], W=[o_v])
            op("dve", lambda e: e.tensor_tensor(out=o_vln.ap[:, :], in0=o_v.ap[:, :], in1=lnb.ap[:], op=ALU.add),
               R=[o_v, lnb], W=[o_vln])
            for g in range(8):
                mbk = 3 + g // 2
                op("pe", lambda e, g=g, mbk=mbk: e.matmul(bk(mbk)[:, (g % 2) * 256:(g % 2 + 1) * 256], lhsT=wsTm.ap[:, g, :],
                                                          rhs=o_vln.ap[:, g * 256:(g + 1) * 256], start=(g % 2 == 0),
                                                          stop=True, skip_group_check=True), R=[wsTm, o_vln], W=[banks[mbk]])
            for uu in range(4):
                pb = uu % 3
                proj_unit(pb)
                op("act", lambda e, uu=uu, pb=pb: e.copy(out=o_u.ap[:, uu * 512:(uu + 1) * 512], in_=bk(pb)[:, :]),
                   R=[banks[pb]], W=[o_u])
            for g in range(8):
                mbk = 3 + g // 2
                op("dve", lambda e, g=g, mbk=mbk: e.scalar_tensor_tensor(
                    out=o_um.ap[:, g * 256:(g + 1) * 256], in0=bk(mbk)[:, (g % 2) * 256:(g % 2 + 1) * 256],
                    scalar=bsT.ap[:, g:g + 1], in1=o_u.ap[:, g * 256:(g + 1) * 256], op0=ALU.add, op1=ALU.mult),
                   R=[banks[mbk], bsT, o_u], W=[o_um])
            for uu in range(4):
                pb = uu % 3
                proj_unit(pb)
                zb = o_zs1[uu % 2]
                op("act", lambda e, pb=pb, zb=zb: e.activation(out=zb.ap[:, :], in_=bk(pb)[:, :], func=AF.Silu),
                   R=[banks[pb]], W=[zb])
                op("dve", lambda e, uu=uu, zb=zb: e.tensor_tensor(out=o_y1.ap[:, uu * 512:(uu + 1) * 512], in0=zb.ap[:, :],
                                                                   in1=o_um.ap[:, uu * 512:(uu + 1) * 512], op=ALU.mult),
                   R=[zb, o_um], W=[o_y1])
            transpose_to(o_y1, lambda c: o_y1.ap[:, c * 128:(c + 1) * 128], 16, o_y1T,
                         lambda r0: o_y1T.ap[:, r0 * 128:(r0 + 8) * 128])
            for nh in range(2):
                pb = nh
                for kh in range(2):
                    wu = get_unit()
                    for c in range(8):
                        kc = kh * 8 + c
                        op("pe", lambda e, c=c, kc=kc: e.matmul(bk(pb)[:, :], lhsT=o_y1T.ap[:, kc * 128:(kc + 1) * 128],
                                                                rhs=wu.c(c), start=(kc == 0), stop=(kc == 15)),
                           R=[wu.b(c), o_y1T], W=[banks[pb]])
                op("dve", lambda e, nh=nh: e.tensor_tensor(out=o_x2.ap[:, nh * 512:(nh + 1) * 512], in0=bk(pb)[:, :],
                                                           in1=o_x1.ap[:, nh * 512:(nh + 1) * 512], op=ALU.add),
                   R=[banks[pb], o_x1], W=[o_x2])
            rms_rstd(o_x2, o_x2.ap[:, :], 20)
            op("dve", lambda e: e.scalar_tensor_tensor(out=o_o.ap[:, :], in0=o_x2.ap[:, :], scalar=sm.ap[:, 20:21],
                                                       in1=gf.ap[:], op0=ALU.mult, op1=ALU.mult), R=[o_x2, sm, gf], W=[o_o])
            dma(out=out_d[j * 128:(j + 1) * 128, :], in_=o_o.ap[:, :], anchor=dmA, R=[o_o])
        Sx.barrier()
        print("instructions:", Sx.nins, "waits:", Sx.nwait, "sems:", len(Sx.anchors) + 5)
    return nc


def _t5_bucket_np(rel):
    import jax
    import jax.numpy as jnp
    with jax.default_device(jax.devices("cpu")[0]):
        return _t5_bucket_cpu(jnp, rel)


def _t5_bucket_cpu(jnp, rel):
    rel = jnp.asarray(rel, dtype=jnp.int32)
    half = 16
    max_exact = 8
    ret = jnp.where(rel < 0, half, 0)
    n = jnp.abs(rel)
    nf = jnp.maximum(n, 1).astype(jnp.float32)
    large = max_exact + (jnp.log(nf / max_exact) / math.log(128 / max_exact) * (half - max_exact)).astype(jnp.int32)
    large = jnp.minimum(large, half - 1)
    return np.asarray(ret + jnp.where(n < max_exact, n, large))


_NC_CACHE = {}


def _host_inputs(x, norm_g, final_g, rel_bias, a_w_in, a_w_out, b_w_in, b_ln_g, b_ln_b, b_w_s, b_b_s, b_w_out):
    B, S, _ = x.shape
    NBLK = S // 128
    NQ = NBLK // 4
    f = np.float32
    bc = lambda v, n: np.ascontiguousarray(np.broadcast_to(np.asarray(v, f)[None, :], (128, n)))
    common = {
        "w_in0": np.ascontiguousarray(a_w_in[0], f), "w_out0": np.ascontiguousarray(a_w_out[0], f),
        "w_in1": np.ascontiguousarray(b_w_in[0], f), "w_out1": np.ascontiguousarray(b_w_out[0], f),
        "g0c": np.ascontiguousarray(np.asarray(norm_g[0], f).reshape(8, 128).T),
        "g1c": np.ascontiguousarray(np.asarray(norm_g[1], f).reshape(8, 128).T),
        "gf": bc(final_g, D), "lng": bc(b_ln_g[0], 2048), "lnb": bc(b_ln_b[0], 2048),
        "bsT": np.ascontiguousarray(np.asarray(b_b_s[0], f).T),
        "wsT": np.ascontiguousarray(np.transpose(np.asarray(b_w_s[0], f), (2, 0, 1))),
        "tri": np.ascontiguousarray(np.triu(np.ones((128, 128), f))),
        "cb": bc(np.asarray(rel_bias, f)[15], 8),
        "ident": np.eye(128, dtype=f),
    }
    s_loc = np.arange(128)[:, None]
    t_loc = np.arange(128)[None, :]
    rb = np.asarray(rel_bias, f)
    in_maps = []
    for c in range(8):
        b, r = c // 4, c % 4
        m = dict(common)
        m["xb"] = np.ascontiguousarray(x[b], f)
        xr = np.asarray(x[b], f).reshape(NBLK, 128, D)
        m["xq"] = np.ascontiguousarray(xr[r::4].reshape(NQ * 128, D))
        bt = np.zeros((128, 5, 8, 128), f)
        for i in range(5):
            rel = (t_loc - s_loc) - 128 * (i - 1 - r)
            bidx = _t5_bucket_np(rel)
            bt[:, i, :, :] = np.transpose(rb[bidx], (0, 2, 1))
        m["biasT"] = np.ascontiguousarray(bt.reshape(128, 5 * 8 * 128))
        ad = np.zeros((128, 4, 128), f)
        for rp in range(4):
            if rp > r:
                ad[:, rp, :] = -1e30
            elif rp == r:
                ad[:64, rp, 64:] = -1e30
        m["admis"] = np.ascontiguousarray(ad.reshape(128, 512))
        in_maps.append(m)
    return in_maps, NBLK, NQ


def kernel(x, norm_g, final_g, rel_bias, a_w_in, a_w_out, b_w_in, b_ln_g, b_ln_b, b_w_s, b_b_s, b_w_out, _dbg=False):
    x = np.asarray(x)
    in_maps, NBLK, NQ = _host_inputs(x, norm_g, final_g, rel_bias, a_w_in, a_w_out, b_w_in, b_ln_g, b_ln_b,
                                     b_w_s, b_b_s, b_w_out)
    key = (NBLK, NQ, _dbg)
    if key not in _NC_CACHE:
        _NC_CACHE[key] = build(NBLK, NQ, dbg=_dbg)
    nc = _NC_CACHE[key]
    res = run_bass_kernel_spmd(nc, in_maps, core_ids=list(range(8)))
    B, S, _ = x.shape
    out = np.zeros((B, NBLK, 128, D), np.float32)
    for c in range(8):
        b, r = c // 4, c % 4
        out[b, r::4] = np.asarray(res.results[c]["out"]).reshape(NQ, 128, D)
    out = out.reshape(B, S, D)
    if _dbg:
        return out, res
    return out
```

```python
import contextlib
import math
import numpy as np
import concourse.bass as bass
import concourse.mybir as mybir
from concourse.bass_utils import run_bass_kernel_spmd

F32 = mybir.dt.float32
BF16 = mybir.dt.bfloat16
AF = mybir.ActivationFunctionType
ALU = mybir.AluOpType
AX = mybir.AxisListType

D = 1024
TOPK = 256
RMS_EPS = 1e-6
LN_EPS = 1e-5
KBIS = 16
KVG = 2
KVS = 3
WS = 2
NEG = -30000.0
SAME_ENGINE_SYNC = True
USE_ACT_COUNT = True
ACT_FRAC = 0.5
PV_LAG = 2
NHS_LIMIT = 10


class Buf:
    def __init__(self, name, ap=None):
        self.name = name
        self.ap = ap
        self.w = None
        self.r = {}
        self.dsem = None
        self.dcnt = 0


class Sched:
    def __init__(self, nc, es):
        self.nc = nc
        self.es = es
        self.eng = {"pe": nc.tensor, "act": nc.scalar, "dve": nc.vector, "pool": nc.gpsimd, "sp": nc.sync}
        self.sem = {k: es.enter_context(nc.semaphore("s_" + k)) for k in self.eng}
        self.cnt = {k: 0 for k in self.eng}
        self.seen = {k: {} for k in self.eng}
        self.anchors = []
        self.nwait = 0
        self.nins = 0

    def _semof(self, key):
        if isinstance(key, str):
            return self.sem[key]
        return key.dsem

    def _wait(self, E, key, val):
        if val <= 0:
            return
        if self.seen[E].get(key, 0) >= val:
            return
        self.eng[E].wait_ge(self._semof(key), val)
        self.seen[E][key] = val
        self.nwait += 1

    def _deps(self, E, R, W):
        deps = {}

        def add(tok):
            k, v = tok
            if deps.get(k, 0) < v:
                deps[k] = v
        for b in R:
            if b.w is not None:
                add(b.w)
        for b in W:
            if b.w is not None:
                add(b.w)
            for k, v in b.r.items():
                add((k, v))
        for k, v in deps.items():
            if k == E:
                if E == "pe" or not SAME_ENGINE_SYNC:
                    continue
            self._wait(E, k, v)

    def _mark(self, tok, R, W):
        k, v = tok
        for b in R:
            if b.r.get(k, 0) < v:
                b.r[k] = v
        for b in W:
            b.w = tok
            b.r = {}

    def op(self, E, fn, R=(), W=()):
        self._deps(E, R, W)
        ins = fn(self.eng[E])
        self.cnt[E] += 1
        ins.then_inc(self.sem[E], 1)
        self.nins += 1
        self._mark((E, self.cnt[E]), R, W)
        return ins

    def dma(self, out, in_, anchor, R=(), W=(), Q="sp"):
        self._deps(Q, R, W)
        if anchor.dsem is None:
            anchor.dsem = self.es.enter_context(self.nc.semaphore("d_" + anchor.name))
            self.anchors.append(anchor)
        ins = self.eng[Q].dma_start(out=out, in_=in_)
        anchor.dcnt += 16
        ins.then_inc(anchor.dsem, 16)
        self.nins += 1
        self._mark((anchor, anchor.dcnt), R, W)
        return ins

    def barrier(self, skip=()):
        skip_ids = {id(b) for b in skip}
        toks = [(k, self.cnt[k]) for k in ("pe", "act", "dve", "pool")]
        toks += [(a, a.dcnt) for a in self.anchors if id(a) not in skip_ids]
        for E in self.eng:
            for k, v in toks:
                if k == E:
                    continue
                self._wait(E, k, v)


def build(NBLK, NQ, dbg=False):
    assert NBLK == 4 * NQ
    S = NBLK * 128
    NG = NBLK // 4
    nc = bass.Bass("TRN2", target_bir_lowering=False)

    def din(name, shape):
        return nc.dram_tensor(name, list(shape), F32, kind="ExternalInput").ap()

    xb = din("xb", [S, D])
    xq = din("xq", [NQ * 128, D])
    w_in0 = din("w_in0", [D, 4680])
    w_out0 = din("w_out0", [D, D])
    w_in1 = din("w_in1", [D, 6144])
    w_out1 = din("w_out1", [2048, D])
    g0c_d = din("g0c", [128, 8])
    g1c_d = din("g1c", [128, 8])
    gf_d = din("gf", [128, D])
    lng_d = din("lng", [128, 2048])
    lnb_d = din("lnb", [128, 2048])
    bsT_d = din("bsT", [128, 8])
    wsT_d = din("wsT", [128, 8, 128])
    tri_d = din("tri", [128, 128])
    biasT_d = din("biasT", [128, 5 * 8 * 128])
    cb_d = din("cb", [128, 8])
    admis_d = din("admis", [128, 512])
    ident_d = din("ident", [128, 128])
    out_d = nc.dram_tensor("out", [NQ * 128, D], F32, kind="ExternalOutput").ap()
    if dbg:
        dbg_x1 = nc.dram_tensor("dbg_x1", [NQ * 128, D], F32, kind="ExternalOutput").ap()
        dbg_thr = nc.dram_tensor("dbg_thr", [NQ * 128, 4], F32, kind="ExternalOutput").ap()

    if dbg:
        dbg_isc = nc.dram_tensor("dbg_isc", [NQ * 128, S], F32, kind="ExternalOutput").ap()
        kT_scr = nc.dram_tensor("kT_scr", [NBLK, 128, 1024], BF16, kind="ExternalOutput").ap()
        v_scr = nc.dram_tensor("v_scr", [NBLK, 128, 1032], BF16, kind="ExternalOutput").ap()
    else:
        kT_scr = nc.dram_tensor("kT_scr", [NBLK, 128, 1024], BF16).ap()
        v_scr = nc.dram_tensor("v_scr", [NBLK, 128, 1032], BF16).ap()
    wq_scr = nc.dram_tensor("wq_scr", [5, 128, 8, 512], BF16).ap()
    wo0_scr = nc.dram_tensor("wo0_scr", [2, 128, 8, 512], BF16).ap()
    w1_scr = nc.dram_tensor("w1_scr", [12, 128, 8, 512], BF16).ap()
    wo1_scr = nc.dram_tensor("wo1_scr", [4, 128, 8, 512], BF16).ap()
    scrK = Buf("scrK")
    scrV = Buf("scrV")
    scrW = Buf("scrW")

    es = contextlib.ExitStack()
    with es:
        Sx = Sched(nc, es)
        op = Sx.op
        dma = Sx.dma

        def sb(name, shape, dt, stack=es):
            t = stack.enter_context(nc.sbuf_tensor("sb_" + name, list(shape), dt))
            return Buf(name, t)

        banks = []
        for i in range(8):
            t = es.enter_context(nc.psum_tensor("bank%d" % i, [128, 512], F32))
            banks.append(Buf("bank%d" % i, t))

        def bk(i):
            return banks[i].ap

        def bkb(i):
            return banks[i].ap[:, 0:512].bitcast(BF16)

        ident_f = sb("ident_f", [128, 128], F32)
        ident_b = sb("ident_b", [128, 128], BF16)
        ident4 = sb("ident4", [128, 512], BF16)
        gf = sb("gf", [128, D], F32)
        lng = sb("lng", [128, 2048], F32)
        lnb = sb("lnb", [128, 2048], F32)
        bsT = sb("bsT", [128, 8], F32)
        wsTm = sb("wsTm", [128, 8, 128], BF16)
        cb = sb("cb", [128, 8], F32)
        g0c = sb("g0c", [128, 8], F32)
        g1c = sb("g1c", [128, 8], F32)
        admis = sb("admis", [128, 512], F32)
        delta = sb("delta", [128, 5, 8, 128], BF16)
        wwi = sb("wwi", [128, 8, 8], BF16)
        kiT = sb("kiT", [128, S], BF16)
        cpow = sb("cpow", [128, KBIS + 1], F32)
        junk_act = sb("junk_act", [128, 2], BF16)
        junk_dve = sb("junk_dve", [128, 2], BF16)
        constA = Buf("constA")

        consts = []

        def cload(buf, src):
            dma(out=buf.ap[:], in_=src, anchor=constA, W=[buf])
            consts.append(buf)

        cload(ident_f, ident_d[:, :])
        cload(gf, gf_d[:, :])
        cload(lng, lng_d[:, :])
        cload(lnb, lnb_d[:, :])
        cload(bsT, bsT_d[:, :])
        cload(cb, cb_d[:, :])
        cload(g0c, g0c_d[:, :])
        cload(g1c, g1c_d[:, :])
        cload(admis, admis_d[:, :])

        p1 = contextlib.ExitStack()
        es.enter_context(p1)
        wkv = sb("wkv", [128, 8, 2112], BF16, p1)

        with contextlib.ExitStack() as ps:
            stgf = [sb("stgf%d" % i, [128, 6144], F32, ps) for i in range(2)]
            stgb = [sb("stgb%d" % i, [128, 6144], BF16, ps) for i in range(2)]
            wsT_f = sb("wsT_f", [128, 8, 128], F32, ps)
            tri_f = sb("tri_f", [128, 128], F32, ps)
            cload(wsT_f, wsT_d[:, :, :])
            cload(tri_f, tri_d[:, :])
            for b in consts:
                b.w = (constA, constA.dcnt)
            op("dve", lambda e: e.tensor_copy(out=ident_b.ap[:], in_=ident_f.ap[:]), R=[ident_f], W=[ident_b])
            for q in range(4):
                op("dve", lambda e, q=q: e.tensor_copy(out=ident4.ap[:, q * 128:(q + 1) * 128], in_=ident_f.ap[:]),
                   R=[ident_f], W=[ident4])
            for k in range(KBIS + 1):
                op("pool", lambda e, k=k: e.memset(cpow.ap[:, k:k + 1], float(2.0 ** -(k + 1))), W=[cpow])
            for g in range(8):
                op("dve", lambda e, g=g: e.tensor_tensor(out=wsTm.ap[:, g, :], in0=wsT_f.ap[:, g, :], in1=tri_f.ap[:],
                                                         op=ALU.mult), R=[wsT_f, tri_f], W=[wsTm])
            for i in range(5):
                st = stgf[i % 2]
                dma(out=st.ap[:, 0:1024], in_=biasT_d[:, i * 1024:(i + 1) * 1024], anchor=st, W=[st])
                for h in range(8):
                    op("dve", lambda e, i=i, h=h, st=st: e.tensor_scalar(
                        out=delta.ap[:, i, h, :], in0=st.ap[:, h * 128:(h + 1) * 128], scalar1=cb.ap[:, h:h + 1],
                        scalar2=None, op0=ALU.subtract), R=[st, cb], W=[delta])

            nprep = [0]

            def prep_weight(W_ap, K, C, gcol, dest_fn):
                for c in range(K // 128):
                    i = nprep[0] % 2
                    nprep[0] += 1
                    sf, sbb = stgf[i], stgb[i]
                    dma(out=sf.ap[:, 0:C], in_=W_ap[c * 128:(c + 1) * 128, :], anchor=sf, W=[sf])
                    if gcol is not None:
                        op("dve", lambda e, c=c: e.tensor_scalar(out=sbb.ap[:, 0:C], in0=sf.ap[:, 0:C],
                                                                 scalar1=gcol.ap[:, c:c + 1], scalar2=None,
                                                                 op0=ALU.mult), R=[sf, gcol], W=[sbb])
                    else:
                        op("act", lambda e: e.copy(out=sbb.ap[:, 0:C], in_=sf.ap[:, 0:C]), R=[sf], W=[sbb])
                    dest_fn(c, sbb)

            def units_store(scr, u0, nu, c, sbb, col0):
                dma(out=scr[u0:u0 + nu, :, c, :].rearrange("u p n -> p u n"),
                    in_=sbb.ap[:, col0:col0 + nu * 512].rearrange("p (u n) -> p u n", n=512),
                    anchor=sbb, R=[sbb], W=[scrW])

            def dest_in0(c, sbb):
                units_store(wq_scr, 0, 2, c, sbb, 0)
                units_store(wq_scr, 2, 2, c, sbb, 3072)
                units_store(wq_scr, 4, 1, c, sbb, 4096)
                op("pool", lambda e: e.tensor_copy(out=wkv.ap[:, c, 0:2048], in_=sbb.ap[:, 1024:3072]), R=[sbb], W=[wkv])
                op("pool", lambda e: e.tensor_copy(out=wkv.ap[:, c, 2048:2112], in_=sbb.ap[:, 4608:4672]), R=[sbb], W=[wkv])
                op("pool", lambda e: e.tensor_copy(out=wwi.ap[:, c, :], in_=sbb.ap[:, 4672:4680]), R=[sbb], W=[wwi])

            prep_weight(w_in0, 1024, 4680, g0c, dest_in0)
            prep_weight(w_out0, 1024, 1024, None, lambda c, sbb: units_store(wo0_scr, 0, 2, c, sbb, 0))
            prep_weight(w_in1, 1024, 6144, g1c, lambda c, sbb: units_store(w1_scr, 0, 12, c, sbb, 0))

            def dest_out1(kc, sbb):
                kh, c = kc // 8, kc % 8
                units_store(wo1_scr, kh, 1, c, sbb, 0)
                units_store(wo1_scr, 2 + kh, 1, c, sbb, 512)

            prep_weight(w_out1, 2048, 1024, None, dest_out1)
            Sx.barrier()

        with contextlib.ExitStack() as s1:
            xg = [sb("xg%d" % i, [128, 4, D], F32, s1) for i in range(2)]
            xn = sb("xn", [128, 4, D], BF16, s1)
            hT = [sb("hT%d" % i, [128, 8, 512], BF16, s1) for i in range(2)]
            kTs = [sb("kTs%d" % i, [128, 4, 8, 128], BF16, s1) for i in range(2)]
            Vs = [sb("Vs%d" % i, [128, 4, 8, 129], BF16, s1) for i in range(2)]
            st1 = sb("st1", [128, 16], F32, s1)
            for i in range(2):
                op("pool", lambda e, i=i: e.memset(Vs[i].ap[:], 1.0), W=[Vs[i]])
            op("pool", lambda e: e.memset(kiT.ap[64:128, :], 0.0), W=[kiT])
            ev = [0]

            def evac(fn_act, fn_dve, R, W):
                ev[0] += 1
                if ev[0] % 2 == 0:
                    op("act", fn_act, R=R, W=W)
                else:
                    op("dve", fn_dve, R=R, W=W)

            def load_xg(g):
                b = xg[g % 2]
                dma(out=b.ap[:], in_=xb[g * 512:(g + 1) * 512, :].rearrange("(t p) d -> p t d", p=128), anchor=b, W=[b])

            load_xg(0)
            for g in range(NG):
                if g + 1 < NG:
                    load_xg(g + 1)
                xgb = xg[g % 2]
                h_ = hT[g % 2]
                for t in range(4):
                    op("act", lambda e, t=t: e.activation(out=junk_act.ap[:, 0:1].to_broadcast([128, D]), in_=xgb.ap[:, t, :], func=AF.Square,
                                                          accum_out=st1.ap[:, t:t + 1]), R=[xgb], W=[junk_act, st1])
                op("dve", lambda e: e.tensor_scalar(out=st1.ap[:, 4:8], in0=st1.ap[:, 0:4], scalar1=1.0 / D,
                                                    scalar2=RMS_EPS, op0=ALU.mult, op1=ALU.add), R=[st1], W=[st1])
                op("act", lambda e: e.sqrt(out=st1.ap[:, 8:12], in_=st1.ap[:, 4:8]), R=[st1], W=[st1])
                op("dve", lambda e: e.reciprocal(out=st1.ap[:, 12:16], in_=st1.ap[:, 8:12]), R=[st1], W=[st1])
                for t in range(4):
                    op("dve", lambda e, t=t: e.tensor_scalar(out=xn.ap[:, t, :], in0=xgb.ap[:, t, :],
                                                             scalar1=st1.ap[:, 12 + t:13 + t], scalar2=None,
                                                             op0=ALU.mult), R=[xgb, st1], W=[xn])
                    tb = 6 + (t % 2)
                    for c in range(8):
                        op("pe", lambda e, t=t, c=c, tb=tb: e.transpose(out=bkb(tb)[:, c * 128:(c + 1) * 128],
                                                                        in_=xn.ap[:, t, c * 128:(c + 1) * 128],
                                                                        identity=ident_b.ap[:]),
                           R=[xn, ident_b], W=[banks[tb]])
                    evac(lambda e, t=t, tb=tb: e.copy(out=h_.ap[:, :, t * 128:(t + 1) * 128],
                                                      in_=bkb(tb).rearrange("p (c n) -> p c n", n=128)),
                         lambda e, t=t, tb=tb: e.tensor_copy(out=h_.ap[:, :, t * 128:(t + 1) * 128],
                                                             in_=bkb(tb).rearrange("p (c n) -> p c n", n=128)),
                         R=[banks[tb]], W=[h_])
                kb_, vb_ = kTs[g % 2], Vs[g % 2]
                for h in range(8):
                    pb = h % 4
                    for c in range(8):
                        op("pe", lambda e, h=h, c=c, pb=pb: e.matmul(bk(pb)[:, :], lhsT=wkv.ap[:, c, h * 128:(h + 1) * 128],
                                                                     rhs=h_.ap[:, c, :], start=(c == 0), stop=(c == 7)),
                           R=[wkv, h_], W=[banks[pb]])
                    evac(lambda e, h=h, pb=pb: e.copy(out=kb_.ap[:, :, h, :], in_=bk(pb).rearrange("p (b n) -> p b n", n=128)),
                         lambda e, h=h, pb=pb: e.tensor_copy(out=kb_.ap[:, :, h, :], in_=bk(pb).rearrange("p (b n) -> p b n", n=128)),
                         R=[banks[pb]], W=[kb_])
                for t in range(4):
                    for hg in range(2):
                        pb = (t * 2 + hg) % 4
                        for c in range(8):
                            op("pe", lambda e, t=t, hg=hg, c=c, pb=pb: e.matmul(
                                bk(pb)[:, :], lhsT=h_.ap[:, c, t * 128:(t + 1) * 128],
                                rhs=wkv.ap[:, c, 1024 + hg * 512:1024 + (hg + 1) * 512], start=(c == 0), stop=(c == 7)),
                               R=[wkv, h_], W=[banks[pb]])
                        evac(lambda e, t=t, hg=hg, pb=pb: e.copy(out=vb_.ap[:, t, hg * 4:(hg + 1) * 4, 0:128],
                                                                 in_=bk(pb).rearrange("p (h n) -> p h n", n=128)),
                             lambda e, t=t, hg=hg, pb=pb: e.tensor_copy(out=vb_.ap[:, t, hg * 4:(hg + 1) * 4, 0:128],
                                                                        in_=bk(pb).rearrange("p (h n) -> p h n", n=128)),
                             R=[banks[pb]], W=[vb_])
                for c in range(8):
                    op("pe", lambda e, c=c: e.matmul(bk(4)[0:64, :], lhsT=wkv.ap[:, c, 2048:2112], rhs=h_.ap[:, c, :],
                                                     start=(c == 0), stop=(c == 7)), R=[wkv, h_], W=[banks[4]])
                evac(lambda e: e.copy(out=kiT.ap[0:64, g * 512:(g + 1) * 512], in_=bk(4)[0:64, :]),
                     lambda e: e.tensor_copy(out=kiT.ap[0:64, g * 512:(g + 1) * 512], in_=bk(4)[0:64, :]),
                     R=[banks[4]], W=[kiT])
                dma(out=kT_scr[g * 4:(g + 1) * 4].rearrange("b d n -> d b n"),
                    in_=kb_.ap[:].rearrange("p b h n -> p b (h n)"), anchor=kb_, R=[kb_], W=[scrK])
                dma(out=v_scr[g * 4:(g + 1) * 4].rearrange("b s n -> s b n"),
                    in_=vb_.ap[:].rearrange("p b h n -> p b (h n)"), anchor=vb_, R=[vb_], W=[scrV])
            Sx.barrier()
        p1.close()

        big = sb("big", [128, 16384], F32)
        Kc = [sb("Kc%d" % i, [128, KVG, 1024], BF16) for i in range(KVS)]
        Vc = [sb("Vc%d" % i, [128, KVG, 1032], BF16) for i in range(KVS)]
        wslh = [sb("wslh%d" % i, [128, 4, 512], BF16) for i in range(4)]
        xqt = [sb("xqt%d" % i, [128, D], F32) for i in range(2)]
        xnq = sb("xnq", [128, D], BF16)
        hTq = sb("hTq", [128, 8, 128], BF16)
        QT = sb("QT", [128, 8, 128], BF16)
        qiT = sb("qiT", [128, 8, 128], BF16)
        zs = sb("zs", [128, D], F32)
        diagw = sb("diagw", [128, 8, 128], BF16)
        Rb_all = sb("Rb_all", [128, 6, 512], BF16)
        Rb = [Buf("Rb%d" % i, Rb_all.ap[:, i, :]) for i in range(6)]
        Rb_flat = Rb_all.ap[:].rearrange("p a n -> p (a n)")
        bisa = sb("bisa", [128, 8], F32)
        bisc = sb("bisc", [128, 2], F32)
        PT = [sb("PT%d" % i, [128, 512], BF16) for i in range(4)]
        mb = [sb("mb%d" % i, [128, 512], BF16) for i in range(2)]
        sm = sb("sm", [128, 64], F32)
        Wt = sb("Wt", [128, KBIS + 1], F32)
        bis = sb("bis", [128, 8], F32)

        def ov(name, off, n, dt):
            a = big.ap[:, off:off + n]
            if dt == BF16:
                a = a.bitcast(BF16)
            return Buf(name, a)
        o_y = ov("o_y", 0, 512, BF16)
        o_yT = ov("o_yT", 512, 512, BF16)
        o_x1 = ov("o_x1", 1024, 1024, F32)
        o_h1 = ov("o_h1", 2048, 512, BF16)
        o_h1T = ov("o_h1T", 2560, 512, BF16)
        o_v = ov("o_v", 3072, 2048, F32)
        o_u = ov("o_u", 5120, 2048, F32)
        o_vln = ov("o_vln", 7168, 1024, BF16)
        o_um = ov("o_um", 8192, 2048, F32)
        o_zs1 = [ov("o_zs1%d" % i, 10240 + 512 * i, 512, F32) for i in range(2)]
        o_y1 = ov("o_y1", 11264, 1024, BF16)
        o_y1T = ov("o_y1T", 12288, 1024, BF16)
        o_x2 = ov("o_x2", 13312, 1024, F32)
        o_o = ov("o_o", 14336, 1024, F32)

        overlays = [o_y, o_yT, o_x1, o_h1, o_h1T, o_v, o_u, o_vln, o_um] + o_zs1 + [o_y1, o_y1T, o_x2, o_o]

        def fold_tokens(srcs, dsts):
            for s_ in srcs:
                toks = ([s_.w] if s_.w is not None else []) + list(s_.r.items())
                for d_ in dsts:
                    for k_, v_ in toks:
                        if d_.r.get(k_, 0) < v_:
                            d_.r[k_] = v_

        units = []
        for j in range(NQ):
            units += [wq_scr[0], wq_scr[1], wq_scr[4], wq_scr[2], wq_scr[3], wo0_scr[0], wo0_scr[1]]
            units += [w1_scr[u] for u in (4, 5, 6, 7, 0, 1, 2, 3, 8, 9, 10, 11)]
            units += [wo1_scr[u] for u in range(4)]
        hslots = [(b, b.ap[:]) for b in wslh]
        hslots += [(b, b.ap[:].rearrange("p a (b n) -> p (a b) n", n=512)) for b in Kc]
        hslots += [(b, b.ap[:].rearrange("p a n -> p (a n)")[:, 0:2048].rearrange("p (c n) -> p c n", n=512)) for b in Vc]
        hslots = hslots[:NHS_LIMIT]
        NHS = len(hslots)
        halves = []
        seg_sizes = [5] + [23] * (NQ - 1) + [18]
        ui = 0
        for sg, nu in enumerate(seg_sizes):
            for q in range(2 * nu):
                halves.append((units[ui + q // 2], q % 2, sg, q))
            ui += nu
        assert ui == len(units)
        wst = {"issued": 0, "next": 0, "released": {0}, "occ": [None] * NHS}

        def w_pump():
            consumed = 2 * wst["next"]
            while wst["issued"] < len(halves) and wst["issued"] <= consumed + 9:
                m = wst["issued"]
                uap, hf, sg, q = halves[m]
                sl = q % NHS
                if q >= 4 and sg not in wst["released"]:
                    break
                oc = wst["occ"][sl]
                if oc is not None and oc >= consumed:
                    break
                b, view = hslots[sl]
                dma(out=view, in_=uap[:, 4 * hf:4 * hf + 4, :], anchor=b, R=[scrW], W=[b])
                wst["occ"][sl] = m
                wst["issued"] += 1

        class WU:
            def __init__(self, n):
                self.h = []
                for hf in range(2):
                    uap, hf_, sg, q = halves[2 * n + hf]
                    self.h.append(hslots[q % NHS])

            def c(self, c):
                return self.h[c // 4][1][:, c % 4, :]

            def b(self, c):
                return self.h[c // 4][0]

        def get_unit():
            n = wst["next"]
            w_pump()
            assert wst["issued"] >= 2 * n + 2, "weight half-unit not issued"
            wst["next"] += 1
            return WU(n)

        def kv_slots_free_of_weights():
            consumed = 2 * wst["next"]
            for sl in range(4, NHS):
                oc = wst["occ"][sl]
                assert oc is None or oc < consumed, "K/V slot still holds unconsumed weights"

        kvst = {"issued": 0, "next": 0, "limit": 0}
        kvlist = []
        for j in range(NQ):
            for gk in range((4 * j + 4) // KVG):
                kvlist.append(gk * KVG)

        def kv_prefetch(upto):
            while kvst["issued"] < min(upto, kvst["limit"]):
                m = kvst["issued"]
                kb0 = kvlist[m]
                kbuf, vbuf = Kc[m % KVS], Vc[m % KVS]
                dma(out=kbuf.ap[:], in_=kT_scr[kb0:kb0 + KVG].rearrange("b d n -> d b n"), anchor=kbuf, R=[scrK], W=[kbuf])
                dma(out=vbuf.ap[:], in_=v_scr[kb0:kb0 + KVG].rearrange("b s n -> s b n"), anchor=vbuf, R=[scrV], W=[vbuf])
                kvst["issued"] += 1

        def get_kv():
            n = kvst["next"]
            kvst["next"] += 1
            kv_prefetch(n + KVS - 1)
            return Kc[n % KVS], Vc[n % KVS]

        ev2 = [0]

        def evac2(fn_act, fn_dve, R, W):
            ev2[0] += 1
            if ev2[0] % 2 == 0:
                op("act", fn_act, R=R, W=W)
            else:
                op("dve", fn_dve, R=R, W=W)

        def rms_rstd(src_buf, src_ap, col):
            op("act", lambda e: e.activation(out=junk_act.ap[:, 0:1].to_broadcast([128, D]), in_=src_ap, func=AF.Square,
                                             accum_out=sm.ap[:, col:col + 1]), R=[src_buf], W=[junk_act, sm])
            op("dve", lambda e: e.tensor_scalar(out=sm.ap[:, col + 1:col + 2], in0=sm.ap[:, col:col + 1], scalar1=1.0 / D,
                                                scalar2=RMS_EPS, op0=ALU.mult, op1=ALU.add), R=[sm], W=[sm])
            op("act", lambda e: e.sqrt(out=sm.ap[:, col + 2:col + 3], in_=sm.ap[:, col + 1:col + 2]), R=[sm], W=[sm])
            op("dve", lambda e: e.reciprocal(out=sm.ap[:, col:col + 1], in_=sm.ap[:, col + 2:col + 3]), R=[sm], W=[sm])

        def transpose_to(src_buf, src_ap_fn, nch, dst_buf, dst_ap_fn):
            for r0 in range(0, nch, 8):
                for c in range(r0, r0 + 8):
                    op("pe", lambda e, c=c: e.transpose(out=bkb(7)[:, (c - r0) * 128:(c - r0 + 1) * 128],
                                                        in_=src_ap_fn(c), identity=ident_b.ap[:]),
                       R=[src_buf, ident_b], W=[banks[7]])
                evac2(lambda e: e.copy(out=dst_ap_fn(r0), in_=bkb(7)),
                      lambda e: e.tensor_copy(out=dst_ap_fn(r0), in_=bkb(7)), R=[banks[7]], W=[dst_buf])

        op("pool", lambda e: e.memset(qiT.ap[64:128, :, :], 0.0), W=[qiT])
        prefetch_bufs = wslh + Kc + Vc + xqt
        dmA = Buf("storeA")
        dmB = Buf("storeB")
        dmC = Buf("storeC")
        dmD = Buf("storeD")

        dma(out=xqt[0].ap[:], in_=xq[0:128, :], anchor=xqt[0], W=[xqt[0]])
        for j in range(NQ):
            n_kb = 4 * j + 4
            n_ch = j + 1
            N = n_ch * 512
            xt = xqt[j % 2]
            if j + 1 < NQ:
                nx = xqt[(j + 1) % 2]
                dma(out=nx.ap[:], in_=xq[(j + 1) * 128:(j + 2) * 128, :], anchor=nx, W=[nx])
            rms_rstd(xt, xt.ap[:], 0)
            op("dve", lambda e: e.tensor_scalar(out=xnq.ap[:], in0=xt.ap[:], scalar1=sm.ap[:, 0:1], scalar2=None,
                                                op0=ALU.mult), R=[xt, sm], W=[xnq])
            transpose_to(xnq, lambda c: xnq.ap[:, c * 128:(c + 1) * 128], 8, hTq,
                         lambda r0: hTq.ap[:].rearrange("p c n -> p (c n)"))
            for u in range(2):
                wu = get_unit()
                pb = u % 4
                first = True
                for h4 in range(4):
                    for c in range(8):
                        op("pe", lambda e, h4=h4, c=c, first=first: e.matmul(
                            bk(pb)[:, h4 * 128:(h4 + 1) * 128], lhsT=wu.c(c)[:, h4 * 128:(h4 + 1) * 128],
                            rhs=hTq.ap[:, c, :], start=first, stop=(c == 7), skip_group_check=True),
                           R=[wu.b(c), hTq], W=[banks[pb]])
                        first = False
                op("act", lambda e, u=u: e.mul(out=QT.ap[:, u * 4:(u + 1) * 4, :].rearrange("p h n -> p (h n)"),
                                               in_=bk(pb)[:, :], mul=128.0 ** -0.5), R=[banks[pb]], W=[QT])
            wu = get_unit()
            for hg in range(2):
                pb = 2 + hg
                first = True
                for h4 in range(4):
                    h = hg * 4 + h4
                    for c in range(8):
                        op("pe", lambda e, h=h, h4=h4, c=c, first=first: e.matmul(
                            bk(pb)[0:64, h4 * 128:(h4 + 1) * 128], lhsT=wu.c(c)[:, h * 64:(h + 1) * 64],
                            rhs=hTq.ap[:, c, :], start=first, stop=(c == 7), skip_group_check=True),
                           R=[wu.b(c), hTq], W=[banks[pb]])
                        first = False
                op("dve", lambda e, hg=hg: e.tensor_scalar(
                    out=qiT.ap[0:64, hg * 4:(hg + 1) * 4, :].rearrange("p h n -> p (h n)"), in0=bk(pb)[0:64, :],
                    scalar1=64.0 ** -0.5, scalar2=None, op0=ALU.mult), R=[banks[pb]], W=[qiT])
            for c in range(8):
                op("pe", lambda e, c=c: e.matmul(bk(4)[:, 0:8], lhsT=hTq.ap[:, c, :], rhs=wwi.ap[:, c, :],
                                                 start=(c == 0), stop=(c == 7)), R=[hTq, wwi], W=[banks[4]])
            op("dve", lambda e: e.tensor_scalar(out=sm.ap[:, 8:16], in0=bk(4)[:, 0:8], scalar1=8.0 ** -0.5, scalar2=None,
                                                op0=ALU.mult), R=[banks[4]], W=[sm])
            for h in range(8):
                op("dve", lambda e, h=h: e.tensor_scalar(out=diagw.ap[:, h, :], in0=ident_f.ap[:],
                                                         scalar1=sm.ap[:, 8 + h:9 + h], scalar2=None, op0=ALU.mult),
                   R=[ident_f, sm], W=[diagw])
            for u in range(2):
                wu = get_unit()
                pb = u % 2
                for c in range(8):
                    op("pe", lambda e, c=c: e.matmul(bk(pb)[:, :], lhsT=hTq.ap[:, c, :], rhs=wu.c(c),
                                                     start=(c == 0), stop=(c == 7)), R=[wu.b(c), hTq], W=[banks[pb]])
                op("act", lambda e, u=u: e.activation(out=zs.ap[:, u * 512:(u + 1) * 512], in_=bk(pb)[:, :], func=AF.Silu),
                   R=[banks[pb]], W=[zs])

            kv_slots_free_of_weights()
            kvst["limit"] = kvst["next"] + n_kb // KVG
            kv_prefetch(kvst["next"] + KVS - 1)
            fold_tokens(overlays, [big])
            tot = n_ch * 8
            DB = (0, 1, 2, 3, 6, 7)
            LAG = 3

            def wsum(n):
                c, h = n // 8, n % 8
                acc = 4 + (c % 2)
                R_ = Rb[n % 6]
                op("pe", lambda e: e.matmul(bk(acc)[:, :], lhsT=diagw.ap[:, h, :], rhs=R_.ap, start=(h == 0),
                                            stop=(h == 7)), R=[diagw, R_], W=[banks[acc]])
                if h == 7:
                    evac2(lambda e: e.copy(out=big.ap[:, c * 512:(c + 1) * 512], in_=bk(acc)[:, :]),
                          lambda e: e.tensor_copy(out=big.ap[:, c * 512:(c + 1) * 512], in_=bk(acc)[:, :]),
                          R=[banks[acc]], W=[big])

            for n in range(tot):
                c, h = n // 8, n % 8
                pb = DB[n % 6]
                R_ = Rb[n % 6]
                op("pe", lambda e: e.matmul(bk(pb)[:, :], lhsT=qiT.ap[:, h, :], rhs=kiT.ap[:, c * 512:(c + 1) * 512],
                                            start=True, stop=True), R=[qiT, kiT], W=[banks[pb]])
                if n % 2 == 0:
                    op("act", lambda e: e.activation(out=R_.ap, in_=bk(pb)[:, :], func=AF.Relu), R=[banks[pb]], W=[R_])
                else:
                    op("dve", lambda e: e.tensor_scalar(out=R_.ap, in0=bk(pb)[:, :], scalar1=0.0, scalar2=None,
                                                        op0=ALU.max), R=[banks[pb]], W=[R_])
                if n >= LAG:
                    wsum(n - LAG)
            for n in range(max(0, tot - LAG), tot):
                wsum(n)

            isc = big.ap[:, 0:N]
            nA = (int(N * ACT_FRAC) // 64) * 64 if USE_ACT_COUNT else 0
            nD = N - nA
            op("dve", lambda e: e.tensor_reduce(out=sm.ap[:, 16:17], in_=big.ap[:, 0:512], axis=AX.X, op=ALU.min), R=[big], W=[sm])
            op("dve", lambda e: e.tensor_reduce(out=sm.ap[:, 17:18], in_=isc, axis=AX.X, op=ALU.max), R=[big], W=[sm])
            op("dve", lambda e: e.tensor_tensor(out=big.ap[:, N - 512:N], in0=big.ap[:, N - 512:N], in1=admis.ap[:],
                                                op=ALU.add), R=[big, admis], W=[big])
            op("dve", lambda e: e.tensor_scalar(out=bis.ap[:, 0:1], in0=sm.ap[:, 16:17], scalar1=-1.0, scalar2=None,
                                                op0=ALU.add), R=[sm], W=[bis])
            op("dve", lambda e: e.tensor_tensor(out=sm.ap[:, 18:19], in0=sm.ap[:, 17:18], in1=sm.ap[:, 16:17],
                                                op=ALU.subtract), R=[sm], W=[sm])
            op("dve", lambda e: e.tensor_scalar(out=sm.ap[:, 18:19], in0=sm.ap[:, 18:19], scalar1=2.0, scalar2=None,
                                                op0=ALU.add), R=[sm], W=[sm])
            op("dve", lambda e: e.tensor_scalar(out=Wt.ap[:], in0=cpow.ap[:], scalar1=sm.ap[:, 18:19], scalar2=None,
                                                op0=ALU.mult), R=[cpow, sm], W=[Wt])
            op("dve", lambda e: e.tensor_tensor(out=bis.ap[:, 1:2], in0=bis.ap[:, 0:1], in1=Wt.ap[:, 0:1], op=ALU.add),
               R=[bis, Wt], W=[bis])
            for k in range(KBIS):
                if nA > 0:
                    npc = 0
                    for p0 in range(nD, N, 3072):
                        sz = min(3072, N - p0)
                        op("act", lambda e, p0=p0, sz=sz, npc=npc: e.activation(
                            out=Rb_flat[:, 0:sz], in_=big.ap[:, p0:p0 + sz], func=AF.Sign, bias=bis.ap[:, 1:2], scale=-1.0,
                            accum_out=bisa.ap[:, npc:npc + 1]), R=[big, bis], W=[bisa] + Rb)
                        npc += 1
                op("dve", lambda e: e.tensor_scalar(out=junk_dve.ap[:, 0:1].to_broadcast([128, nD]), in0=big.ap[:, 0:nD],
                                                    scalar1=bis.ap[:, 1:2], scalar2=None, op0=ALU.is_ge, op1=ALU.add,
                                                    accum_out=bisc.ap[:, 0:1]), R=[big, bis], W=[bisc, junk_dve])
                if nA > 0:
                    if npc > 1:
                        op("dve", lambda e, npc=npc: e.tensor_reduce(out=bisa.ap[:, 7:8], in_=bisa.ap[:, 0:npc], axis=AX.X,
                                                                     op=ALU.add), R=[bisa], W=[bisa])
                    sa_col = 7 if npc > 1 else 0
                    op("dve", lambda e: e.scalar_tensor_tensor(out=bisc.ap[:, 1:2], in0=bisc.ap[:, 0:1], scalar=2.0,
                                                               in1=bisa.ap[:, sa_col:sa_col + 1], op0=ALU.mult,
                                                               op1=ALU.subtract), R=[bisc, bisa], W=[bisc])
                    op("dve", lambda e, k=k: e.tensor_scalar(out=bis.ap[:, 3:4], in0=bisc.ap[:, 1:2],
                                                             scalar1=float(2 * TOPK - 1 - nA), scalar2=Wt.ap[:, k:k + 1],
                                                             op0=ALU.is_ge, op1=ALU.mult), R=[bisc, Wt], W=[bis])
                else:
                    op("dve", lambda e, k=k: e.tensor_scalar(out=bis.ap[:, 3:4], in0=bisc.ap[:, 0:1], scalar1=TOPK - 0.5,
                                                             scalar2=Wt.ap[:, k:k + 1], op0=ALU.is_ge, op1=ALU.mult),
                       R=[bisc, Wt], W=[bis])
                op("dve", lambda e, k=k: e.scalar_tensor_tensor(out=bis.ap[:, 1:2], in0=bis.ap[:, 1:2],
                                                                scalar=Wt.ap[:, k + 1:k + 2], in1=bis.ap[:, 3:4],
                                                                op0=ALU.subtract, op1=ALU.add), R=[bis, Wt], W=[bis])
            op("dve", lambda e: e.tensor_tensor(out=bis.ap[:, 0:1], in0=bis.ap[:, 1:2], in1=Wt.ap[:, KBIS:KBIS + 1],
                                                op=ALU.subtract), R=[bis, Wt], W=[bis])
            if dbg:
                dma(out=dbg_thr[j * 128:(j + 1) * 128, :], in_=bis.ap[:, 0:4], anchor=dmB, R=[bis])
                dma(out=dbg_isc[j * 128:(j + 1) * 128, 0:N], in_=big.ap[:, 0:N], anchor=dmD, R=[big])

            obank = lambda h: 4 + h // 3
            ooff = lambda h: (h % 3) * 129
            steps = []
            pend = []

            def emit_pv(item):
                kb, hg, kbuf, vbuf, kbl, pt = item
                for h4 in range(4):
                    h = hg * 4 + h4
                    ob = obank(h)
                    first = (kb == 0 and h % 3 == 0)
                    op("pe", lambda e, h=h, h4=h4, ob=ob, first=first: e.matmul(
                        bk(ob)[:, ooff(h):ooff(h) + 129], lhsT=pt.ap[:, h4 * 128:(h4 + 1) * 128],
                        rhs=vbuf.ap[:, kbl, h * 129:(h + 1) * 129], start=first, stop=(kb == n_kb - 1),
                        skip_group_check=True), R=[pt, vbuf], W=[banks[ob]])

            nqk = 0
            for gk in range(n_kb // KVG):
                kbuf, vbuf = get_kv()
                for kbl in range(KVG):
                    kb = gk * KVG + kbl
                    c = kb // 4
                    if kb % 4 == 0:
                        mbuf = mb[c % 2]
                        op("dve", lambda e, c=c, mbuf=mbuf: e.tensor_scalar(
                            out=mbuf.ap[:], in0=big.ap[:, c * 512:(c + 1) * 512], scalar1=bis.ap[:, 0:1], scalar2=NEG,
                            op0=ALU.is_lt, op1=ALU.mult), R=[big, bis], W=[mbuf])
                    mbuf = mb[c % 2]
                    near_i = kb - (4 * j - 1)
                    near = near_i >= 0
                    for hg in range(2):
                        lb = nqk % 4
                        pt = PT[nqk % 4]
                        nqk += 1
                        op("pe", lambda e, lb=lb, kb=kb, mbuf=mbuf: e.matmul(
                            bk(lb)[:, :], lhsT=mbuf.ap[:, (kb % 4) * 128:(kb % 4 + 1) * 128], rhs=ident4.ap[:],
                            start=True, stop=False, skip_group_check=True), R=[mbuf, ident4], W=[banks[lb]])
                        for h4 in range(4):
                            h = hg * 4 + h4
                            op("pe", lambda e, lb=lb, h=h, h4=h4, kbl=kbl, kbuf=kbuf, near=near: e.matmul(
                                bk(lb)[:, h4 * 128:(h4 + 1) * 128], lhsT=kbuf.ap[:, kbl, h * 128:(h + 1) * 128],
                                rhs=QT.ap[:, h, :], start=False, stop=(not near), skip_group_check=True),
                               R=[kbuf, QT], W=[banks[lb]])
                        if near:
                            for h4 in range(4):
                                h = hg * 4 + h4
                                op("pe", lambda e, lb=lb, h=h, h4=h4, near_i=near_i: e.matmul(
                                    bk(lb)[:, h4 * 128:(h4 + 1) * 128], lhsT=ident_b.ap[:], rhs=delta.ap[:, near_i, h, :],
                                    start=False, stop=True, skip_group_check=True), R=[ident_b, delta], W=[banks[lb]])
                        op("act", lambda e, lb=lb, pt=pt: e.activation(out=pt.ap[:], in_=bk(lb)[:, :], func=AF.Exp),
                           R=[banks[lb]], W=[pt])
                        pend.append((kb, hg, kbuf, vbuf, kbl, pt))
                        if len(pend) > PV_LAG:
                            emit_pv(pend.pop(0))
            while pend:
                emit_pv(pend.pop(0))
            wst["released"].add(j + 1)
            w_pump()

            fold_tokens([big], overlays)
            for b3, nh in ((4, 3), (5, 3), (6, 2)):
                h0 = (b3 - 4) * 3
                op("dve", lambda e, b3=b3, nh=nh, h0=h0: e.reciprocal(
                    out=sm.ap[:, 24 + h0:24 + h0 + nh],
                    in_=bk(b3)[:, 0:nh * 129].rearrange("p (h n) -> p h n", n=129)[:, :, 128]),
                   R=[banks[b3]], W=[sm])
            for h in range(8):
                op("dve", lambda e, h=h: e.scalar_tensor_tensor(
                    out=o_y.ap[:, h * 128:(h + 1) * 128], in0=bk(obank(h))[:, ooff(h):ooff(h) + 128],
                    scalar=sm.ap[:, 24 + h:25 + h], in1=zs.ap[:, h * 128:(h + 1) * 128], op0=ALU.mult, op1=ALU.mult),
                   R=[banks[obank(h)], sm, zs], W=[o_y])
            transpose_to(o_y, lambda c: o_y.ap[:, c * 128:(c + 1) * 128], 8, o_yT, lambda r0: o_yT.ap[:, :])
            for nh in range(2):
                wu = get_unit()
                pb = nh
                for c in range(8):
                    op("pe", lambda e, c=c: e.matmul(bk(pb)[:, :], lhsT=o_yT.ap[:, c * 128:(c + 1) * 128], rhs=wu.c(c),
                                                     start=(c == 0), stop=(c == 7)), R=[wu.b(c), o_yT], W=[banks[pb]])
                op("dve", lambda e, nh=nh: e.tensor_tensor(out=o_x1.ap[:, nh * 512:(nh + 1) * 512], in0=bk(pb)[:, :],
                                                           in1=xt.ap[:, nh * 512:(nh + 1) * 512], op=ALU.add),
                   R=[banks[pb], xt], W=[o_x1])
            if dbg:
                dma(out=dbg_x1[j * 128:(j + 1) * 128, :], in_=o_x1.ap[:, :], anchor=dmC, R=[o_x1])

            rms_rstd(o_x1, o_x1.ap[:, :], 32)
            op("dve", lambda e: e.tensor_scalar(out=o_h1.ap[:, :], in0=o_x1.ap[:, :], scalar1=sm.ap[:, 32:33], scalar2=None,
                                                op0=ALU.mult), R=[o_x1, sm], W=[o_h1])
            transpose_to(o_h1, lambda c: o_h1.ap[:, c * 128:(c + 1) * 128], 8, o_h1T, lambda r0: o_h1T.ap[:, :])

            def proj_unit(pb):
                wu = get_unit()
                for c in range(8):
                    op("pe", lambda e, c=c: e.matmul(bk(pb)[:, :], lhsT=o_h1T.ap[:, c * 128:(c + 1) * 128], rhs=wu.c(c),
                                                     start=(c == 0), stop=(c == 7)), R=[wu.b(c), o_h1T], W=[banks[pb]])
            for uu in range(4):
                pb = uu % 3
                proj_unit(pb)
                op("act", lambda e, uu=uu, pb=pb: e.copy(out=o_v.ap[:, uu * 512:(uu + 1) * 512], in_=bk(pb)[:, :]),
                   R=[banks[pb]], W=[o_v])
                op("dve", lambda e, uu=uu: e.bn_stats(out=sm.ap[:, 36 + 6 * uu:42 + 6 * uu], in_=o_v.ap[:, uu * 512:(uu + 1) * 512]),
                   R=[o_v], W=[sm])
            op("dve", lambda e: e.bn_aggr(out=sm.ap[:, 60:62], in_=sm.ap[:, 36:60]), R=[sm], W=[sm])
            op("dve", lambda e: e.tensor_scalar(out=sm.ap[:, 62:63], in0=sm.ap[:, 61:62], scalar1=LN_EPS, scalar2=None,
                                                op0=ALU.add), R=[sm], W=[sm])
            op("act", lambda e: e.sqrt(out=sm.ap[:, 63:64], in_=sm.ap[:, 62:63]), R=[sm], W=[sm])
            op("dve", lambda e: e.reciprocal(out=sm.ap[:, 62:63], in_=sm.ap[:, 63:64]), R=[sm], W=[sm])
            op("dve", lambda e: e.tensor_scalar(out=o_v.ap[:, :], in0=o_v.ap[:, :], scalar1=sm.ap[:, 60:61],
                                                scalar2=sm.ap[:, 62:63], op0=ALU.subtract, op1=ALU.mult), R=[o_v, sm], W=[o_v])
            op("dve", lambda e: e.tensor_tensor(out=o_v.ap[:, :], in0=o_v.ap[:, :], in1=lng.ap[:], op=ALU.mult),
               R=[o_v, lng], W=[o_v])
            op("dve", lambda e: e.tensor_tensor(out=o_vln.ap[:, :], in0=o_v.ap[:, :], in1=lnb.ap[:], op=ALU.add),
               R=[o_v, lnb], W=[o_vln])
            for g in range(8):
                mbk = 3 + g // 2
                op("pe", lambda e, g=g, mbk=mbk: e.matmul(bk(mbk)[:, (g % 2) * 256:(g % 2 + 1) * 256], lhsT=wsTm.ap[:, g, :],
                                                          rhs=o_vln.ap[:, g * 256:(g + 1) * 256], start=(g % 2 == 0),
                                                          stop=True, skip_group_check=True), R=[wsTm, o_vln], W=[banks[mbk]])
            for uu in range(4):
                pb = uu % 3
                proj_unit(pb)
                op("act", lambda e, uu=uu, pb=pb: e.copy(out=o_u.ap[:, uu * 512:(uu + 1) * 512], in_=bk(pb)[:, :]),
                   R=[banks[pb]], W=[o_u])
            for g in range(8):
                mbk = 3 + g // 2
                op("dve", lambda e, g=g, mbk=mbk: e.scalar_tensor_tensor(
                    out=o_um.ap[:, g * 256:(g + 1) * 256], in0=bk(mbk)[:, (g % 2) * 256:(g % 2 + 1) * 256],
                    scalar=bsT.ap[:, g:g + 1], in1=o_u.ap[:, g * 256:(g + 1) * 256], op0=ALU.add, op1=ALU.mult),
                   R=[banks[mbk], bsT, o_u], W=[o_um])
            for uu in range(4):
                pb = uu % 3
                proj_unit(pb)
                zb = o_zs1[uu % 2]
                op("act", lambda e, pb=pb, zb=zb: e.activation(out=zb.ap[:, :], in_=bk(pb)[:, :], func=AF.Silu),
                   R=[banks[pb]], W=[zb])
                op("dve", lambda e, uu=uu, zb=zb: e.tensor_tensor(out=o_y1.ap[:, uu * 512:(uu + 1) * 512], in0=zb.ap[:, :],
                                                                   in1=o_um.ap[:, uu * 512:(uu + 1) * 512], op=ALU.mult),
                   R=[zb, o_um], W=[o_y1])
            transpose_to(o_y1, lambda c: o_y1.ap[:, c * 128:(c + 1) * 128], 16, o_y1T,
                         lambda r0: o_y1T.ap[:, r0 * 128:(r0 + 8) * 128])
            for nh in range(2):
                pb = nh
                for kh in range(2):
                    wu = get_unit()
                    for c in range(8):
                        kc = kh * 8 + c
                        op("pe", lambda e, c=c, kc=kc: e.matmul(bk(pb)[:, :], lhsT=o_y1T.ap[:, kc * 128:(kc + 1) * 128],
                                                                rhs=wu.c(c), start=(kc == 0), stop=(kc == 15)),
                           R=[wu.b(c), o_y1T], W=[banks[pb]])
                op("dve", lambda e, nh=nh: e.tensor_tensor(out=o_x2.ap[:, nh * 512:(nh + 1) * 512], in0=bk(pb)[:, :],
                                                           in1=o_x1.ap[:, nh * 512:(nh + 1) * 512], op=ALU.add),
                   R=[banks[pb], o_x1], W=[o_x2])
            rms_rstd(o_x2, o_x2.ap[:, :], 20)
            op("dve", lambda e: e.scalar_tensor_tensor(out=o_o.ap[:, :], in0=o_x2.ap[:, :], scalar=sm.ap[:, 20:21],
                                                       in1=gf.ap[:], op0=ALU.mult, op1=ALU.mult), R=[o_x2, sm, gf], W=[o_o])
            dma(out=out_d[j * 128:(j + 1) * 128, :], in_=o_o.ap[:, :], anchor=dmA, R=[o_o])
        Sx.barrier()
        print("instructions:", Sx.nins, "waits:", Sx.nwait, "sems:", len(Sx.anchors) + 5)
    return nc


def _t5_bucket_np(rel):
    import jax
    import jax.numpy as jnp
    with jax.default_device(jax.devices("cpu")[0]):
        return _t5_bucket_cpu(jnp, rel)


def _t5_bucket_cpu(jnp, rel):
    rel = jnp.asarray(rel, dtype=jnp.int32)
    half = 16
    max_exact = 8
    ret = jnp.where(rel < 0, half, 0)
    n = jnp.abs(rel)
    nf = jnp.maximum(n, 1).astype(jnp.float32)
    large = max_exact + (jnp.log(nf / max_exact) / math.log(128 / max_exact) * (half - max_exact)).astype(jnp.int32)
    large = jnp.minimum(large, half - 1)
    return np.asarray(ret + jnp.where(n < max_exact, n, large))


_NC_CACHE = {}


def _host_inputs(x, norm_g, final_g, rel_bias, a_w_in, a_w_out, b_w_in, b_ln_g, b_ln_b, b_w_s, b_b_s, b_w_out):
    B, S, _ = x.shape
    NBLK = S // 128
    NQ = NBLK // 4
    f = np.float32
    bc = lambda v, n: np.ascontiguousarray(np.broadcast_to(np.asarray(v, f)[None, :], (128, n)))
    common = {
        "w_in0": np.ascontiguousarray(a_w_in[0], f), "w_out0": np.ascontiguousarray(a_w_out[0], f),
        "w_in1": np.ascontiguousarray(b_w_in[0], f), "w_out1": np.ascontiguousarray(b_w_out[0], f),
        "g0c": np.ascontiguousarray(np.asarray(norm_g[0], f).reshape(8, 128).T),
        "g1c": np.ascontiguousarray(np.asarray(norm_g[1], f).reshape(8, 128).T),
        "gf": bc(final_g, D), "lng": bc(b_ln_g[0], 2048), "lnb": bc(b_ln_b[0], 2048),
        "bsT": np.ascontiguousarray(np.asarray(b_b_s[0], f).T),
        "wsT": np.ascontiguousarray(np.transpose(np.asarray(b_w_s[0], f), (2, 0, 1))),
        "tri": np.ascontiguousarray(np.triu(np.ones((128, 128), f))),
        "cb": bc(np.asarray(rel_bias, f)[15], 8),
        "ident": np.eye(128, dtype=f),
    }
    s_loc = np.arange(128)[:, None]
    t_loc = np.arange(128)[None, :]
    rb = np.asarray(rel_bias, f)
    in_maps = []
    for c in range(8):
        b, r = c // 4, c % 4
        m = dict(common)
        m["xb"] = np.ascontiguousarray(x[b], f)
        xr = np.asarray(x[b], f).reshape(NBLK, 128, D)
        m["xq"] = np.ascontiguousarray(xr[r::4].reshape(NQ * 128, D))
        bt = np.zeros((128, 5, 8, 128), f)
        for i in range(5):
            rel = (t_loc - s_loc) - 128 * (i - 1 - r)
            bidx = _t5_bucket_np(rel)
            bt[:, i, :, :] = np.transpose(rb[bidx], (0, 2, 1))
        m["biasT"] = np.ascontiguousarray(bt.reshape(128, 5 * 8 * 128))
        ad = np.zeros((128, 4, 128), f)
        for rp in range(4):
            if rp > r:
                ad[:, rp, :] = -1e30
            elif rp == r:
                ad[:64, rp, 64:] = -1e30
        m["admis"] = np.ascontiguousarray(ad.reshape(128, 512))
        in_maps.append(m)
    return in_maps, NBLK, NQ


def kernel(x, norm_g, final_g, rel_bias, a_w_in, a_w_out, b_w_in, b_ln_g, b_ln_b, b_w_s, b_b_s, b_w_out, _dbg=False):
    x = np.asarray(x)
    in_maps, NBLK, NQ = _host_inputs(x, norm_g, final_g, rel_bias, a_w_in, a_w_out, b_w_in, b_ln_g, b_ln_b,
                                     b_w_s, b_b_s, b_w_out)
    key = (NBLK, NQ, _dbg)
    if key not in _NC_CACHE:
        _NC_CACHE[key] = build(NBLK, NQ, dbg=_dbg)
    nc = _NC_CACHE[key]
    res = run_bass_kernel_spmd(nc, in_maps, core_ids=list(range(8)))
    B, S, _ = x.shape
    out = np.zeros((B, NBLK, 128, D), np.float32)
    for c in range(8):
        b, r = c // 4, c % 4
        out[b, r::4] = np.asarray(res.results[c]["out"]).reshape(NQ, 128, D)
    out = out.reshape(B, S, D)
    if _dbg:
        return out, res
    return out
```
